# Optimizing a Trainium2 kernel written in Bass

```python
import math
import jax
import jax.numpy as jnp
from jax import lax
import numpy as np

D_MODEL = 1024
BATCH = 16
SEQ = 2048
DEPTH = 2

CTX_LEN = 256
GRID_W = 64
HEAD_DIM = 64
N_GROUPS = 4
GROUP_W = D_MODEL // N_GROUPS
MIX_W = N_GROUPS * GROUP_W
HEADS_PER_GROUP = GROUP_W // HEAD_DIM
CHUNK = 128
DIFF_D = HEAD_DIM // 2
NA_ROWS = 8
NA_COLS = 16
NA_QCOLS = 16
NA_KCOLS = 32
N_EXPERTS = 16
EXPERT_FF = D_MODEL
CAPACITY_FACTOR = 2
ROPE_BASE = 10000.0
EPS = 1e-6
DIFF_Q_BLOCK = 128
A_END = 2 * GROUP_W
B_END = A_END + GROUP_W
C_END = B_END + 3 * GROUP_W
IN_W = C_END + 3 * GROUP_W

kernel_name = "hybrid_diffusion_parallel_mixers_ec_moe"


def rms_norm(x, g=None, eps=EPS):
    xf = x.astype(jnp.float32)
    y = xf * lax.rsqrt(jnp.mean(xf * xf, axis=-1, keepdims=True) + eps)
    if g is not None:
        y = y * g.astype(jnp.float32)
    return y.astype(x.dtype)


def axial_rope_tables(n_tokens, dim):
    half = dim // 2
    inv = 1.0 / (ROPE_BASE ** (jnp.arange(0, half, 2, dtype=jnp.float32) / half))
    t = jnp.arange(n_tokens)
    row = (t // GRID_W).astype(jnp.float32)[:, None] * inv
    col = (t % GRID_W).astype(jnp.float32)[:, None] * inv
    return (jnp.cos(row), jnp.sin(row), jnp.cos(col), jnp.sin(col))


def _rotate(x, cos, sin):
    x1, x2 = jnp.split(x, 2, axis=-1)
    return jnp.concatenate([x1 * cos - x2 * sin, x2 * cos + x1 * sin], axis=-1)


def apply_axial_rope(x, rope):
    n = x.shape[1]
    shp = (1, n) + (1,) * (x.ndim - 3) + (-1,)
    cr, sr, cc, sc = [t.reshape(shp).astype(x.dtype) for t in rope]
    xr, xc = jnp.split(x, 2, axis=-1)
    return jnp.concatenate([_rotate(xr, cr, sr), _rotate(xc, cc, sc)], axis=-1)


def chunk_spatial_gating(p, w_s, b_s):
    bn, n, _ = p.shape
    z = jax.nn.gelu(p)
    u, v = jnp.split(z, 2, axis=-1)
    v = rms_norm(v.reshape(bn, n // CHUNK, CHUNK, HEADS_PER_GROUP, HEAD_DIM))
    sv = jnp.einsum('hpq,bnqhc->bnphc', w_s, v) + b_s.T[None, None, :, :, None]
    return u * sv.reshape(bn, n, GROUP_W)


def fourier_mix(p):
    bn, n, _ = p.shape
    z = p.astype(jnp.float32).reshape(bn, n, HEADS_PER_GROUP, HEAD_DIM)
    f = jnp.fft.fft2(z, axes=(1, 3), norm="ortho").real
    return f.reshape(bn, n, GROUP_W).astype(p.dtype)


def lambda_value(lam, lam_init):
    lf = lam.astype(jnp.float32)
    return jnp.exp(jnp.sum(lf[0] * lf[1])) - jnp.exp(jnp.sum(lf[2] * lf[3])) + lam_init


def diff_attn_core(q, k, v, lam):
    s = jnp.einsum('bqhmd,bkhmd->bhmqk', q, k).astype(jnp.float32) * (DIFF_D ** -0.5)
    a = jax.nn.softmax(s, axis=-1)
    w = a[:, :, 0] - lam * a[:, :, 1]
    return jnp.einsum('bhqk,bkhd->bqhd', w.astype(v.dtype), v)


def ctx_attention(q, k, v):
    s = jnp.einsum('bqhd,bkhd->bhqk', q, k).astype(jnp.float32) * (HEAD_DIM ** -0.5)
    p = jax.nn.softmax(s, axis=-1).astype(v.dtype)
    return jnp.einsum('bhqk,bkhd->bqhd', p, v)


def neighbourhood_attn_latent(q, k, v, kc, vc, rpb):
    bn, n, h, dh = q.shape
    rows = n // GRID_W
    kr = min(NA_ROWS, rows)
    n_cb = GRID_W // NA_QCOLS
    scale = HEAD_DIM ** -0.5
    qg = q.reshape(bn, rows, n_cb, NA_QCOLS, h, dh).transpose(1, 0, 2, 3, 4, 5)
    kg = k.reshape(bn, rows, GRID_W, h, dh)
    vg = v.reshape(bn, rows, GRID_W, h, dh)
    qcol = jnp.arange(GRID_W).reshape(n_cb, NA_QCOLS)
    kstart = jnp.clip(qcol[:, 0] - NA_COLS // 2, 0, GRID_W - NA_KCOLS)
    kcol = kstart[:, None] + jnp.arange(NA_KCOLS)
    wstart = jnp.clip(qcol - NA_COLS // 2, 0, GRID_W - NA_COLS)
    kc3 = kcol[:, None, :]
    valid = (kc3 >= wstart[..., None]) & (kc3 < wstart[..., None] + NA_COLS)
    coff = jnp.clip(kc3 - qcol[..., None], -(NA_COLS - 1), NA_COLS - 1) + NA_COLS - 1
    rpb_c = rpb[:, :, coff]
    n_loc = kr * NA_KCOLS

    def one_row(args):
        r, q_row = args
        rstart = jnp.clip(r - kr // 2, 0, rows - kr)
        k_rows = lax.dynamic_slice_in_dim(kg, rstart, kr, axis=1)
        v_rows = lax.dynamic_slice_in_dim(vg, rstart, kr, axis=1)
        k_blk = k_rows[:, :, kcol]
        v_blk = v_rows[:, :, kcol]
        roff = rstart + jnp.arange(kr) - r + NA_ROWS - 1
        bias = rpb_c[:, roff].transpose(0, 2, 3, 1, 4).astype(jnp.float32)
        s_loc = jnp.einsum('bjqhd,brjkhd->bhjqrk', q_row, k_blk).astype(jnp.float32) * scale + bias
        s_loc = jnp.where(valid[:, :, None, :], s_loc, -jnp.inf)
        s_loc = s_loc.reshape(bn, h, n_cb, NA_QCOLS, n_loc)
        s_ctx = jnp.einsum('bjqhd,bkhd->bhjqk', q_row, kc).astype(jnp.float32) * scale
        p = jax.nn.softmax(jnp.concatenate([s_loc, s_ctx], axis=-1), axis=-1).astype(v.dtype)
        p_loc = p[..., :n_loc].reshape(bn, h, n_cb, NA_QCOLS, kr, NA_KCOLS)
        p_ctx = p[..., n_loc:]
        o = (jnp.einsum('bhjqrk,brjkhd->bjqhd', p_loc, v_blk)
             + jnp.einsum('bhjqk,bkhd->bjqhd', p_ctx, vc))
        return o.reshape(bn, GRID_W, h * dh)

    out = lax.map(one_row, (jnp.arange(rows), qg))
    return out.transpose(1, 0, 2, 3).reshape(bn, n, h * dh)


def token_mixers(hx, hc, w_in, w_out, head_g, sgu_w, sgu_b, dqn, dkn, dlam,
                 nqn, nkn, rpb, rope, lam_init, ctx_out):
    bn, n, _ = hx.shape
    h = HEADS_PER_GROUP
    px = hx @ w_in
    pc = hc @ w_in
    lam = lambda_value(dlam, lam_init)
    head_scale = jnp.asarray(np.array([1.0] * (2 * h) + [1.0 - lam_init] * h + [1.0] * h,
                                      dtype=np.float32), dtype=hx.dtype)

    def groups(p):
        return p[..., :A_END], p[..., A_END:B_END], p[..., B_END:C_END], p[..., C_END:]

    def diff_qkv(p, use_rope):
        m = p.shape[1]
        q, k, v = jnp.split(p, 3, axis=-1)
        q = rms_norm(q.reshape(bn, m, h, 2, DIFF_D), dqn)
        k = rms_norm(k.reshape(bn, m, h, 2, DIFF_D), dkn)
        if use_rope:
            q = apply_axial_rope(q, rope)
            k = apply_axial_rope(k, rope)
        return q, k, v.reshape(bn, m, h, HEAD_DIM)

    def na_qkv(p):
        m = p.shape[1]
        q, k, v = jnp.split(p, 3, axis=-1)
        q = rms_norm(q.reshape(bn, m, h, HEAD_DIM), nqn)
        k = rms_norm(k.reshape(bn, m, h, HEAD_DIM), nkn)
        return q, k, v.reshape(bn, m, h, HEAD_DIM)

    def merge(ya, yb, yc, yd):
        y = jnp.concatenate([ya, yb, yc, yd], axis=-1)
        m = y.shape[1]
        y = rms_norm(y.reshape(bn, m, N_GROUPS * h, HEAD_DIM), head_g.reshape(N_GROUPS * h, HEAD_DIM))
        y = y * head_scale[:, None]
        return y.reshape(bn, m, MIX_W) @ w_out

    aX, bX, cX, dX = groups(px)
    aC, bC, cC, dC = groups(pc)

    cq_c, ck_c, cv_c = diff_qkv(cC, False)
    nq_c, nk_c, nv_c = na_qkv(dC)

    ya = chunk_spatial_gating(aX, sgu_w, sgu_b)
    yb = fourier_mix(bX)
    cq, ck, cv = diff_qkv(cX, True)
    k_all = jnp.concatenate([ck, ck_c], axis=1)
    v_all = jnp.concatenate([cv, cv_c], axis=1)
    qb = cq.reshape(bn, n // DIFF_Q_BLOCK, DIFF_Q_BLOCK, h, 2, DIFF_D).transpose(1, 0, 2, 3, 4, 5)
    yc = lax.map(lambda qblk: diff_attn_core(qblk, k_all, v_all, lam), qb)
    yc = yc.transpose(1, 0, 2, 3, 4).reshape(bn, n, GROUP_W)
    nq, nk, nv = na_qkv(dX)
    yd = neighbourhood_attn_latent(nq, nk, nv, nk_c, nv_c, rpb)
    y_lat = merge(ya, yb, yc, yd)

    if not ctx_out:
        return y_lat, None
    m = hc.shape[1]
    ya_c = chunk_spatial_gating(aC, sgu_w, sgu_b)
    yb_c = fourier_mix(bC)
    yc_c = diff_attn_core(cq_c, ck_c, cv_c, lam).reshape(bn, m, GROUP_W)
    yd_c = ctx_attention(nq_c, nk_c, nv_c).reshape(bn, m, GROUP_W)
    return y_lat, merge(ya_c, yb_c, yc_c, yd_c)


def expert_choice_ffn(x, router_w, wg, wu, wd):
    bn, n, _ = x.shape
    cap = CAPACITY_FACTOR * n // N_EXPERTS
    aff = jax.nn.softmax(jnp.einsum('bnd,de->bne', x, router_w).astype(jnp.float32), axis=-1)
    gate, idx = lax.top_k(aff.transpose(0, 2, 1), cap)
    bidx = jnp.arange(bn)[:, None, None]
    xg = x[bidx, idx]
    hid = jax.nn.silu(jnp.einsum('becd,edf->becf', xg, wg)) * jnp.einsum('becd,edf->becf', xg, wu)
    o = jnp.einsum('becf,efd->becd', hid, wd) * gate[..., None].astype(x.dtype)
    return jnp.zeros_like(x).at[bidx, idx].add(o)


def setup_inputs(seed: int = 0) -> dict:
    key = jax.random.key(seed)
    ks = jax.random.split(key, 24)
    f32 = jnp.float32
    L, D, H = DEPTH, D_MODEL, HEADS_PER_GROUP

    def nrm(k, shape, s):
        return jax.random.normal(k, shape, f32) * s

    return {
        "x": nrm(ks[0], (BATCH, SEQ, D), 1.0),
        "c": nrm(ks[1], (BATCH, D), 1.0),
        "ctx": nrm(ks[2], (BATCH, CTX_LEN, D), 1.0),
        "c_ctx": nrm(ks[3], (D,), 1.0),
        "ada_w": nrm(ks[4], (L, D, 6 * D), 0.5 * D ** -0.5),
        "ada_b": nrm(ks[5], (L, 6 * D), 0.02),
        "norm1_g": 1.0 + nrm(ks[6], (L, D), 0.02),
        "norm2_g": 1.0 + nrm(ks[7], (L, D), 0.02),
        "w_in": nrm(ks[8], (L, D, IN_W), D ** -0.5),
        "w_out": nrm(ks[9], (L, MIX_W, D), MIX_W ** -0.5),
        "head_out_g": 1.0 + nrm(ks[10], (L, MIX_W), 0.02),
        "sgu_w": nrm(ks[11], (L, H, CHUNK, CHUNK), CHUNK ** -0.5),
        "sgu_b": 1.0 + nrm(ks[12], (L, H, CHUNK), 0.02),
        "diff_qn_g": 1.0 + nrm(ks[13], (L, DIFF_D), 0.02),
        "diff_kn_g": 1.0 + nrm(ks[14], (L, DIFF_D), 0.02),
        "diff_lambda": nrm(ks[15], (L, 4, DIFF_D), 0.1),
        "na_qn_g": 1.0 + nrm(ks[16], (L, HEAD_DIM), 0.02),
        "na_kn_g": 1.0 + nrm(ks[17], (L, HEAD_DIM), 0.02),
        "na_rpb": nrm(ks[18], (L, H, 2 * NA_ROWS - 1, 2 * NA_COLS - 1), 0.1),
        "router_w": nrm(ks[19], (L, D, N_EXPERTS), D ** -0.5),
        "exp_w_gate": nrm(ks[20], (L, N_EXPERTS, D, EXPERT_FF), D ** -0.5),
        "exp_w_up": nrm(ks[21], (L, N_EXPERTS, D, EXPERT_FF), D ** -0.5),
        "exp_w_down": nrm(ks[22], (L, N_EXPERTS, EXPERT_FF, D), EXPERT_FF ** -0.5),
    }


def reference(x, c, ctx, c_ctx, ada_w, ada_b, norm1_g, norm2_g, w_in, w_out, head_out_g,
              sgu_w, sgu_b, diff_qn_g, diff_kn_g, diff_lambda, na_qn_g, na_kn_g, na_rpb,
              router_w, exp_w_gate, exp_w_up, exp_w_down):
    n = x.shape[1]
    rope = axial_rope_tables(n, DIFF_D)
    s_c = jax.nn.silu(c)
    s_cc = jax.nn.silu(c_ctx)
    xc = ctx
    for l in range(DEPTH):
        last = l == DEPTH - 1
        lam_init = 0.8 - 0.6 * math.exp(-0.3 * l)
        mod = (s_c @ ada_w[l] + ada_b[l])[:, None, :]
        mod_c = (s_cc @ ada_w[l] + ada_b[l])[None, None, :]
        sh1, sc1, g1, sh2, sc2, g2 = jnp.split(mod, 6, axis=-1)
        csh1, csc1, cg1, csh2, csc2, cg2 = jnp.split(mod_c, 6, axis=-1)
        hx = rms_norm(x, norm1_g[l]) * (1.0 + sc1) + sh1
        hc = rms_norm(xc, norm1_g[l]) * (1.0 + csc1) + csh1
        y_lat, y_ctx = token_mixers(hx, hc, w_in[l], w_out[l], head_out_g[l], sgu_w[l], sgu_b[l],
                                    diff_qn_g[l], diff_kn_g[l], diff_lambda[l],
                                    na_qn_g[l], na_kn_g[l], na_rpb[l], rope, lam_init,
                                    not last)
        x = x + g1 * y_lat
        x = x + g2 * expert_choice_ffn(rms_norm(x, norm2_g[l]) * (1.0 + sc2) + sh2,
                                       router_w[l], exp_w_gate[l], exp_w_up[l], exp_w_down[l])
        if not last:
            xc = xc + cg1 * y_ctx
            xc = xc + cg2 * expert_choice_ffn(rms_norm(xc, norm2_g[l]) * (1.0 + csc2) + csh2,
                                              router_w[l], exp_w_gate[l], exp_w_up[l], exp_w_down[l])
    return x
```

```python
import math
import numpy as np
import ml_dtypes
from contextlib import ExitStack
import concourse.bass as bass
import concourse.mybir as mybir
from concourse.bass_utils import run_bass_kernel_spmd

F32 = mybir.dt.float32
BF16 = mybir.dt.bfloat16
I32 = mybir.dt.int32
U32 = mybir.dt.uint32
AF = mybir.ActivationFunctionType
ALU = mybir.AluOpType
AX = mybir.AxisListType

D = 1024
SEQ = 2048
CTXL = 256
NT = 16
NTC = 2
NTA = NT + NTC
DEPTH = 2
IN_W = 2304
NE = 16
CAP = 256
CAPC = 32
EPS = 1e-6
NEGBIG = -30000.0
LAST_NAMES = {}


class Sched:
    NDS = 24

    def __init__(self, nc, stack):
        self.nc = nc
        self.stack = stack
        self.E = dict(pe=nc.tensor, act=nc.scalar, dve=nc.vector, pool=nc.gpsimd, sp=nc.sync)
        self.esem = {}
        self.ecnt = {}
        self.etok = {e: None for e in self.E}
        self.nsem = 0
        for e in self.E:
            self._newsem(e)
        self.dsem = [stack.enter_context(nc.semaphore(f"dq{i}")) for i in range(self.NDS)]
        self.dcnt = [0] * self.NDS
        self.dnext = {'sp': 0, 'pool': 0, 'act': 0}
        self.drange = {'sp': (0, 14), 'pool': (14, 24), 'act': (0, 14)}
        self.last = {}
        self.waited = {e: {} for e in self.E}
        self.pending = {e: [] for e in self.E}
        self.nwaits = 0
        self.ninst = 0

    def _newsem(self, e):
        self.esem[e] = self.stack.enter_context(self.nc.semaphore(f"s_{e}_{self.nsem}"))
        self.nsem += 1
        self.ecnt[e] = 0

    def _wait(self, e, tok):
        if tok is None:
            return
        sem, val, sid = tok[0], tok[1], tok[2]
        w = self.waited[e]
        if w.get(sid, 0) >= val:
            return
        w[sid] = val
        self.E[e].wait_ge(sem, val)
        self.nwaits += 1

    def _deps(self, e, reads, writes, is_pe=False):
        for k in reads:
            st = self.last.get(k)
            if st is not None:
                self._wait(e, st['w'])
                if k.startswith('ps'):
                    for rt in st['r']:
                        if rt[3] != e:
                            self._wait(e, rt)
        for k in writes:
            st = self.last.get(k)
            if st is not None:
                wt = st['w']
                if not (is_pe and wt is not None and wt[3] == 'pe'):
                    self._wait(e, wt)
                for rt in st['r']:
                    if is_pe and rt[3] == 'pe':
                        continue
                    self._wait(e, rt)

    def _record(self, tok, reads, writes):
        for k in reads:
            st = self.last.setdefault(k, {'w': None, 'r': []})
            st['r'].append(tok)
            if len(st['r']) > 48:
                st['r'] = st['r'][-48:]
        for k in writes:
            self.last[k] = {'w': tok, 'r': []}

    def op(self, e, fn, reads=(), writes=(), sig=True):
        self._deps(e, reads, writes, is_pe=(e == 'pe'))
        inst = fn()
        self.ninst += 1
        if not sig:
            self.pending[e].append((tuple(reads), tuple(writes)))
            return inst
        if self.ecnt[e] >= 30000:
            self._newsem(e)
        self.ecnt[e] += 1
        sem = self.esem[e]
        inst.then_inc(sem, 1)
        tok = (sem, self.ecnt[e], id(sem), e)
        self.etok[e] = tok
        for (r, w) in self.pending[e]:
            self._record(tok, r, w)
        self.pending[e] = []
        self._record(tok, reads, writes)
        return inst

    def dmaf(self, q, fn, reads=(), writes=()):
        lo, hi = self.drange[q]
        i = lo + self.dnext[q]
        self.dnext[q] = (self.dnext[q] + 1) % (hi - lo)
        sem = self.dsem[i]
        if self.dcnt[i] > 0:
            self._wait(q, (sem, 16 * self.dcnt[i], id(sem), 'dma'))
        self._deps(q, reads, writes)
        inst = fn()
        self.ninst += 1
        self.dcnt[i] += 1
        inst.then_inc(sem, 16)
        tok = (sem, 16 * self.dcnt[i], id(sem), 'dma')
        self._record(tok, reads, writes)
        return tok

    def dma(self, q, out, in_, reads=(), writes=(), **kw):
        return self.dmaf(q, lambda: self.E[q].dma_start(out=out, in_=in_, **kw), reads, writes)

    def barrier(self):
        for e in self.E:
            assert not self.pending[e], f"pending non-signaled ops on {e}"
        toks = [t for t in self.etok.values() if t is not None]
        for i in range(self.NDS):
            if self.dcnt[i] > 0:
                toks.append((self.dsem[i], 16 * self.dcnt[i], id(self.dsem[i]), 'dma'))
        for e in self.E:
            for t in toks:
                if t[3] == e:
                    continue
                self._wait(e, t)
        self.last = {}

    def finish(self):
        self.barrier()


def _na_structure():
    rows = 32
    variants = {}
    var_list = []
    per_t = []
    for t in range(NT):
        lst = []
        for kt in range(NT):
            sig = []
            anyv = False
            for a in range(2):
                for b_ in range(2):
                    r = 2 * t + b_
                    kr = 2 * kt + a
                    rs = min(max(r - 4, 0), rows - 8)
                    if rs <= kr <= rs + 7:
                        sig.append(kr - r + 7)
                        anyv = True
                    else:
                        sig.append(-1)
            if not anyv:
                continue
            sig = tuple(sig)
            if sig not in variants:
                variants[sig] = len(var_list)
                var_list.append(sig)
            lst.append((kt, variants[sig]))
        per_t.append(lst)
    return per_t, var_list


NA_PER_T, NA_VARS = _na_structure()
NVAR = len(NA_VARS)


def _na_bias_host(rpb):
    L, H = rpb.shape[0], rpb.shape[1]
    qc = np.arange(64)
    kc = np.arange(64)
    wstart = np.clip(qc - 8, 0, 48)
    valid_c = (kc[:, None] >= wstart[None, :]) & (kc[:, None] < wstart[None, :] + 16)
    coff = np.clip(kc[:, None] - qc[None, :], -15, 15) + 15
    flat = np.concatenate([rpb.reshape(L, H, 15 * 31), np.full((L, H, 1), NEGBIG, np.float32)], axis=-1)
    idx = np.full((NVAR, 128, 128), 15 * 31, np.int64)
    for v, sig in enumerate(NA_VARS):
        n = 0
        for a in range(2):
            for b_ in range(2):
                dr = sig[n]
                n += 1
                if dr < 0:
                    continue
                blk = np.where(valid_c, dr * 31 + coff, 15 * 31)
                idx[v, a * 64:(a + 1) * 64, b_ * 64:(b_ + 1) * 64] = blk
    out = flat[:, :, idx.reshape(-1)].reshape(L, H, NVAR, 128, 128)
    return np.ascontiguousarray(out.astype(np.float32))


def _consts():
    c = {}
    half = 16
    inv = 1.0 / (10000.0 ** (np.arange(0, half, 2, dtype=np.float32) / half))
    t = np.arange(SEQ)
    row = (t // 64).astype(np.float32)[:, None] * inv
    col = (t % 64).astype(np.float32)[:, None] * inv
    cr, sr, cc, sc = np.cos(row), np.sin(row), np.cos(col), np.sin(col)
    cos32 = np.concatenate([cr, cr, cc, cc], axis=1)
    sin32 = np.concatenate([-sr, sr, -sc, sc], axis=1)
    rope = np.stack([np.tile(cos32, (1, 16)), np.tile(sin32, (1, 16))], axis=1)
    c["ropeT"] = np.ascontiguousarray(rope.astype(np.float32))

    def dft(n, scale):
        k = np.arange(n, dtype=np.float64)
        ang = 2.0 * np.pi * np.outer(k, k) / n
        return np.cos(ang) * scale, -np.sin(ang) * scale
    cN, sN = dft(SEQ, 1.0 / math.sqrt(SEQ * 64.0))
    tb = np.stack([cN, sN]).reshape(2, NT, 128, NT, 128)
    c["dftN"] = np.ascontiguousarray(tb.transpose(3, 2, 0, 1, 4)).astype(ml_dtypes.bfloat16)
    cC, sC = dft(CTXL, 1.0 / math.sqrt(CTXL * 64.0))
    c["dftC"] = np.stack([cC, sC]).astype(ml_dtypes.bfloat16)
    c64, s64 = dft(64, 1.0)
    s64 = -s64
    cb = np.zeros((256, 256)); sb = np.zeros((256, 256))
    for h in range(4):
        cb[h * 64:(h + 1) * 64, h * 64:(h + 1) * 64] = c64
        sb[h * 64:(h + 1) * 64, h * 64:(h + 1) * 64] = s64
    c["dftH"] = np.stack([cb, sb]).astype(ml_dtypes.bfloat16)
    c["identb"] = np.eye(128).astype(ml_dtypes.bfloat16)
    c["identf"] = np.eye(128, dtype=np.float32)
    ob = np.zeros((48, 48), np.float32)
    for g in range(3):
        ob[g * 16:(g + 1) * 16, g * 16:(g + 1) * 16] = 1.0
    c["onesblk"] = ob
    return c


def build_program(stop_after=None, n_layers=DEPTH, samples=(0, 1)):
    nc = bass.Bass("TRN2", target_bir_lowering=False)
    dt_in = lambda name, shape, dt=F32: nc.dram_tensor(name, list(shape), dt, kind="ExternalInput").ap()
    x_in = dt_in("x", [2, SEQ, D])
    ctx_in = dt_in("ctx", [2, CTXL, D])
    cT_in = dt_in("cT", [128, 8, 3])
    ada_w = dt_in("ada_w", [DEPTH, D, 6 * D])
    ada_b = dt_in("ada_b", [DEPTH, 6 * D])
    n1g = dt_in("n1g", [DEPTH, D])
    n2g = dt_in("n2g", [DEPTH, D])
    w_in = dt_in("w_in", [DEPTH, D, IN_W])
    w_out = dt_in("w_out", [DEPTH, D, D])
    hog = dt_in("hog", [DEPTH, D])
    sguT = dt_in("sguT", [DEPTH, 4, 128, 128])
    sgubT = dt_in("sgubT", [DEPTH, 128, 4])
    qkgC = dt_in("qkgC", [DEPTH, 512])
    qkgD = dt_in("qkgD", [DEPTH, 512])
    dlam = dt_in("dlam", [DEPTH, 128])
    nbias = dt_in("nbias", [DEPTH, 4, NVAR, 128, 128])
    rw = dt_in("rw", [DEPTH, D, NE])
    wg = dt_in("wg", [DEPTH, NE, D, D])
    wu = dt_in("wu", [DEPTH, NE, D, D])
    wd = dt_in("wd", [DEPTH, NE, D, D])
    ropeT = dt_in("ropeT", [SEQ, 2, 512])
    dftN = dt_in("dftN", [NT, 128, 2, NT, 128], BF16)
    dftC = dt_in("dftC", [2, CTXL, CTXL], BF16)
    dftH = dt_in("dftH", [2, 256, 256], BF16)
    identb_in = dt_in("identb", [128, 128], BF16)
    identf_in = dt_in("identf", [128, 128])
    onesblk_in = dt_in("onesblk", [48, 48])
    out = nc.dram_tensor("out", [2, SEQ, D], F32, kind="ExternalOutput").ap()
    xc = nc.dram_tensor("xc_s", [2, CTXL, D], F32).ap()
    modd = nc.dram_tensor("modd_s", [DEPTH, 3, 6 * D], F32).ap()
    ynd = nc.dram_tensor("yn_s", [NTA * 128, D], BF16).ap()
    h2l = [nc.dram_tensor(f"h2l_s{b}", [SEQ, D], BF16).ap() for b in range(2)]
    h2c = [nc.dram_tensor(f"h2c_s{b}", [CTXL, D], BF16).ap() for b in range(2)]

    with ExitStack() as top:
        S = Sched(nc, top)

        uid = [0]

        def T(st, name, shape, dt):
            uid[0] += 1
            LAST_NAMES[name] = f"{name}__{uid[0]}"
            return st.enter_context(nc.sbuf_tensor(LAST_NAMES[name], list(shape), dt))

        def PS(st, name, shape, dt=F32):
            uid[0] += 1
            return st.enter_context(nc.psum_tensor(f"{name}__{uid[0]}", list(shape), dt))

        V, A_, G_, P_ = nc.vector, nc.scalar, nc.gpsimd, nc.tensor

        identb = T(top, "identb_t", [128, 128], BF16)
        identf = T(top, "identf_t", [128, 128], F32)
        onesblk = T(top, "onesblk_t", [48, 48], F32)
        Eaff = T(top, "Eaff", [48, SEQ + CTXL], F32)
        S.dma('sp', identb[:], identb_in[:, :], writes=['identb'])
        S.dma('sp', identf[:], identf_in[:, :], writes=['identf'])
        S.dma('sp', onesblk[:], onesblk_in[:, :], writes=['onesblk'])
        S.op('dve', lambda: V.memset(Eaff[:], 1.0), writes=['Eaff'])

        def run(g):
            for _ in g:
                pass

        def run_interleaved(gens):
            gens = list(gens)
            while gens:
                for g in list(gens):
                    try:
                        next(g)
                    except StopIteration:
                        gens.remove(g)

        def rstd_g(st_key, ssq_ap, n_inv):
            S.op('dve', lambda: V.tensor_scalar(out=ssq_ap, in0=ssq_ap, scalar1=n_inv, scalar2=EPS, op0=ALU.mult, op1=ALU.add),
                 reads=[st_key], writes=[st_key])
            yield
            S.op('act', lambda: A_.activation(out=ssq_ap, in_=ssq_ap, func=AF.Sqrt), reads=[st_key], writes=[st_key])
            yield
            S.op('dve', lambda: V.reciprocal(out=ssq_ap, in_=ssq_ap), reads=[st_key], writes=[st_key])
            yield

        def rstd_from_ssq(st_key, ssq_ap, n_inv, tmp_ap):
            run(rstd_g(st_key, ssq_ap, n_inv))

        def gnorm_g(src_ap, src_keys, G, E, sq_ap, sq_key, ss_ap, ss_key, out_ap, out_key, gain_ap=None, gain_key=None, sq_eng='act', gain_eng='dve'):
            if sq_eng == 'act':
                S.op('act', lambda: A_.activation(out=sq_ap, in_=src_ap, func=AF.Square), reads=src_keys, writes=[sq_key])
            else:
                S.op('pool', lambda: G_.tensor_tensor(out=sq_ap, in0=src_ap, in1=src_ap, op=ALU.mult), reads=src_keys, writes=[sq_key])
            yield
            S.op('dve', lambda: V.tensor_reduce(out=ss_ap, in_=sq_ap.rearrange("p (g e) -> p g e", e=E), axis=AX.X, op=ALU.add),
                 reads=[sq_key], writes=[ss_key])
            yield
            yield from rstd_g(ss_key, ss_ap, 1.0 / E)
            bc = ss_ap.unsqueeze(2).to_broadcast([128, G, E])
            if gain_ap is None:
                S.op('dve', lambda: V.tensor_tensor(out=out_ap.rearrange("p (g e) -> p g e", e=E),
                                                    in0=src_ap.rearrange("p (g e) -> p g e", e=E), in1=bc, op=ALU.mult),
                     reads=list(src_keys) + [ss_key], writes=[out_key])
                yield
            else:
                S.op('dve', lambda: V.tensor_tensor(out=sq_ap.rearrange("p (g e) -> p g e", e=E),
                                                    in0=src_ap.rearrange("p (g e) -> p g e", e=E), in1=bc, op=ALU.mult),
                     reads=list(src_keys) + [ss_key], writes=[sq_key])
                yield
                if gain_eng == 'pool':
                    S.op('pool', lambda: G_.tensor_tensor(out=out_ap, in0=sq_ap, in1=gain_ap, op=ALU.mult),
                         reads=[sq_key, gain_key], writes=[out_key])
                else:
                    S.op('dve', lambda: V.tensor_tensor(out=out_ap, in0=sq_ap, in1=gain_ap, op=ALU.mult),
                         reads=[sq_key, gain_key], writes=[out_key])
                yield

        def gnorm(*a, **k):
            run(gnorm_g(*a, **k))

        def bcast_row(st, q, dst, src_row_ap, key):
            S.dma(q, dst, src_row_ap.partition_broadcast(128), writes=[key])

        for l in range(n_layers):
            last = (l == DEPTH - 1)
            lam_init = 0.8 - 0.6 * math.exp(-0.3 * l)
            xsrc = (lambda b: x_in[b]) if l == 0 else (lambda b: out[b])
            csrc = (lambda b: ctx_in[b]) if l == 0 else (lambda b: xc[b])

            with ExitStack() as st:
                cT = T(st, "cT_t", [128, 8, 3], F32)
                sT = T(st, "sT_t", [128, 8, 3], F32)
                awb = [T(st, f"awb{i}", [128, 8, 512], F32) for i in range(2)]
                abt = T(st, "abt", [3, 6 * D], F32)
                modt = T(st, "modt", [3, 6 * D], F32)
                psm = PS(st, "psm", [128, 512])
                S.dma('sp', cT[:], cT_in[:, :, :], writes=['cT'])
                S.dma('sp', abt[:], ada_b[l].partition_broadcast(3), writes=['abt'])
                S.op('act', lambda: A_.activation(out=sT[:], in_=cT[:], func=AF.Silu), reads=['cT'], writes=['sT'])
                for nb in range(12):
                    bw = awb[nb % 2]
                    S.dma('sp', bw[:], ada_w[l][:, nb * 512:(nb + 1) * 512].rearrange("(c p) n -> p c n", p=128),
                          writes=[f'awb{nb % 2}'])
                    for c in range(8):
                        S.op('pe', lambda c=c, bw=bw: P_.matmul(psm[0:3, :], lhsT=sT[:, c, :], rhs=bw[:, c, :], start=(c == 0), stop=(c == 7)),
                             reads=['sT', f'awb{nb % 2}'], writes=['psm'], sig=(c == 7))
                    S.op('dve', lambda nb=nb: V.tensor_tensor(out=modt[:, nb * 512:(nb + 1) * 512], in0=psm[0:3, :],
                                                             in1=abt[:, nb * 512:(nb + 1) * 512], op=ALU.add),
                         reads=['psm', 'abt'], writes=['modt'])
                S.dma('sp', modd[l], modt[:], reads=['modt'], writes=['modd'])
                S.barrier()
            if stop_after == 'ada' and l == n_layers - 1:
                S.finish()
                return nc

            def modrow(r, k):
                return modd[l][r, k * D:(k + 1) * D]

            for b in samples:
                sb = ExitStack()
                Ub = T(sb, "Ub", [128, NTA, 512], BF16)
                hgb = T(sb, "hgb", [128, D], F32)
                with ExitStack() as st:
                    qTC = T(st, "qTC", [64, 4, NTA * 128], BF16)
                    kTC = T(st, "kTC", [64, 4, NTA * 128], BF16)
                    vC = T(st, "vC", [128, NTA, 4, 65], BF16)
                    qTD = T(st, "qTD", [128, 2, NTA * 128], BF16)
                    kTD = T(st, "kTD", [128, 2, NTA * 128], BF16)
                    vD = T(st, "vD", [128, NTA, 4, 65], BF16)
                    lamc = T(st, "lamc", [128, 4], F32)
                    S.op('pool', lambda: G_.memset(vC[:], 1.0), writes=['vC'])
                    S.op('pool', lambda: G_.memset(vD[:], 1.0), writes=['vD'])
                    bcast_row(st, 'sp', hgb[:], hog[l], 'hgb')
                    S.op('dve', lambda: V.tensor_scalar(out=hgb[:, 512:768], in0=hgb[:, 512:768], scalar1=1.0 - lam_init, scalar2=None, op0=ALU.mult),
                         reads=['hgb'], writes=['hgb'])

                    def head_norm_g(y_ap, y_keys, g, rows0, stw, pfx, q='sp', sq_eng='act'):
                        sq, ss, ynb = stw
                        yield from gnorm_g(y_ap, y_keys, 4, 64, sq[:, 0:256], pfx + 'sq', ss[:, 0:4], pfx + 'ss', ynb[:, :], pfx + 'ynb',
                                           gain_ap=hgb[:, g * 256:(g + 1) * 256], gain_key='hgb', sq_eng=sq_eng)
                        S.dma(q, ynd[rows0:rows0 + 128, g * 256:(g + 1) * 256], ynb[:, :], reads=[pfx + 'ynb'], writes=[f'ynd{rows0}_{g}'])
                        yield

                    def head_norm(*a, **k):
                        run(head_norm_g(*a, **k))

                    with ExitStack() as p1:
                        winb = T(p1, "winb", [128, 8, IN_W], BF16)
                        A1 = [T(p1, "A1_0", [128, D], F32)]
                        sh1 = [T(p1, "sh1_0", [128, D], F32)]
                        gC = T(p1, "gC", [128, 512], F32)
                        gD = T(p1, "gD", [128, 512], F32)
                        sguTb = T(p1, "sguTb", [128, 4, 128], BF16)
                        bsT = T(p1, "bsT", [128, 4], F32)
                        dHb = T(p1, "dHb", [128, 2, 2, 256], BF16)
                        xt = [T(p1, "xt0", [128, D], F32)]
                        rp = [T(p1, f"rp{i}", [128, 2, 512], F32) for i in range(2)]
                        sqx = T(p1, "sqx", [128, D], F32)
                        ssx = T(p1, "ssx", [128, 1], F32)
                        hxb = [T(p1, f"hxb{i}", [128, D], BF16) for i in range(2)]
                        hxT = [T(p1, f"hxT{i}", [128, 8, 128], BF16) for i in range(2)]
                        zA = [T(p1, f"zA{i}", [128, 512], F32) for i in range(2)]
                        qkC = [T(p1, f"qkC{i}", [128, 512], F32) for i in range(2)]
                        qkD = [T(p1, f"qkD{i}", [128, 512], F32) for i in range(2)]
                        vnb = T(p1, "vnb", [128, 256], BF16)
                        yA = T(p1, "yA", [128, 256], F32)
                        hn_sq = T(p1, "hn_sq", [128, 256], F32)
                        hn_ss = T(p1, "hn_ss", [128, 4], F32)
                        hn_yb = T(p1, "hn_yb", [128, 256], BF16)
                        hn_sq2 = T(p1, "hn_sqb", [128, 256], F32)
                        hn_ss2 = T(p1, "hn_ssb", [128, 4], F32)
                        ZTb = T(p1, "ZTb", [128, 2, 128], BF16)
                        qk2 = T(p1, "qk2", [128, 512], F32)
                        qk3 = T(p1, "qk3", [128, 512], F32)
                        qk4 = T(p1, "qk4", [128, 512], F32)
                        ssC = T(p1, "ssC", [128, 16], F32)
                        qkbC = T(p1, "qkbC", [128, 512], BF16)
                        sqD = T(p1, "sqD", [128, 512], F32)
                        ssD = T(p1, "ssD", [128, 8], F32)
                        qkbD = T(p1, "qkbD", [128, 512], BF16)
                        psP = [PS(p1, f"psP{i}", [128, 512]) for i in range(2)]
                        psT = PS(p1, "psT", [128, 8, 128], BF16)
                        psX = PS(p1, "psX", [128, 512])
                        psG1 = PS(p1, "psG1", [128, 512])
                        psQc = PS(p1, "psQc", [128, 8, 128], BF16)
                        psQd = PS(p1, "psQd", [128, 8, 128], BF16)

                        S.dmaf('pool', lambda: G_.dma_start(out=winb[:], in_=w_in[l].rearrange("(c p) n -> p c n", p=128)), writes=['winb'])
                        S.dmaf('pool', lambda: G_.dma_start(out=sguTb[:], in_=sguT[l].rearrange("h q p -> q h p")), writes=['sguTb'])
                        S.dmaf('pool', lambda: G_.dma_start(out=dHb[:], in_=dftH.rearrange("s (t p) n -> p s t n", p=128)), writes=['dHb'])
                        S.dma('sp', bsT[:], sgubT[l], writes=['bsT'])
                        bcast_row(p1, 'sp', gC[:], qkgC[l], 'gC')
                        bcast_row(p1, 'sp', gD[:], qkgD[l], 'gD')
                        def load_mod1(r):
                            bcast_row(p1, 'sp', sqx[:], modrow(r, 1), 'sqx')
                            bcast_row(p1, 'sp', A1[0][:], n1g[l], 'A1_0')
                            S.op('dve', lambda: V.scalar_tensor_tensor(out=A1[0][:], in0=sqx[:], scalar=1.0, in1=A1[0][:], op0=ALU.add, op1=ALU.mult),
                                 reads=['sqx', 'A1_0'], writes=['A1_0'])
                            bcast_row(p1, 'sp', sh1[0][:], modrow(r, 0), 'sh1_0')

                        load_mod1(b)

                        def front(ti):
                            isctx = ti >= NT
                            mi = 0
                            sl = ti % 2
                            xb_ = xt[0]
                            xk = 'xt0'
                            if ti == NT:
                                load_mod1(2)
                                yield
                            src = csrc(b)[(ti - NT) * 128:(ti - NT + 1) * 128, :] if isctx else xsrc(b)[ti * 128:(ti + 1) * 128, :]
                            S.dma('sp', xb_[:], src, reads=['xres'], writes=[xk])
                            yield
                            S.op('act', lambda: A_.activation(out=sqx[:], in_=xb_[:], func=AF.Square), reads=[xk], writes=['sqx'])
                            yield
                            S.op('dve', lambda: V.tensor_reduce(out=ssx[:, 0:1], in_=sqx[:], axis=AX.X, op=ALU.add), reads=['sqx'], writes=['ssx'])
                            yield
                            yield from rstd_g('ssx', ssx[:, 0:1], 1.0 / D)
                            S.op('dve', lambda: V.scalar_tensor_tensor(out=sqx[:], in0=xb_[:], scalar=ssx[:, 0:1], in1=A1[mi][:], op0=ALU.mult, op1=ALU.mult),
                                 reads=[xk, 'ssx', f'A1_{mi}'], writes=['sqx'])
                            yield
                            S.op('dve', lambda: V.tensor_tensor(out=hxb[sl][:], in0=sqx[:], in1=sh1[mi][:], op=ALU.add),
                                 reads=['sqx', f'sh1_{mi}'], writes=[f'hxb{sl}'])
                            yield
                            for c in range(8):
                                S.op('pe', lambda c=c: P_.transpose(out=psT[:, c, :], in_=hxb[sl][:, c * 128:(c + 1) * 128], identity=identb[:]),
                                     reads=[f'hxb{sl}', 'identb'], writes=['psT'], sig=(c == 7))
                            yield
                            S.op('act', lambda: A_.copy(out=hxT[sl][:], in_=psT[:]), reads=['psT'], writes=[f'hxT{sl}'])
                            yield

                        def mid(ti):
                            isctx = ti >= NT
                            sl = ti % 2
                            hT = hxT[sl]
                            hk = f'hxT{sl}'
                            needA = not (isctx and last)
                            blocks = []
                            if needA:
                                blocks.append((0, 512, 'A'))
                            blocks += [(768, 1024, 'Cq'), (1024, 1536, 'CkCv'), (1536, 2048, 'Dqk'), (2048, 2304, 'Dv')]
                            for bi, (n0, n1, kind) in enumerate(blocks):
                                pp = psP[bi % 2]
                                pk = f'psP{bi % 2}'
                                for c in range(8):
                                    S.op('pe', lambda c=c, n0=n0, n1=n1, pp=pp: P_.matmul(pp[:, 0:n1 - n0], lhsT=hT[:, c, :], rhs=winb[:, c, n0:n1], start=(c == 0), stop=(c == 7)),
                                         reads=[hk, 'winb'], writes=[pk], sig=(c == 7))
                                yield
                                if kind == 'A':
                                    S.op('act', lambda pp=pp: A_.activation(out=zA[sl][:], in_=pp[:, :], func=AF.Gelu), reads=[pk], writes=[f'zA{sl}'])
                                elif kind == 'Cq':
                                    S.op('dve', lambda pp=pp: V.tensor_copy(out=qkC[sl][:, 0:256], in_=pp[:, 0:256]), reads=[pk], writes=[f'qkC{sl}'])
                                elif kind == 'CkCv':
                                    S.op('dve', lambda pp=pp: V.tensor_copy(out=qkC[sl][:, 256:512], in_=pp[:, 0:256]), reads=[pk], writes=[f'qkC{sl}'])
                                    yield
                                    S.op('dve', lambda pp=pp: V.tensor_copy(out=vC[:, ti, :, 0:64], in_=pp[:, 256:512].rearrange("p (h e) -> p h e", e=64)), reads=[pk], writes=['vC'])
                                elif kind == 'Dqk':
                                    S.op('act', lambda pp=pp: A_.copy(out=qkD[sl][:, :], in_=pp[:, :]), reads=[pk], writes=[f'qkD{sl}'])
                                else:
                                    S.op('dve', lambda pp=pp: V.tensor_copy(out=vD[:, ti, :, 0:64], in_=pp[:, 0:256].rearrange("p (h e) -> p h e", e=64)), reads=[pk], writes=['vD'])
                                yield
                            for tch in range(2):
                                for c in range(8):
                                    S.op('pe', lambda c=c, tch=tch: P_.matmul(psX[:, tch * 128:(tch + 1) * 128], lhsT=winb[:, c, 512 + tch * 128:512 + (tch + 1) * 128],
                                                                           rhs=hT[:, c, :], start=(c == 0), stop=(c == 7)),
                                         reads=[hk, 'winb'], writes=['psX'], sig=(c == 7 and tch == 1))
                            yield
                            S.op('act', lambda: A_.copy(out=ZTb[:].rearrange("p t n -> p (t n)"), in_=psX[:, 0:256]), reads=['psX'], writes=['ZTb'])
                            yield
                            for cs in range(2):
                                for tch in range(2):
                                    S.op('pe', lambda cs=cs, tch=tch: P_.matmul(psX[:, cs * 256:(cs + 1) * 256], lhsT=ZTb[:, tch, :], rhs=dHb[:, cs, tch, :], start=(tch == 0), stop=(tch == 1)),
                                         reads=['ZTb', 'dHb'], writes=['psX'], sig=(tch == 1 and cs == 1))
                            yield
                            S.op('dve', lambda: V.tensor_copy(out=Ub[:, ti, :], in_=psX[:, :]), reads=['psX'], writes=['Ub'])
                            yield

                        def backA(ti):
                            isctx = ti >= NT
                            if isctx and last:
                                return
                            sl = ti % 2
                            z = zA[sl]
                            zk = f'zA{sl}'
                            yield from gnorm_g(z[:, 256:512], [zk], 4, 64, hn_sq2[:, :], 'hnsqb', hn_ss2[:, :], 'hnssb', vnb[:, :], 'vnb')
                            for h in range(4):
                                S.op('pe', lambda h=h: P_.matmul(psG1[:, h * 64:(h + 1) * 64], lhsT=sguTb[:, h, :], rhs=vnb[:, h * 64:(h + 1) * 64], start=True, stop=True),
                                     reads=['vnb', 'sguTb'], writes=['psG1'], sig=(h == 3))
                            yield
                            for h in range(4):
                                S.op('dve', lambda h=h: V.scalar_tensor_tensor(out=yA[:, h * 64:(h + 1) * 64], in0=psG1[:, h * 64:(h + 1) * 64], scalar=bsT[:, h:h + 1],
                                                                             in1=z[:, h * 64:(h + 1) * 64], op0=ALU.add, op1=ALU.mult),
                                     reads=['psG1', 'bsT', zk], writes=['yA'])
                                yield
                            yield from head_norm_g(yA[:, :], ['yA'], 0, ti * 128, (hn_sq, hn_ss, hn_yb), 'hn', q='sp')

                        def backC(ti):
                            isctx = ti >= NT
                            sl = ti % 2
                            src = qkC[sl]
                            sk = f'qkC{sl}'
                            if isctx:
                                yield from gnorm_g(src[:, :], [sk], 16, 32, qk2[:, :], 'qk2', ssC[:, :], 'ssC', qkbC[:, :], 'qkbC', gain_ap=gC[:, :], gain_key='gC')
                            else:
                                rpt = rp[sl]
                                rk = f'rp{sl}'
                                S.dma('sp', rpt[:], ropeT[ti * 128:(ti + 1) * 128, :, :], writes=[rk])
                                yield
                                yield from gnorm_g(src[:, :], [sk], 16, 32, qk2[:, :], 'qk2', ssC[:, :], 'ssC', qk3[:, :], 'qk3', gain_ap=gC[:, :], gain_key='gC')
                                S.op('pool', lambda: G_.tensor_tensor(out=qk2[:, :], in0=qk3[:, :], in1=rpt[:, 0, :], op=ALU.mult), reads=['qk3', rk], writes=['qk2'])
                                yield
                                q4 = qk3[:, :].rearrange("p (g t e) -> p g t e", t=2, e=8)
                                o4 = qk4[:, :].rearrange("p (g t e) -> p g t e", t=2, e=8)
                                s4 = rpt[:, 1, :].rearrange("p (g t e) -> p g t e", t=2, e=8)
                                S.op('pool', lambda: G_.tensor_tensor(out=o4[:, :, 0, :], in0=q4[:, :, 1, :], in1=s4[:, :, 0, :], op=ALU.mult), reads=['qk3', rk], writes=['qk4'])
                                yield
                                S.op('pool', lambda: G_.tensor_tensor(out=o4[:, :, 1, :], in0=q4[:, :, 0, :], in1=s4[:, :, 1, :], op=ALU.mult), reads=['qk3', rk], writes=['qk4'])
                                yield
                                S.op('dve', lambda: V.tensor_tensor(out=qkbC[:, :], in0=qk2[:, :], in1=qk4[:, :], op=ALU.add), reads=['qk2', 'qk4'], writes=['qkbC'])
                                yield
                            for half, dstT, dk in ((0, qTC, 'qTC'), (1, kTC, 'kTC')):
                                for h in range(4):
                                    S.op('pe', lambda h=h, half=half: P_.transpose(out=psQc[0:64, h, :], in_=qkbC[:, half * 256 + h * 64: half * 256 + (h + 1) * 64], identity=identb[:]),
                                         reads=['qkbC', 'identb'], writes=['psQc'], sig=(h == 3))
                                yield
                                S.op('act', lambda dstT=dstT: A_.copy(out=dstT[:, :, ti * 128:(ti + 1) * 128], in_=psQc[0:64, 0:4, :]), reads=['psQc'], writes=[dk])
                                yield

                        def backD(ti):
                            sl = ti % 2
                            yield from gnorm_g(qkD[sl][:, :], [f'qkD{sl}'], 8, 64, sqD[:, :], 'sqD', ssD[:, :], 'ssD', qkbD[:, :], 'qkbD', gain_ap=gD[:, :], gain_key='gD')
                            for half, dstT, dk in ((0, qTD, 'qTD'), (1, kTD, 'kTD')):
                                for t2 in range(2):
                                    S.op('pe', lambda t2=t2, half=half: P_.transpose(out=psQd[:, t2, :], in_=qkbD[:, half * 256 + t2 * 128: half * 256 + (t2 + 1) * 128], identity=identb[:]),
                                         reads=['qkbD', 'identb'], writes=['psQd'], sig=(t2 == 1))
                                yield
                                S.op('act', lambda dstT=dstT: A_.copy(out=dstT[:, :, ti * 128:(ti + 1) * 128], in_=psQd[:, 0:2, :]), reads=['psQd'], writes=[dk])
                                yield

                        def fm(ti):
                            yield from front(ti)
                            yield from mid(ti)

                        for k in range(NTA + 2):
                            gens = []
                            if k < NTA:
                                gens.append(front(k))
                            if 0 <= k - 1 < NTA:
                                gens.append(mid(k - 1))
                            if 0 <= k - 2 < NTA:
                                gens += [backA(k - 2), backC(k - 2), backD(k - 2)]
                            run_interleaved(gens)
                        S.barrier()
                    if stop_after == 'p1':
                        S.finish()
                        return nc

                    qtiles = list(range(NT)) + ([] if last else [NT, NT + 1])

                    with ExitStack() as p3:
                        dl = T(p3, "dl", [128, 128], F32)
                        dl2 = T(p3, "dl2", [128, 64], F32)
                        ET = [T(p3, f"ET{i}", [128, 512], BF16) for i in range(4)]
                        yC = T(p3, "yC", [128, 4, 256], F32)
                        rr = T(p3, "rr", [128, 2, 4], F32)
                        hn_sq = T(p3, "hn_sq3", [128, 256], F32)
                        hn_ss = T(p3, "hn_ss3", [128, 4], F32)
                        hn_yb = T(p3, "hn_yb3", [128, 256], BF16)
                        psS = [PS(p3, f"psS{i}", [128, 512]) for i in range(4)]
                        psO = [PS(p3, f"psO{i}", [128, 4, 128]) for i in range(4)]
                        bcast_row(p3, 'sp', dl[:], dlam[l], 'dl')
                        d4 = dl[:, :].rearrange("p (a b e) -> p a b e", a=2, b=2)
                        S.op('dve', lambda: V.tensor_tensor(out=dl2[:, :].rearrange("p (a e) -> p a e", a=2), in0=d4[:, :, 0, :], in1=d4[:, :, 1, :], op=ALU.mult),
                             reads=['dl'], writes=['dl2'])
                        S.op('dve', lambda: V.tensor_reduce(out=lamc[:, 0:2], in_=dl2[:, :].rearrange("p (a e) -> p a e", a=2), axis=AX.X, op=ALU.add),
                             reads=['dl2'], writes=['lamc'])
                        S.op('act', lambda: A_.activation(out=lamc[:, 0:2], in_=lamc[:, 0:2], func=AF.Exp), reads=['lamc'], writes=['lamc'])
                        S.op('dve', lambda: V.tensor_tensor(out=lamc[:, 2:3], in0=lamc[:, 1:2], in1=lamc[:, 0:1], op=ALU.subtract), reads=['lamc'], writes=['lamc'])
                        S.op('dve', lambda: V.tensor_scalar(out=lamc[:, 3:4], in0=lamc[:, 2:3], scalar1=-lam_init, scalar2=None, op0=ALU.add), reads=['lamc'], writes=['lamc'])
                        qblocks = [(qb * 512, 512, list(range(NTA))) for qb in range(4)]
                        if not last:
                            qblocks.append((SEQ, 256, [NT, NT + 1]))
                        steps = []
                        gi = 0
                        for (q0, qn, kts) in qblocks:
                            for h in range(4):
                                for ki, kt in enumerate(kts):
                                    for m in range(2):
                                        steps.append(dict(q0=q0, qn=qn, kts=kts, h=h, m=m, ki=ki, kt=kt, g=gi,
                                                          glast=(m == 1 and ki == len(kts) - 1), blast=(h == 3 and m == 1 and ki == len(kts) - 1)))
                                gi += 1
                        NB3 = 4
                        NBE = 4
                        LOOK = 2

                        def emit_S(n):
                            sp_ = steps[n]
                            pss = psS[n % NB3]
                            et = ET[n % NBE]
                            h, m, kt, q0, qn = sp_['h'], sp_['m'], sp_['kt'], sp_['q0'], sp_['qn']
                            S.op('pe', lambda: P_.matmul(pss[:, 0:qn], lhsT=kTC[32 * m:32 * m + 32, h, kt * 128:(kt + 1) * 128],
                                                         rhs=qTC[32 * m:32 * m + 32, h, q0:q0 + qn], start=True, stop=True),
                                 reads=['kTC', 'qTC'], writes=[f'psS{n % NB3}'])
                            S.op('act', lambda: A_.activation(out=et[:, 0:qn], in_=pss[:, 0:qn], func=AF.Exp, scale=32.0 ** -0.5),
                                 reads=[f'psS{n % NB3}'], writes=[f'ET{n % NBE}'])

                        def emit_AV(n):
                            sp_ = steps[n]
                            et = ET[n % NBE]
                            h, m, kt, ki, kts, q0, qn, g = sp_['h'], sp_['m'], sp_['kt'], sp_['ki'], sp_['kts'], sp_['q0'], sp_['qn'], sp_['g']
                            nq = qn // 128
                            po = psO[2 * (g % 2) + m]
                            pk = f'psO{2 * (g % 2) + m}'
                            for qi in range(nq):
                                S.op('pe', lambda qi=qi: P_.matmul(po[:, qi, 0:65], lhsT=et[:, qi * 128:(qi + 1) * 128], rhs=vC[:, kt, h, :],
                                                                   start=(ki == 0 and qi == 0), stop=(ki == len(kts) - 1), skip_group_check=True),
                                     reads=[f'ET{n % NBE}', 'vC'], writes=[pk], sig=(qi == nq - 1))
                            if sp_['glast']:
                                p0, p1_ = psO[2 * (g % 2)], psO[2 * (g % 2) + 1]
                                k0, k1 = f'psO{2 * (g % 2)}', f'psO{2 * (g % 2) + 1}'
                                S.op('dve', lambda: V.reciprocal(out=rr[:, 0, 0:nq], in_=p0[:, 0:nq, 64]), reads=[k0], writes=['rr'])
                                S.op('dve', lambda: V.reciprocal(out=rr[:, 1, 0:nq], in_=p1_[:, 0:nq, 64]), reads=[k1], writes=['rr'])
                                S.op('dve', lambda: V.tensor_scalar(out=rr[:, 1, 0:nq], in0=rr[:, 1, 0:nq], scalar1=lamc[:, 3:4], scalar2=None, op0=ALU.mult),
                                     reads=['rr', 'lamc'], writes=['rr'])
                                for qi in range(nq):
                                    S.op('dve', lambda qi=qi: V.tensor_scalar(out=yC[:, qi, h * 64:(h + 1) * 64], in0=p0[:, qi, 0:64], scalar1=rr[:, 0, qi:qi + 1], scalar2=None, op0=ALU.mult),
                                         reads=[k0, 'rr'], writes=['yC'])
                                    S.op('dve', lambda qi=qi: V.scalar_tensor_tensor(out=yC[:, qi, h * 64:(h + 1) * 64], in0=p1_[:, qi, 0:64], scalar=rr[:, 1, qi:qi + 1],
                                                                                   in1=yC[:, qi, h * 64:(h + 1) * 64], op0=ALU.mult, op1=ALU.add),
                                         reads=[k1, 'rr', 'yC'], writes=['yC'])
                            if sp_['blast']:
                                for qi in range(nq):
                                    head_norm(yC[:, qi, :], ['yC'], 2, q0 + qi * 128, (hn_sq, hn_ss, hn_yb), 'hn')

                        for n in range(0, len(steps) + LOOK, 2):
                            for d_ in range(2):
                                if n + d_ < len(steps):
                                    emit_S(n + d_)
                            for d_ in range(2):
                                if 0 <= n + d_ - LOOK < len(steps):
                                    emit_AV(n + d_ - LOOK)
                        S.barrier()
                    if stop_after == 'p3':
                        S.finish()
                        return nc

                    with ExitStack() as p4:
                        nbf = [T(p4, f"nbf{i}", [128, NVAR, 128], F32) for i in range(2)]
                        nbb = T(p4, "nbb", [128, 4, NVAR, 128], BF16)
                        ETd = [T(p4, f"ETd{i}", [128, 7, 128], BF16) for i in range(4)]
                        yD = T(p4, "yD", [128, 256], F32)
                        rr = T(p4, "rr4", [128, 4], F32)
                        hn_sq = T(p4, "hn_sq4", [128, 256], F32)
                        hn_ss = T(p4, "hn_ss4", [128, 4], F32)
                        hn_yb = T(p4, "hn_yb4", [128, 256], BF16)
                        psS = [PS(p4, f"psS4{i}", [128, 8, 128]) for i in range(2)]
                        psO = [PS(p4, f"psO4{i}", [128, 4, 128]) for i in range(2)]
                        for h in range(4):
                            nb_ = nbf[h % 2]
                            S.dma('sp' if h % 2 == 0 else 'pool', nb_[:], nbias[l, h].rearrange("v k q -> k v q"), writes=[f'nbf{h % 2}'])
                            S.op('dve' if h % 2 == 0 else 'act', (lambda h=h, nb_=nb_: V.tensor_scalar(out=nbb[:, h, :, :], in0=nb_[:], scalar1=8.0, scalar2=None, op0=ALU.mult)) if h % 2 == 0
                                 else (lambda h=h, nb_=nb_: A_.mul(out=nbb[:, h, :, :], in_=nb_[:], mul=8.0)),
                                 reads=[f'nbf{h % 2}'], writes=['nbb'])
                        units = []
                        for t in qtiles:
                            if t < NT:
                                kl = [(kt, v) for (kt, v) in NA_PER_T[t]] + [(NT, None), (NT + 1, None)]
                            else:
                                kl = [(NT, None), (NT + 1, None)]
                            for h in range(4):
                                units.append((t, h, kl))

                        def s4_mms(n):
                            t, h, kl = units[n]
                            pss = psS[n % 2]
                            pb = 64 * (h % 2)
                            mms = []
                            for idx, (kt, v) in enumerate(kl):
                                mms.append(lambda idx=idx, kt=kt, v=v: P_.matmul(pss[:, idx, :], lhsT=kTD[pb:pb + 64, h // 2, kt * 128:(kt + 1) * 128],
                                                                                 rhs=qTD[pb:pb + 64, h // 2, t * 128:(t + 1) * 128], start=True, stop=(v is None)))
                                if v is not None:
                                    mms.append(lambda idx=idx, v=v: P_.matmul(pss[:, idx, :], lhsT=identb[:], rhs=nbb[:, h, v, :], start=False, stop=True))
                            return mms

                        def emit_S4pair(p):
                            ns = [2 * p, 2 * p + 1]
                            lists = [s4_mms(n) for n in ns]
                            L_ = len(lists[0])
                            for i_ in range(L_):
                                for n, mm in zip(ns, lists):
                                    S.op('pe', mm[i_], reads=['kTD', 'qTD', 'identb', 'nbb'], writes=[f'psS4{n % 2}'], sig=(i_ == L_ - 1))
                            for n in ns:
                                t, h, kl = units[n]
                                nk = len(kl)
                                pss = psS[n % 2]
                                et = ETd[n % 4]
                                S.op('act', lambda pss=pss, et=et, nk=nk: A_.activation(out=et[:, 0:nk, :], in_=pss[:, 0:nk, :], func=AF.Exp, scale=0.125),
                                     reads=[f'psS4{n % 2}'], writes=[f'ETd{n % 4}'])

                        def emit_AV4(n):
                            t, h, kl = units[n]
                            et = ETd[n % 4]
                            nk = len(kl)
                            tp = (n // 4) % 2
                            po = psO[tp]
                            pk = f'psO4{tp}'
                            for idx, (kt, v) in enumerate(kl):
                                S.op('pe', lambda idx=idx, kt=kt: P_.matmul(po[:, h, 0:65], lhsT=et[:, idx, :], rhs=vD[:, kt, h, :], start=(idx == 0), stop=(idx == nk - 1)),
                                     reads=[f'ETd{n % 4}', 'vD'], writes=[pk], sig=(idx == nk - 1))
                            if h == 3:
                                S.op('dve', lambda: V.reciprocal(out=rr[:, 0:4], in_=po[:, 0:4, 64]), reads=[pk], writes=['rr4'])
                                for hh in range(4):
                                    S.op('dve', lambda hh=hh: V.tensor_scalar(out=yD[:, hh * 64:(hh + 1) * 64], in0=po[:, hh, 0:64], scalar1=rr[:, hh:hh + 1], scalar2=None, op0=ALU.mult),
                                         reads=[pk, 'rr4'], writes=['yD'])
                                head_norm(yD[:, :], ['yD'], 3, t * 128, (hn_sq, hn_ss, hn_yb), 'hn')

                        npairs = len(units) // 2
                        for p in range(npairs + 1):
                            if p < npairs:
                                emit_S4pair(p)
                            if p - 1 >= 0:
                                emit_AV4(2 * (p - 1))
                                emit_AV4(2 * (p - 1) + 1)
                        S.barrier()
                    if stop_after == 'p4':
                        S.finish()
                        return nc

                with ExitStack() as p5:
                    woutb = T(p5, "woutb", [128, 8, D], BF16)
                    g1v = [T(p5, f"g1v{i}", [128, D], F32) for i in range(2)]
                    A2 = [T(p5, f"A2_{i}", [128, D], F32) for i in range(2)]
                    sh2 = [T(p5, f"sh2_{i}", [128, D], F32) for i in range(2)]
                    tmpv = T(p5, "tmpv5", [128, D], F32)
                    rwf = T(p5, "rwf", [128, 8, 48], F32)
                    ynb = [T(p5, f"ynb{i}", [128, D], BF16) for i in range(2)]
                    ynT = T(p5, "ynT", [128, 8, 128], BF16)
                    xt = [T(p5, f"xt5_{i}", [128, D], F32) for i in range(2)]
                    xn = [T(p5, f"xn{i}", [128, D], F32) for i in range(2)]
                    sq5 = T(p5, "sq5", [128, D], F32)
                    ss5 = T(p5, "ss5", [128, 1], F32)
                    h2f = T(p5, "h2f", [128, D], F32)
                    h2b = T(p5, "h2b", [128, D], BF16)
                    h2T = T(p5, "h2T", [128, 8, 128], F32)
                    psT = PS(p5, "psT5", [128, 8, 128], BF16)
                    psM = PS(p5, "psM", [128, 2, 512])
                    psH = PS(p5, "psH", [128, 8, 128])
                    psR = PS(p5, "psR", [128, 512])
                    S.dmaf('pool', lambda: G_.dma_start(out=woutb[:], in_=w_out[l].rearrange("(c p) n -> p c n", p=128)), writes=['woutb'])
                    S.op('dve', lambda: V.memset(rwf[:], 0.0), writes=['rwf'])
                    S.dma('sp', rwf[:, :, 32 * b:32 * b + 16], rw[l].rearrange("(c p) e -> p c e", p=128), reads=['rwf'], writes=['rwf'])
                    for i, r in enumerate((b, 2)):
                        if i == 1 and last:
                            continue
                        bcast_row(p5, 'sp', g1v[i][:], modrow(r, 2), f'g1v{i}')
                        bcast_row(p5, 'sp', tmpv[:], modrow(r, 4), 'tmpv5')
                        bcast_row(p5, 'sp', A2[i][:], n2g[l], f'A2_{i}')
                        S.op('dve', lambda i=i: V.scalar_tensor_tensor(out=A2[i][:], in0=tmpv[:], scalar=1.0, in1=A2[i][:], op0=ALU.add, op1=ALU.mult),
                             reads=['tmpv5', f'A2_{i}'], writes=[f'A2_{i}'])
                        bcast_row(p5, 'sp', sh2[i][:], modrow(r, 3), f'sh2_{i}')
                    qtiles = list(range(NT)) + ([] if last else [NT, NT + 1])
                    M = 32 * b + 16
                    dt = [T(p5, f"dt{i}", [128, 2, NT, 128], BF16) for i in range(2)]
                    f_sq = T(p5, "f_sq", [128, 256], F32)
                    f_ss = T(p5, "f_ss", [128, 4], F32)
                    f_yb = T(p5, "f_yb", [128, 256], BF16)
                    psY = PS(p5, "psY", [128, 512])

                    def fourier_load(j):
                        isctx = j >= NT
                        tb = dt[j % 2]
                        tk = f'dt{j % 2}'
                        if not isctx:
                            S.dma('pool', tb[:], dftN[j], writes=[tk])
                        else:
                            jj = j - NT
                            for cs in range(2):
                                S.dma('pool', tb[:, cs, 0:2, :], dftC[cs][:, jj * 128:(jj + 1) * 128].rearrange("(i p) n -> p i n", p=128), writes=[tk])

                    def fourier_group(j):
                        isctx = j >= NT
                        tb = dt[j % 2]
                        tk = f'dt{j % 2}'
                        ins = [(0, NT), (1, NT + 1)] if isctx else [(i, i) for i in range(NT)]
                        n_mm = 2 * len(ins)
                        k = 0
                        for cs in range(2):
                            for (ii, ti) in ins:
                                S.op('pe', lambda cs=cs, ii=ii, ti=ti, k=k: P_.matmul(psY[:, 0:256], lhsT=tb[:, cs, ii, :], rhs=Ub[:, ti, cs * 256:(cs + 1) * 256],
                                                                                   start=(k == 0), stop=(k == n_mm - 1)),
                                     reads=[tk, 'Ub'], writes=['psY'], sig=(k == n_mm - 1))
                                k += 1
                        head_norm(psY[:, 0:256], ['psY'], 1, j * 128, (f_sq, f_ss, f_yb), 'fh', q='pool')


                    def p5A(ti):
                        isctx = ti >= NT
                        mi = 1 if isctx else 0
                        yb_ = ynb[ti % 2]
                        yk = f'ynb{ti % 2}'
                        xb_ = xt[ti % 2]
                        xk = f'xt5_{ti % 2}'
                        xn_ = xn[ti % 2]
                        xnk = f'xn{ti % 2}'
                        S.dma('sp', yb_[:], ynd[ti * 128:(ti + 1) * 128, :], reads=[f'ynd{ti * 128}_{g}' for g in range(4)], writes=[yk])
                        yield
                        src = csrc(b)[(ti - NT) * 128:(ti - NT + 1) * 128, :] if isctx else xsrc(b)[ti * 128:(ti + 1) * 128, :]
                        dst = xc[b][(ti - NT) * 128:(ti - NT + 1) * 128, :] if isctx else out[b][ti * 128:(ti + 1) * 128, :]
                        S.dma('sp', xb_[:], src, reads=['xres'], writes=[xk])
                        yield
                        for c in range(8):
                            S.op('pe', lambda c=c: P_.transpose(out=psT[:, c, :], in_=yb_[:, c * 128:(c + 1) * 128], identity=identb[:]),
                                 reads=[yk, 'identb'], writes=['psT5'], sig=(c == 7))
                        yield
                        S.op('act', lambda: A_.copy(out=ynT[:], in_=psT[:]), reads=['psT5'], writes=['ynT'])
                        yield
                        for hf in range(2):
                            for c in range(8):
                                S.op('pe', lambda c=c, hf=hf: P_.matmul(psM[:, hf, :], lhsT=ynT[:, c, :], rhs=woutb[:, c, hf * 512:(hf + 1) * 512], start=(c == 0), stop=(c == 7)),
                                     reads=['ynT', 'woutb'], writes=['psM'], sig=(c == 7 and hf == 1))
                        yield
                        S.op('dve', lambda: V.tensor_tensor(out=xn_[:], in0=psM[:].rearrange("p a n -> p (a n)"), in1=g1v[mi][:], op=ALU.mult),
                             reads=['psM', f'g1v{mi}'], writes=[xnk])
                        yield
                        S.op('dve', lambda: V.tensor_tensor(out=xn_[:], in0=xn_[:], in1=xb_[:], op=ALU.add), reads=[xnk, xk], writes=[xnk])
                        yield
                        S.dma('sp', dst, xn_[:], reads=[xnk], writes=['xres_w'])
                        yield

                    def p5B(ti):
                        isctx = ti >= NT
                        mi = 1 if isctx else 0
                        xn_ = xn[ti % 2]
                        xnk = f'xn{ti % 2}'
                        S.op('act', lambda: A_.activation(out=sq5[:], in_=xn_[:], func=AF.Square), reads=[xnk], writes=['sq5'])
                        yield
                        S.op('dve', lambda: V.tensor_reduce(out=ss5[:, 0:1], in_=sq5[:], axis=AX.X, op=ALU.add), reads=['sq5'], writes=['ss5'])
                        yield
                        yield from rstd_g('ss5', ss5[:, 0:1], 1.0 / D)
                        S.op('dve', lambda: V.scalar_tensor_tensor(out=sq5[:], in0=xn_[:], scalar=ss5[:, 0:1], in1=A2[mi][:], op0=ALU.mult, op1=ALU.mult),
                             reads=[xnk, 'ss5', f'A2_{mi}'], writes=['sq5'])
                        yield
                        S.op('dve', lambda: V.tensor_tensor(out=h2f[:], in0=sq5[:], in1=sh2[mi][:], op=ALU.add), reads=['sq5', f'sh2_{mi}'], writes=['h2f'])
                        yield
                        S.op('act', lambda: A_.copy(out=h2b[:], in_=h2f[:]), reads=['h2f'], writes=['h2b'])
                        yield
                        hdst = h2c[b][(ti - NT) * 128:(ti - NT + 1) * 128, :] if isctx else h2l[b][ti * 128:(ti + 1) * 128, :]
                        S.dma('sp', hdst, h2b[:], reads=['h2b'], writes=['h2d'])
                        yield
                        for c in range(8):
                            S.op('pe', lambda c=c: P_.transpose(out=psH[:, c, :], in_=h2f[:, c * 128:(c + 1) * 128], identity=identf[:]),
                                 reads=['h2f', 'identf'], writes=['psH'], sig=(c == 7))
                        yield
                        S.op('dve', lambda: V.tensor_copy(out=h2T[:], in_=psH[:]), reads=['psH'], writes=['h2T'])
                        yield
                        for c in range(8):
                            S.op('pe', lambda c=c: P_.matmul(psR[0:M, 0:128], lhsT=rwf[:, c, 0:M], rhs=h2T[:, c, :], start=(c == 0), stop=(c == 7)),
                                 reads=['rwf', 'h2T'], writes=['psR'], sig=(c == 7))
                        yield
                        S.op('act', lambda: A_.activation(out=Eaff[32 * b:32 * b + 16, ti * 128:(ti + 1) * 128], in_=psR[32 * b:32 * b + 16, 0:128], func=AF.Exp),
                             reads=['psR'], writes=['Eaff'])
                        yield

                    FLEAD = 2
                    fourier_load(qtiles[0])
                    for j_ in range(min(FLEAD, len(qtiles))):
                        if j_ + 1 < len(qtiles):
                            fourier_load(qtiles[j_ + 1])
                        fourier_group(qtiles[j_])
                    for k in range(len(qtiles) + 1):
                        if k + FLEAD < len(qtiles):
                            if k + FLEAD + 1 < len(qtiles):
                                fourier_load(qtiles[k + FLEAD + 1])
                            fourier_group(qtiles[k + FLEAD])
                        gens = []
                        if k < len(qtiles):
                            gens.append(p5A(qtiles[k]))
                        if k - 1 >= 0:
                            gens.append(p5B(qtiles[k - 1]))
                        run_interleaved(gens)
                    S.barrier()
                sb.close()
                if stop_after == 'p5' and b == samples[-1]:
                    S.finish()
                    return nc

            with ExitStack() as p6:
                nslot = 512 + (0 if last else 64)
                wbuf = [[T(p6, f"w{n}{i}", [128, 8, D], BF16) for n in ('g', 'u', 'd')] for i in range(2)]
                g2v = [T(p6, f"g2v{i}", [128, D], F32) for i in range(3)]
                aff = T(p6, "aff", [48, SEQ + CTXL], F32)
                aff0 = T(p6, "aff0", [48, SEQ + CTXL], F32)
                rec = T(p6, "rec", [48, 512], F32)
                vals = T(p6, "vals", [48, CAP + CAPC], F32)
                idxu = T(p6, "idxu", [48, CAP + CAPC], U32)
                idxf = T(p6, "idxf", [48, CAP + CAPC], F32)
                gateT = T(p6, "gateT", [128, 3, 48], F32)
                idxI = T(p6, "idxI", [128, 3, 48], I32)
                idxS = T(p6, "idxS", [128, 3, 48], I32)
                xg = [[T(p6, f"xg{p_}_{i}", [128, D], BF16) for i in range(4)] for p_ in range(2)]
                xgc = [[T(p6, f"xgc{p_}_{i}", [32, D], BF16) for i in range(2)] for p_ in range(2)]
                xgT = T(p6, "xgT", [128, 8, 576], BF16)
                hidT = T(p6, "hidT", [128, 8, 576], BF16)
                sil = [T(p6, f"sil{i}", [128, 576], F32) for i in range(2)]
                osb = [T(p6, f"osb{i}", [128, D], F32) for i in range(2)]
                psA = PS(p6, "psA6", [128, 512])
                psG = [PS(p6, f"psG6{i}", [128, 512]) for i in range(2)]
                psU = [PS(p6, f"psU6{i}", [128, 512]) for i in range(2)]
                psT = PS(p6, "psT6", [128, 8, 128], BF16)
                psD = [PS(p6, f"psD6{i}", [128, 512]) for i in range(2)]
                for i, r in enumerate((0, 1, 2)):
                    bcast_row(p6, 'sp', g2v[i][:], modrow(r, 5), f'g2v{i}')
                segs = [(0, 512), (512, 512), (1024, 512), (1536, 512)] + ([] if last else [(SEQ, 256)])
                for (c0, cn) in segs:
                    S.op('pe', lambda c0=c0, cn=cn: P_.matmul(psA[0:48, 0:cn], lhsT=onesblk[:, :], rhs=Eaff[:, c0:c0 + cn], start=True, stop=True),
                         reads=['onesblk', 'Eaff'], writes=['psA6'])
                    S.op('dve', lambda cn=cn: V.reciprocal(out=rec[:, 0:cn], in_=psA[0:48, 0:cn]), reads=['psA6'], writes=['rec'])
                    S.op('dve', lambda c0=c0, cn=cn: V.tensor_tensor(out=aff[:, c0:c0 + cn], in0=Eaff[:, c0:c0 + cn], in1=rec[:, 0:cn], op=ALU.mult),
                         reads=['Eaff', 'rec'], writes=['aff'])
                    S.op('act', lambda c0=c0, cn=cn: A_.copy(out=aff0[:, c0:c0 + cn], in_=aff[:, c0:c0 + cn]), reads=['aff'], writes=['aff0'])
                tsegs = [(0, SEQ, CAP, 0)] + ([] if last else [(SEQ, CTXL, CAPC, CAP)])
                for (c0, cn, cap, o0) in tsegs:
                    for j in range(cap // 8):
                        vk = f'vals{o0}_{j}'
                        S.op('dve', lambda c0=c0, cn=cn, j=j, o0=o0: V.max(out=vals[:, o0 + 8 * j:o0 + 8 * j + 8], in_=aff[:, c0:c0 + cn]), reads=['aff'], writes=[vk])
                        S.op('dve', lambda c0=c0, cn=cn, j=j, o0=o0: V.match_replace(out=aff[:, c0:c0 + cn], in_to_replace=vals[:, o0 + 8 * j:o0 + 8 * j + 8], in_values=aff[:, c0:c0 + cn], imm_value=-1.0),
                             reads=['aff', vk], writes=['aff'])
                        S.op('dve', lambda c0=c0, cn=cn, j=j, o0=o0: V.max_index(out=idxu[:, o0 + 8 * j:o0 + 8 * j + 8], in_max=vals[:, o0 + 8 * j:o0 + 8 * j + 8], in_values=aff0[:, c0:c0 + cn]),
                             reads=['aff0', vk], writes=['idxu'])
                ncol_ = CAP + (0 if last else CAPC)
                S.op('dve', lambda: V.tensor_copy(out=idxf[:, 0:ncol_], in_=idxu[:, 0:ncol_]), reads=['idxu'], writes=['idxf', 'vals_all'])
                tl = [(0, 128, 0), (128, 128, 1)] + ([] if last else [(CAP, CAPC, 2)])
                for (o0, on, j) in tl:
                    for srcT, dstT, dk in ((vals, gateT, 'gateT'), (idxf, idxI, 'idxI')):
                        S.op('pe', lambda srcT=srcT, o0=o0, on=on: P_.transpose(out=psA[0:on, 0:48], in_=srcT[0:48, o0:o0 + on], identity=identf[0:48, 0:48]),
                             reads=['vals_all', 'idxf', 'identf'], writes=['psA6'])
                        S.op('dve', lambda dstT=dstT, on=on, j=j: V.tensor_copy(out=dstT[0:on, j, :], in_=psA[0:on, 0:48]), reads=['psA6'], writes=[dk])
                        if dk == 'idxI':
                            off = float(SEQ if j < 2 else CTXL)
                            S.op('dve', lambda on=on, j=j: V.tensor_copy(out=idxS[0:on, j, 0:32], in_=psA[0:on, 0:32]), reads=['psA6'], writes=['idxS'])
                            S.op('dve', lambda on=on, j=j, off=off: V.tensor_scalar(out=idxS[0:on, j, 32:48], in0=psA[0:on, 32:48], scalar1=off, scalar2=None, op0=ALU.add),
                                 reads=['psA6'], writes=['idxS'])

                def load_w(e):
                    bufs = wbuf[e % 2]
                    for n, (wt, bt) in enumerate(zip((wg, wu, wd), bufs)):
                        S.dmaf('pool', lambda wt=wt, bt=bt: G_.dma_start(out=bt[:], in_=wt[l, e].rearrange("(c p) f -> p c f", p=128)),
                               writes=[f'w{"gud"[n]}{e % 2}'])

                def gather(e):
                    par = e % 2
                    for bb in range(2):
                        for hf in range(2):
                            j = bb * 2 + hf
                            S.dmaf('pool', lambda j=j, bb=bb, hf=hf: G_.indirect_dma_start(out=xg[par][j][:, :], out_offset=None, in_=h2l[bb][:, :],
                                                                                         in_offset=bass.IndirectOffsetOnAxis(ap=idxI[:, hf, 32 * bb + e:32 * bb + e + 1], axis=0)),
                                   reads=['idxI', 'h2d'], writes=[f'xg{par}_{j}'])
                        if not last:
                            S.dmaf('pool', lambda bb=bb: G_.indirect_dma_start(out=xgc[par][bb][:, :], out_offset=None, in_=h2c[bb][:, :],
                                                                             in_offset=bass.IndirectOffsetOnAxis(ap=idxI[0:32, 2, 32 * bb + e:32 * bb + e + 1], axis=0)),
                                   reads=['idxI', 'h2d'], writes=[f'xgc{par}_{bb}'])

                load_w(0)
                gather(0)
                for e in range(NE):
                    par = e % 2
                    if e + 1 < NE:
                        load_w(e + 1)
                        gather(e + 1)
                    wgb, wub, wdb = wbuf[e % 2]
                    wk = [f'w{n}{e % 2}' for n in 'gud']
                    for j in range(4):
                        for c in range(8):
                            S.op('pe', lambda j=j, c=c: P_.transpose(out=psT[:, c, :], in_=xg[par][j][:, c * 128:(c + 1) * 128], identity=identb[:]),
                                 reads=[f'xg{par}_{j}', 'identb'], writes=['psT6'], sig=(c == 7))
                        S.op('act', lambda j=j: A_.copy(out=xgT[:, :, j * 128:(j + 1) * 128], in_=psT[:]), reads=['psT6'], writes=['xgT'])
                    if not last:
                        for bb in range(2):
                            for c in range(8):
                                S.op('pe', lambda bb=bb, c=c: P_.transpose(out=psT[:, c, 0:32], in_=xgc[par][bb][0:32, c * 128:(c + 1) * 128], identity=identb[0:32, 0:32]),
                                     reads=[f'xgc{par}_{bb}', 'identb'], writes=['psT6'], sig=(c == 7))
                            S.op('act', lambda bb=bb: A_.copy(out=xgT[:, :, 512 + 32 * bb:512 + 32 * bb + 32], in_=psT[:, :, 0:32]), reads=['psT6'], writes=['xgT'])
                    for f in range(8):
                        fp = f % 2
                        for (wb_, ps_, pk, wkk, coff) in ((wgb, psG[fp], f'psG6{fp}', wk[0], 0), (wub, psU[fp], f'psU6{fp}', wk[1], 64)):
                            for c in range(8):
                                S.op('pe', lambda wb_=wb_, ps_=ps_, c=c, f=f: P_.matmul(ps_[:, :], lhsT=wb_[:, c, f * 128:(f + 1) * 128], rhs=xgT[:, c, 0:512], start=(c == 0), stop=(c == 7)),
                                     reads=[wkk, 'xgT'], writes=[pk], sig=(c == 7))
                            if not last:
                                for c in range(8):
                                    S.op('pe', lambda wb_=wb_, c=c, f=f, coff=coff: P_.matmul(psA[:, coff:coff + 64], lhsT=wb_[:, c, f * 128:(f + 1) * 128], rhs=xgT[:, c, 512:576], start=(c == 0), stop=(c == 7)),
                                         reads=[wkk, 'xgT'], writes=['psA6'], sig=(c == 7))
                        S.op('act', lambda fp=fp: A_.activation(out=sil[fp][:, 0:512], in_=psG[fp][:, :], func=AF.Silu), reads=[f'psG6{fp}'], writes=[f'sil{fp}'])
                        S.op('dve', lambda f=f, fp=fp: V.tensor_tensor(out=hidT[:, f, 0:512], in0=psU[fp][:, :], in1=sil[fp][:, 0:512], op=ALU.mult), reads=[f'psU6{fp}', f'sil{fp}'], writes=['hidT'])
                        if not last:
                            S.op('act', lambda fp=fp: A_.activation(out=sil[fp][:, 512:576], in_=psA[:, 0:64], func=AF.Silu), reads=['psA6'], writes=[f'sil{fp}'])
                            S.op('dve', lambda f=f, fp=fp: V.tensor_tensor(out=hidT[:, f, 512:576], in0=psA[:, 64:128], in1=sil[fp][:, 512:576], op=ALU.mult), reads=['psA6', f'sil{fp}'], writes=['hidT'])
                    jobs = [(bb * 2 + hf, 128, bb, hf, False) for bb in range(2) for hf in range(2)]
                    if not last:
                        jobs += [(None, 32, 0, 2, True), (None, 32, 1, 2, True)]
                    for jn, (j, mrows, bb, hf, isc) in enumerate(jobs):
                        s0 = (512 + 32 * bb) if isc else j * 128
                        ob = osb[jn % 2]
                        ok_ = f'osb{jn % 2}'
                        gi = 2 if isc else bb
                        for hh in range(2):
                            for f in range(8):
                                S.op('pe', lambda s0=s0, mrows=mrows, hh=hh, f=f: P_.matmul(psD[hh][0:mrows, :], lhsT=hidT[:, f, s0:s0 + mrows], rhs=wdb[:, f, hh * 512:(hh + 1) * 512], start=(f == 0), stop=(f == 7)),
                                     reads=['hidT', wk[2]], writes=[f'psD6{hh}'], sig=(f == 7))
                            S.op('dve', lambda ob=ob, mrows=mrows, hf=hf, bb=bb, e=e, gi=gi, hh=hh: V.scalar_tensor_tensor(out=ob[0:mrows, hh * 512:(hh + 1) * 512], in0=psD[hh][0:mrows, :],
                                                                                                            scalar=gateT[0:mrows, hf, 32 * bb + e:32 * bb + e + 1], in1=g2v[gi][0:mrows, hh * 512:(hh + 1) * 512], op0=ALU.mult, op1=ALU.mult),
                                 reads=[f'psD6{hh}', 'gateT', f'g2v{gi}'], writes=[ok_])
                        tgt = xc.rearrange("b n d -> (b n) d") if isc else out.rearrange("b n d -> (b n) d")
                        S.dmaf('pool', lambda ob=ob, mrows=mrows, hf=hf, bb=bb, e=e, tgt=tgt: G_.indirect_dma_start(out=tgt, out_offset=bass.IndirectOffsetOnAxis(ap=idxS[0:mrows, hf, 32 * bb + e:32 * bb + e + 1], axis=0),
                                                                                                           in_=ob[0:mrows, :], in_offset=None, compute_op=ALU.add),
                               reads=[ok_, 'idxS'], writes=['xres_w'])
                S.barrier()
        S.finish()
    return nc


_CONSTS = None


def prep_shared(inp):
    global _CONSTS
    if _CONSTS is None:
        _CONSTS = _consts()
    f = lambda a: np.ascontiguousarray(np.asarray(a, dtype=np.float32))
    sh = dict(_CONSTS)
    sh["ada_w"] = f(inp["ada_w"]); sh["ada_b"] = f(inp["ada_b"])
    sh["n1g"] = f(inp["norm1_g"]); sh["n2g"] = f(inp["norm2_g"])
    sh["w_in"] = f(inp["w_in"]); sh["w_out"] = f(inp["w_out"]); sh["hog"] = f(inp["head_out_g"])
    sh["sguT"] = np.ascontiguousarray(f(inp["sgu_w"]).transpose(0, 1, 3, 2))
    sh["sgubT"] = np.ascontiguousarray(f(inp["sgu_b"]).transpose(0, 2, 1))
    sh["qkgC"] = np.ascontiguousarray(np.concatenate([np.tile(f(inp["diff_qn_g"]), (1, 8)), np.tile(f(inp["diff_kn_g"]), (1, 8))], axis=1))
    sh["qkgD"] = np.ascontiguousarray(np.concatenate([np.tile(f(inp["na_qn_g"]), (1, 4)), np.tile(f(inp["na_kn_g"]), (1, 4))], axis=1))
    sh["dlam"] = np.ascontiguousarray(f(inp["diff_lambda"]).reshape(DEPTH, 128))
    sh["nbias"] = _na_bias_host(f(inp["na_rpb"]))
    sh["rw"] = f(inp["router_w"])
    sh["wg"] = f(inp["exp_w_gate"]); sh["wu"] = f(inp["exp_w_up"]); sh["wd"] = f(inp["exp_w_down"])
    return sh


def prep_core(inp, shared, core):
    m = dict(shared)
    b0 = 2 * core
    m["x"] = np.ascontiguousarray(np.asarray(inp["x"][b0:b0 + 2], dtype=np.float32))
    m["ctx"] = np.ascontiguousarray(np.asarray(inp["ctx"][b0:b0 + 2], dtype=np.float32))
    cv = np.concatenate([np.asarray(inp["c"][b0:b0 + 2], dtype=np.float32), np.asarray(inp["c_ctx"], dtype=np.float32)[None]], axis=0)
    m["cT"] = np.ascontiguousarray(cv.reshape(3, 8, 128).transpose(2, 1, 0))
    return m


def kernel(**inputs):
    inp = {k: np.asarray(v) for k, v in inputs.items()}
    shared = prep_shared(inp)
    nc = build_program()
    in_maps = [prep_core(inp, shared, c) for c in range(8)]
    res = run_bass_kernel_spmd(nc, in_maps, core_ids=list(range(8)))
    outs = [np.asarray(res.results[c]["out"], dtype=np.float32) for c in range(8)]
    return np.concatenate(outs, axis=0)
```

```python
import math
import numpy as np
import ml_dtypes
from contextlib import ExitStack
import concourse.bass as bass
import concourse.mybir as mybir
from concourse.bass_utils import run_bass_kernel_spmd

F32 = mybir.dt.float32
BF16 = mybir.dt.bfloat16
I32 = mybir.dt.int32
U32 = mybir.dt.uint32
AF = mybir.ActivationFunctionType
ALU = mybir.AluOpType
AX = mybir.AxisListType

D = 1024
SEQ = 2048
CTXL = 256
NT = 16
NTC = 2
NTA = NT + NTC
DEPTH = 2
IN_W = 2304
NE = 16
CAP = 256
CAPC = 32
EPS = 1e-6
NEGBIG = -30000.0
LAST_NAMES = {}


class Sched:
    NDS = 24

    def __init__(self, nc, stack):
        self.nc = nc
        self.stack = stack
        self.E = dict(pe=nc.tensor, act=nc.scalar, dve=nc.vector, pool=nc.gpsimd, sp=nc.sync)
        self.esem = {}
        self.ecnt = {}
        self.etok = {e: None for e in self.E}
        self.nsem = 0
        for e in self.E:
            self._newsem(e)
        self.dsem = [stack.enter_context(nc.semaphore(f"dq{i}")) for i in range(self.NDS)]
        self.dcnt = [0] * self.NDS
        self.dnext = {'sp': 0, 'pool': 0, 'act': 0}
        self.drange = {'sp': (0, 14), 'pool': (14, 24), 'act': (0, 14)}
        self.last = {}
        self.waited = {e: {} for e in self.E}
        self.pending = {e: [] for e in self.E}
        self.nwaits = 0
        self.ninst = 0

    def _newsem(self, e):
        self.esem[e] = self.stack.enter_context(self.nc.semaphore(f"s_{e}_{self.nsem}"))
        self.nsem += 1
        self.ecnt[e] = 0

    def _wait(self, e, tok):
        if tok is None:
            return
        sem, val, sid = tok[0], tok[1], tok[2]
        w = self.waited[e]
        if w.get(sid, 0) >= val:
            return
        w[sid] = val
        self.E[e].wait_ge(sem, val)
        self.nwaits += 1

    def _deps(self, e, reads, writes, is_pe=False):
        for k in reads:
            st = self.last.get(k)
            if st is not None:
                self._wait(e, st['w'])
                if k.startswith('ps'):
                    for rt in st['r']:
                        if rt[3] != e:
                            self._wait(e, rt)
        for k in writes:
            st = self.last.get(k)
            if st is not None:
                wt = st['w']
                if not (is_pe and wt is not None and wt[3] == 'pe'):
                    self._wait(e, wt)
                for rt in st['r']:
                    if is_pe and rt[3] == 'pe':
                        continue
                    self._wait(e, rt)

    def _record(self, tok, reads, writes):
        for k in reads:
            st = self.last.setdefault(k, {'w': None, 'r': []})
            st['r'].append(tok)
            if len(st['r']) > 48:
                st['r'] = st['r'][-48:]
        for k in writes:
            self.last[k] = {'w': tok, 'r': []}

    def op(self, e, fn, reads=(), writes=(), sig=True):
        self._deps(e, reads, writes, is_pe=(e == 'pe'))
        inst = fn()
        self.ninst += 1
        if not sig:
            self.pending[e].append((tuple(reads), tuple(writes)))
            return inst
        if self.ecnt[e] >= 30000:
            self._newsem(e)
        self.ecnt[e] += 1
        sem = self.esem[e]
        inst.then_inc(sem, 1)
        tok = (sem, self.ecnt[e], id(sem), e)
        self.etok[e] = tok
        for (r, w) in self.pending[e]:
            self._record(tok, r, w)
        self.pending[e] = []
        self._record(tok, reads, writes)
        return inst

    def dmaf(self, q, fn, reads=(), writes=()):
        lo, hi = self.drange[q]
        i = lo + self.dnext[q]
        self.dnext[q] = (self.dnext[q] + 1) % (hi - lo)
        sem = self.dsem[i]
        if self.dcnt[i] > 0:
            self._wait(q, (sem, 16 * self.dcnt[i], id(sem), 'dma'))
        self._deps(q, reads, writes)
        inst = fn()
        self.ninst += 1
        self.dcnt[i] += 1
        inst.then_inc(sem, 16)
        tok = (sem, 16 * self.dcnt[i], id(sem), 'dma')
        self._record(tok, reads, writes)
        return tok

    def dma(self, q, out, in_, reads=(), writes=(), **kw):
        return self.dmaf(q, lambda: self.E[q].dma_start(out=out, in_=in_, **kw), reads, writes)

    def barrier(self):
        for e in self.E:
            assert not self.pending[e], f"pending non-signaled ops on {e}"
        toks = [t for t in self.etok.values() if t is not None]
        for i in range(self.NDS):
            if self.dcnt[i] > 0:
                toks.append((self.dsem[i], 16 * self.dcnt[i], id(self.dsem[i]), 'dma'))
        for e in self.E:
            for t in toks:
                if t[3] == e:
                    continue
                self._wait(e, t)
        self.last = {}

    def finish(self):
        self.barrier()


def _na_structure():
    rows = 32
    variants = {}
    var_list = []
    per_t = []
    for t in range(NT):
        lst = []
        for kt in range(NT):
            sig = []
            anyv = False
            for a in range(2):
                for b_ in range(2):
                    r = 2 * t + b_
                    kr = 2 * kt + a
                    rs = min(max(r - 4, 0), rows - 8)
                    if rs <= kr <= rs + 7:
                        sig.append(kr - r + 7)
                        anyv = True
                    else:
                        sig.append(-1)
            if not anyv:
                continue
            sig = tuple(sig)
            if sig not in variants:
                variants[sig] = len(var_list)
                var_list.append(sig)
            lst.append((kt, variants[sig]))
        per_t.append(lst)
    return per_t, var_list


NA_PER_T, NA_VARS = _na_structure()
NVAR = len(NA_VARS)


def _na_bias_host(rpb):
    L, H = rpb.shape[0], rpb.shape[1]
    qc = np.arange(64)
    kc = np.arange(64)
    wstart = np.clip(qc - 8, 0, 48)
    valid_c = (kc[:, None] >= wstart[None, :]) & (kc[:, None] < wstart[None, :] + 16)
    coff = np.clip(kc[:, None] - qc[None, :], -15, 15) + 15
    flat = np.concatenate([rpb.reshape(L, H, 15 * 31), np.full((L, H, 1), NEGBIG, np.float32)], axis=-1)
    idx = np.full((NVAR, 128, 128), 15 * 31, np.int64)
    for v, sig in enumerate(NA_VARS):
        n = 0
        for a in range(2):
            for b_ in range(2):
                dr = sig[n]
                n += 1
                if dr < 0:
                    continue
                blk = np.where(valid_c, dr * 31 + coff, 15 * 31)
                idx[v, a * 64:(a + 1) * 64, b_ * 64:(b_ + 1) * 64] = blk
    out = flat[:, :, idx.reshape(-1)].reshape(L, H, NVAR, 128, 128)
    return np.ascontiguousarray(out.astype(np.float32))


def _consts():
    c = {}
    half = 16
    inv = 1.0 / (10000.0 ** (np.arange(0, half, 2, dtype=np.float32) / half))
    t = np.arange(SEQ)
    row = (t // 64).astype(np.float32)[:, None] * inv
    col = (t % 64).astype(np.float32)[:, None] * inv
    cr, sr, cc, sc = np.cos(row), np.sin(row), np.cos(col), np.sin(col)
    cos32 = np.concatenate([cr, cr, cc, cc], axis=1)
    sin32 = np.concatenate([-sr, sr, -sc, sc], axis=1)
    rope = np.stack([np.tile(cos32, (1, 16)), np.tile(sin32, (1, 16))], axis=1)
    c["ropeT"] = np.ascontiguousarray(rope.astype(np.float32))

    def dft(n, scale):
        k = np.arange(n, dtype=np.float64)
        ang = 2.0 * np.pi * np.outer(k, k) / n
        return np.cos(ang) * scale, -np.sin(ang) * scale
    cN, sN = dft(SEQ, 1.0 / math.sqrt(SEQ * 64.0))
    tb = np.stack([cN, sN]).reshape(2, NT, 128, NT, 128)
    c["dftN"] = np.ascontiguousarray(tb.transpose(3, 2, 0, 1, 4)).astype(ml_dtypes.bfloat16)
    cC, sC = dft(CTXL, 1.0 / math.sqrt(CTXL * 64.0))
    c["dftC"] = np.stack([cC, sC]).astype(ml_dtypes.bfloat16)
    c64, s64 = dft(64, 1.0)
    s64 = -s64
    cb = np.zeros((256, 256)); sb = np.zeros((256, 256))
    for h in range(4):
        cb[h * 64:(h + 1) * 64, h * 64:(h + 1) * 64] = c64
        sb[h * 64:(h + 1) * 64, h * 64:(h + 1) * 64] = s64
    c["dftH"] = np.stack([cb, sb]).astype(ml_dtypes.bfloat16)
    c["identb"] = np.eye(128).astype(ml_dtypes.bfloat16)
    c["identf"] = np.eye(128, dtype=np.float32)
    ob = np.zeros((48, 48), np.float32)
    for g in range(3):
        ob[g * 16:(g + 1) * 16, g * 16:(g + 1) * 16] = 1.0
    c["onesblk"] = ob
    return c


def build_program(stop_after=None, n_layers=DEPTH, samples=(0, 1)):
    nc = bass.Bass("TRN2", target_bir_lowering=False)
    dt_in = lambda name, shape, dt=F32: nc.dram_tensor(name, list(shape), dt, kind="ExternalInput").ap()
    x_in = dt_in("x", [2, SEQ, D])
    ctx_in = dt_in("ctx", [2, CTXL, D])
    cT_in = dt_in("cT", [128, 8, 3])
    ada_w = dt_in("ada_w", [DEPTH, D, 6 * D])
    ada_b = dt_in("ada_b", [DEPTH, 6 * D])
    n1g = dt_in("n1g", [DEPTH, D])
    n2g = dt_in("n2g", [DEPTH, D])
    w_in = dt_in("w_in", [DEPTH, D, IN_W])
    w_out = dt_in("w_out", [DEPTH, D, D])
    hog = dt_in("hog", [DEPTH, D])
    sguT = dt_in("sguT", [DEPTH, 4, 128, 128])
    sgubT = dt_in("sgubT", [DEPTH, 128, 4])
    qkgC = dt_in("qkgC", [DEPTH, 512])
    qkgD = dt_in("qkgD", [DEPTH, 512])
    dlam = dt_in("dlam", [DEPTH, 128])
    nbias = dt_in("nbias", [DEPTH, 4, NVAR, 128, 128])
    rw = dt_in("rw", [DEPTH, D, NE])
    wg = dt_in("wg", [DEPTH, NE, D, D])
    wu = dt_in("wu", [DEPTH, NE, D, D])
    wd = dt_in("wd", [DEPTH, NE, D, D])
    ropeT = dt_in("ropeT", [SEQ, 2, 512])
    dftN = dt_in("dftN", [NT, 128, 2, NT, 128], BF16)
    dftC = dt_in("dftC", [2, CTXL, CTXL], BF16)
    dftH = dt_in("dftH", [2, 256, 256], BF16)
    identb_in = dt_in("identb", [128, 128], BF16)
    identf_in = dt_in("identf", [128, 128])
    onesblk_in = dt_in("onesblk", [48, 48])
    out = nc.dram_tensor("out", [2, SEQ, D], F32, kind="ExternalOutput").ap()
    xc = nc.dram_tensor("xc_s", [2, CTXL, D], F32).ap()
    modd = nc.dram_tensor("modd_s", [DEPTH, 3, 6 * D], F32).ap()
    ynd = nc.dram_tensor("yn_s", [NTA * 128, D], BF16).ap()
    h2l = [nc.dram_tensor(f"h2l_s{b}", [SEQ, D], BF16).ap() for b in range(2)]
    h2c = [nc.dram_tensor(f"h2c_s{b}", [CTXL, D], BF16).ap() for b in range(2)]

    with ExitStack() as top:
        S = Sched(nc, top)

        uid = [0]

        def T(st, name, shape, dt):
            uid[0] += 1
            LAST_NAMES[name] = f"{name}__{uid[0]}"
            return st.enter_context(nc.sbuf_tensor(LAST_NAMES[name], list(shape), dt))

        def PS(st, name, shape, dt=F32):
            uid[0] += 1
            return st.enter_context(nc.psum_tensor(f"{name}__{uid[0]}", list(shape), dt))

        V, A_, G_, P_ = nc.vector, nc.scalar, nc.gpsimd, nc.tensor

        identb = T(top, "identb_t", [128, 128], BF16)
        identf = T(top, "identf_t", [128, 128], F32)
        onesblk = T(top, "onesblk_t", [48, 48], F32)
        Eaff = T(top, "Eaff", [48, SEQ + CTXL], F32)
        S.dma('sp', identb[:], identb_in[:, :], writes=['identb'])
        S.dma('sp', identf[:], identf_in[:, :], writes=['identf'])
        S.dma('sp', onesblk[:], onesblk_in[:, :], writes=['onesblk'])
        S.op('dve', lambda: V.memset(Eaff[:], 1.0), writes=['Eaff'])

        def run(g):
            for _ in g:
                pass

        def run_interleaved(gens):
            gens = list(gens)
            while gens:
                for g in list(gens):
                    try:
                        next(g)
                    except StopIteration:
                        gens.remove(g)

        def rstd_g(st_key, ssq_ap, n_inv):
            S.op('dve', lambda: V.tensor_scalar(out=ssq_ap, in0=ssq_ap, scalar1=n_inv, scalar2=EPS, op0=ALU.mult, op1=ALU.add),
                 reads=[st_key], writes=[st_key])
            yield
            S.op('act', lambda: A_.activation(out=ssq_ap, in_=ssq_ap, func=AF.Sqrt), reads=[st_key], writes=[st_key])
            yield
            S.op('dve', lambda: V.reciprocal(out=ssq_ap, in_=ssq_ap), reads=[st_key], writes=[st_key])
            yield

        def rstd_from_ssq(st_key, ssq_ap, n_inv, tmp_ap):
            run(rstd_g(st_key, ssq_ap, n_inv))

        def gnorm_g(src_ap, src_keys, G, E, sq_ap, sq_key, ss_ap, ss_key, out_ap, out_key, gain_ap=None, gain_key=None, sq_eng='act', gain_eng='dve'):
            if sq_eng == 'act':
                S.op('act', lambda: A_.activation(out=sq_ap, in_=src_ap, func=AF.Square), reads=src_keys, writes=[sq_key])
            else:
                S.op('pool', lambda: G_.tensor_tensor(out=sq_ap, in0=src_ap, in1=src_ap, op=ALU.mult), reads=src_keys, writes=[sq_key])
            yield
            S.op('dve', lambda: V.tensor_reduce(out=ss_ap, in_=sq_ap.rearrange("p (g e) -> p g e", e=E), axis=AX.X, op=ALU.add),
                 reads=[sq_key], writes=[ss_key])
            yield
            yield from rstd_g(ss_key, ss_ap, 1.0 / E)
            bc = ss_ap.unsqueeze(2).to_broadcast([128, G, E])
            if gain_ap is None:
                S.op('dve', lambda: V.tensor_tensor(out=out_ap.rearrange("p (g e) -> p g e", e=E),
                                                    in0=src_ap.rearrange("p (g e) -> p g e", e=E), in1=bc, op=ALU.mult),
                     reads=list(src_keys) + [ss_key], writes=[out_key])
                yield
            else:
                S.op('dve', lambda: V.tensor_tensor(out=sq_ap.rearrange("p (g e) -> p g e", e=E),
                                                    in0=src_ap.rearrange("p (g e) -> p g e", e=E), in1=bc, op=ALU.mult),
                     reads=list(src_keys) + [ss_key], writes=[sq_key])
                yield
                if gain_eng == 'pool':
                    S.op('pool', lambda: G_.tensor_tensor(out=out_ap, in0=sq_ap, in1=gain_ap, op=ALU.mult),
                         reads=[sq_key, gain_key], writes=[out_key])
                else:
                    S.op('dve', lambda: V.tensor_tensor(out=out_ap, in0=sq_ap, in1=gain_ap, op=ALU.mult),
                         reads=[sq_key, gain_key], writes=[out_key])
                yield

        def gnorm(*a, **k):
            run(gnorm_g(*a, **k))

        def bcast_row(st, q, dst, src_row_ap, key):
            S.dma(q, dst, src_row_ap.partition_broadcast(128), writes=[key])

        for l in range(n_layers):
            last = (l == DEPTH - 1)
            lam_init = 0.8 - 0.6 * math.exp(-0.3 * l)
            xsrc = (lambda b: x_in[b]) if l == 0 else (lambda b: out[b])
            csrc = (lambda b: ctx_in[b]) if l == 0 else (lambda b: xc[b])

            with ExitStack() as st:
                cT = T(st, "cT_t", [128, 8, 3], F32)
                sT = T(st, "sT_t", [128, 8, 3], F32)
                awb = [T(st, f"awb{i}", [128, 8, 512], F32) for i in range(2)]
                abt = T(st, "abt", [3, 6 * D], F32)
                modt = T(st, "modt", [3, 6 * D], F32)
                psm = PS(st, "psm", [128, 512])
                S.dma('sp', cT[:], cT_in[:, :, :], writes=['cT'])
                S.dma('sp', abt[:], ada_b[l].partition_broadcast(3), writes=['abt'])
                S.op('act', lambda: A_.activation(out=sT[:], in_=cT[:], func=AF.Silu), reads=['cT'], writes=['sT'])
                for nb in range(12):
                    bw = awb[nb % 2]
                    S.dma('sp', bw[:], ada_w[l][:, nb * 512:(nb + 1) * 512].rearrange("(c p) n -> p c n", p=128),
                          writes=[f'awb{nb % 2}'])
                    for c in range(8):
                        S.op('pe', lambda c=c, bw=bw: P_.matmul(psm[0:3, :], lhsT=sT[:, c, :], rhs=bw[:, c, :], start=(c == 0), stop=(c == 7)),
                             reads=['sT', f'awb{nb % 2}'], writes=['psm'], sig=(c == 7))
                    S.op('dve', lambda nb=nb: V.tensor_tensor(out=modt[:, nb * 512:(nb + 1) * 512], in0=psm[0:3, :],
                                                             in1=abt[:, nb * 512:(nb + 1) * 512], op=ALU.add),
                         reads=['psm', 'abt'], writes=['modt'])
                S.dma('sp', modd[l], modt[:], reads=['modt'], writes=['modd'])
                S.barrier()
            if stop_after == 'ada' and l == n_layers - 1:
                S.finish()
                return nc

            def modrow(r, k):
                return modd[l][r, k * D:(k + 1) * D]

            for b in samples:
                sb = ExitStack()
                Ub = T(sb, "Ub", [128, NTA, 512], BF16)
                hgb = T(sb, "hgb", [128, D], F32)
                with ExitStack() as st:
                    qTC = T(st, "qTC", [64, 4, NTA * 128], BF16)
                    kTC = T(st, "kTC", [64, 4, NTA * 128], BF16)
                    vC = T(st, "vC", [128, NTA, 4, 65], BF16)
                    qTD = T(st, "qTD", [128, 2, NTA * 128], BF16)
                    kTD = T(st, "kTD", [128, 2, NTA * 128], BF16)
                    vD = T(st, "vD", [128, NTA, 4, 65], BF16)
                    lamc = T(st, "lamc", [128, 4], F32)
                    S.op('pool', lambda: G_.memset(vC[:], 1.0), writes=['vC'])
                    S.op('pool', lambda: G_.memset(vD[:], 1.0), writes=['vD'])
                    bcast_row(st, 'sp', hgb[:], hog[l], 'hgb')
                    S.op('dve', lambda: V.tensor_scalar(out=hgb[:, 512:768], in0=hgb[:, 512:768], scalar1=1.0 - lam_init, scalar2=None, op0=ALU.mult),
                         reads=['hgb'], writes=['hgb'])

                    def head_norm_g(y_ap, y_keys, g, rows0, stw, pfx, q='sp', sq_eng='act'):
                        sq, ss, ynb = stw
                        yield from gnorm_g(y_ap, y_keys, 4, 64, sq[:, 0:256], pfx + 'sq', ss[:, 0:4], pfx + 'ss', ynb[:, :], pfx + 'ynb',
                                           gain_ap=hgb[:, g * 256:(g + 1) * 256], gain_key='hgb', sq_eng=sq_eng)
                        S.dma(q, ynd[rows0:rows0 + 128, g * 256:(g + 1) * 256], ynb[:, :], reads=[pfx + 'ynb'], writes=[f'ynd{rows0}_{g}'])
                        yield

                    def head_norm(*a, **k):
                        run(head_norm_g(*a, **k))

                    with ExitStack() as p1:
                        winb = T(p1, "winb", [128, 8, IN_W], BF16)
                        A1 = [T(p1, "A1_0", [128, D], F32)]
                        sh1 = [T(p1, "sh1_0", [128, D], F32)]
                        gC = T(p1, "gC", [128, 512], F32)
                        gD = T(p1, "gD", [128, 512], F32)
                        sguTb = T(p1, "sguTb", [128, 4, 128], BF16)
                        bsT = T(p1, "bsT", [128, 4], F32)
                        dHb = T(p1, "dHb", [128, 2, 2, 256], BF16)
                        xt = [T(p1, "xt0", [128, D], F32)]
                        rp = [T(p1, f"rp{i}", [128, 2, 512], F32) for i in range(2)]
                        sqx = T(p1, "sqx", [128, D], F32)
                        ssx = T(p1, "ssx", [128, 1], F32)
                        hxb = [T(p1, f"hxb{i}", [128, D], BF16) for i in range(2)]
                        hxT = [T(p1, f"hxT{i}", [128, 8, 128], BF16) for i in range(2)]
                        zA = [T(p1, f"zA{i}", [128, 512], F32) for i in range(2)]
                        qkC = [T(p1, f"qkC{i}", [128, 512], F32) for i in range(2)]
                        qkD = [T(p1, f"qkD{i}", [128, 512], F32) for i in range(2)]
                        vnb = T(p1, "vnb", [128, 256], BF16)
                        yA = T(p1, "yA", [128, 256], F32)
                        hn_sq = T(p1, "hn_sq", [128, 256], F32)
                        hn_ss = T(p1, "hn_ss", [128, 4], F32)
                        hn_yb = T(p1, "hn_yb", [128, 256], BF16)
                        hn_sq2 = T(p1, "hn_sqb", [128, 256], F32)
                        hn_ss2 = T(p1, "hn_ssb", [128, 4], F32)
                        ZTb = T(p1, "ZTb", [128, 2, 128], BF16)
                        qk2 = T(p1, "qk2", [128, 512], F32)
                        qk3 = T(p1, "qk3", [128, 512], F32)
                        qk4 = T(p1, "qk4", [128, 512], F32)
                        ssC = T(p1, "ssC", [128, 16], F32)
                        qkbC = T(p1, "qkbC", [128, 512], BF16)
                        sqD = T(p1, "sqD", [128, 512], F32)
                        ssD = T(p1, "ssD", [128, 8], F32)
                        qkbD = T(p1, "qkbD", [128, 512], BF16)
                        psP = [PS(p1, f"psP{i}", [128, 512]) for i in range(2)]
                        psT = PS(p1, "psT", [128, 8, 128], BF16)
                        psX = PS(p1, "psX", [128, 512])
                        psG1 = PS(p1, "psG1", [128, 512])
                        psQc = PS(p1, "psQc", [128, 8, 128], BF16)
                        psQd = PS(p1, "psQd", [128, 8, 128], BF16)

                        S.dmaf('pool', lambda: G_.dma_start(out=winb[:], in_=w_in[l].rearrange("(c p) n -> p c n", p=128)), writes=['winb'])
                        S.dmaf('pool', lambda: G_.dma_start(out=sguTb[:], in_=sguT[l].rearrange("h q p -> q h p")), writes=['sguTb'])
                        S.dmaf('pool', lambda: G_.dma_start(out=dHb[:], in_=dftH.rearrange("s (t p) n -> p s t n", p=128)), writes=['dHb'])
                        S.dma('sp', bsT[:], sgubT[l], writes=['bsT'])
                        bcast_row(p1, 'sp', gC[:], qkgC[l], 'gC')
                        bcast_row(p1, 'sp', gD[:], qkgD[l], 'gD')
                        def load_mod1(r):
                            bcast_row(p1, 'sp', sqx[:], modrow(r, 1), 'sqx')
                            bcast_row(p1, 'sp', A1[0][:], n1g[l], 'A1_0')
                            S.op('dve', lambda: V.scalar_tensor_tensor(out=A1[0][:], in0=sqx[:], scalar=1.0, in1=A1[0][:], op0=ALU.add, op1=ALU.mult),
                                 reads=['sqx', 'A1_0'], writes=['A1_0'])
                            bcast_row(p1, 'sp', sh1[0][:], modrow(r, 0), 'sh1_0')

                        load_mod1(b)

                        def front(ti):
                            isctx = ti >= NT
                            mi = 0
                            sl = ti % 2
                            xb_ = xt[0]
                            xk = 'xt0'
                            if ti == NT:
                                load_mod1(2)
                                yield
                            src = csrc(b)[(ti - NT) * 128:(ti - NT + 1) * 128, :] if isctx else xsrc(b)[ti * 128:(ti + 1) * 128, :]
                            S.dma('sp', xb_[:], src, reads=['xres'], writes=[xk])
                            yield
                            S.op('act', lambda: A_.activation(out=sqx[:], in_=xb_[:], func=AF.Square), reads=[xk], writes=['sqx'])
                            yield
                            S.op('dve', lambda: V.tensor_reduce(out=ssx[:, 0:1], in_=sqx[:], axis=AX.X, op=ALU.add), reads=['sqx'], writes=['ssx'])
                            yield
                            yield from rstd_g('ssx', ssx[:, 0:1], 1.0 / D)
                            S.op('dve', lambda: V.scalar_tensor_tensor(out=sqx[:], in0=xb_[:], scalar=ssx[:, 0:1], in1=A1[mi][:], op0=ALU.mult, op1=ALU.mult),
                                 reads=[xk, 'ssx', f'A1_{mi}'], writes=['sqx'])
                            yield
                            S.op('dve', lambda: V.tensor_tensor(out=hxb[sl][:], in0=sqx[:], in1=sh1[mi][:], op=ALU.add),
                                 reads=['sqx', f'sh1_{mi}'], writes=[f'hxb{sl}'])
                            yield
                            for c in range(8):
                                S.op('pe', lambda c=c: P_.transpose(out=psT[:, c, :], in_=hxb[sl][:, c * 128:(c + 1) * 128], identity=identb[:]),
                                     reads=[f'hxb{sl}', 'identb'], writes=['psT'], sig=(c == 7))
                            yield
                            S.op('act', lambda: A_.copy(out=hxT[sl][:], in_=psT[:]), reads=['psT'], writes=[f'hxT{sl}'])
                            yield

                        def mid(ti):
                            isctx = ti >= NT
                            sl = ti % 2
                            hT = hxT[sl]
                            hk = f'hxT{sl}'
                            needA = not (isctx and last)
                            blocks = []
                            if needA:
                                blocks.append((0, 512, 'A'))
                            blocks += [(768, 1024, 'Cq'), (1024, 1536, 'CkCv'), (1536, 2048, 'Dqk'), (2048, 2304, 'Dv')]
                            for bi, (n0, n1, kind) in enumerate(blocks):
                                pp = psP[bi % 2]
                                pk = f'psP{bi % 2}'
                                for c in range(8):
                                    S.op('pe', lambda c=c, n0=n0, n1=n1, pp=pp: P_.matmul(pp[:, 0:n1 - n0], lhsT=hT[:, c, :], rhs=winb[:, c, n0:n1], start=(c == 0), stop=(c == 7)),
                                         reads=[hk, 'winb'], writes=[pk], sig=(c == 7))
                                yield
                                if kind == 'A':
                                    S.op('act', lambda pp=pp: A_.activation(out=zA[sl][:], in_=pp[:, :], func=AF.Gelu), reads=[pk], writes=[f'zA{sl}'])
                                elif kind == 'Cq':
                                    S.op('act', lambda pp=pp: A_.copy(out=qkC[sl][:, 0:256], in_=pp[:, 0:256]), reads=[pk], writes=[f'qkC{sl}'])
                                elif kind == 'CkCv':
                                    S.op('act', lambda pp=pp: A_.copy(out=qkC[sl][:, 256:512], in_=pp[:, 0:256]), reads=[pk], writes=[f'qkC{sl}'])
                                    yield
                                    S.op('act', lambda pp=pp: A_.copy(out=vC[:, ti, :, 0:64], in_=pp[:, 256:512].rearrange("p (h e) -> p h e", e=64)), reads=[pk], writes=['vC'])
                                elif kind == 'Dqk':
                                    S.op('act', lambda pp=pp: A_.copy(out=qkD[sl][:, :], in_=pp[:, :]), reads=[pk], writes=[f'qkD{sl}'])
                                else:
                                    S.op('dve', lambda pp=pp: V.tensor_copy(out=vD[:, ti, :, 0:64], in_=pp[:, 0:256].rearrange("p (h e) -> p h e", e=64)), reads=[pk], writes=['vD'])
                                yield
                            for tch in range(2):
                                for c in range(8):
                                    S.op('pe', lambda c=c, tch=tch: P_.matmul(psX[:, tch * 128:(tch + 1) * 128], lhsT=winb[:, c, 512 + tch * 128:512 + (tch + 1) * 128],
                                                                           rhs=hT[:, c, :], start=(c == 0), stop=(c == 7)),
                                         reads=[hk, 'winb'], writes=['psX'], sig=(c == 7 and tch == 1))
                            yield
                            S.op('act', lambda: A_.copy(out=ZTb[:].rearrange("p t n -> p (t n)"), in_=psX[:, 0:256]), reads=['psX'], writes=['ZTb'])
                            yield
                            for cs in range(2):
                                for tch in range(2):
                                    S.op('pe', lambda cs=cs, tch=tch: P_.matmul(psX[:, cs * 256:(cs + 1) * 256], lhsT=ZTb[:, tch, :], rhs=dHb[:, cs, tch, :], start=(tch == 0), stop=(tch == 1)),
                                         reads=['ZTb', 'dHb'], writes=['psX'], sig=(tch == 1 and cs == 1))
                            yield
                            S.op('act', lambda: A_.copy(out=Ub[:, ti, :], in_=psX[:, :]), reads=['psX'], writes=['Ub'])
                            yield

                        def backA(ti):
                            isctx = ti >= NT
                            if isctx and last:
                                return
                            sl = ti % 2
                            z = zA[sl]
                            zk = f'zA{sl}'
                            yield from gnorm_g(z[:, 256:512], [zk], 4, 64, hn_sq2[:, :], 'hnsqb', hn_ss2[:, :], 'hnssb', vnb[:, :], 'vnb')
                            for h in range(4):
                                S.op('pe', lambda h=h: P_.matmul(psG1[:, h * 64:(h + 1) * 64], lhsT=sguTb[:, h, :], rhs=vnb[:, h * 64:(h + 1) * 64], start=True, stop=True),
                                     reads=['vnb', 'sguTb'], writes=['psG1'], sig=(h == 3))
                            yield
                            for h in range(4):
                                S.op('dve', lambda h=h: V.scalar_tensor_tensor(out=yA[:, h * 64:(h + 1) * 64], in0=psG1[:, h * 64:(h + 1) * 64], scalar=bsT[:, h:h + 1],
                                                                             in1=z[:, h * 64:(h + 1) * 64], op0=ALU.add, op1=ALU.mult),
                                     reads=['psG1', 'bsT', zk], writes=['yA'])
                                yield
                            yield from head_norm_g(yA[:, :], ['yA'], 0, ti * 128, (hn_sq, hn_ss, hn_yb), 'hn', q='sp')

                        def backC(ti):
                            isctx = ti >= NT
                            sl = ti % 2
                            src = qkC[sl]
                            sk = f'qkC{sl}'
                            if isctx:
                                yield from gnorm_g(src[:, :], [sk], 16, 32, qk2[:, :], 'qk2', ssC[:, :], 'ssC', qkbC[:, :], 'qkbC', gain_ap=gC[:, :], gain_key='gC')
                            else:
                                rpt = rp[sl]
                                rk = f'rp{sl}'
                                S.dma('sp', rpt[:], ropeT[ti * 128:(ti + 1) * 128, :, :], writes=[rk])
                                yield
                                yield from gnorm_g(src[:, :], [sk], 16, 32, qk2[:, :], 'qk2', ssC[:, :], 'ssC', qk3[:, :], 'qk3', gain_ap=gC[:, :], gain_key='gC')
                                S.op('pool', lambda: G_.tensor_tensor(out=qk2[:, :], in0=qk3[:, :], in1=rpt[:, 0, :], op=ALU.mult), reads=['qk3', rk], writes=['qk2'])
                                yield
                                q4 = qk3[:, :].rearrange("p (g t e) -> p g t e", t=2, e=8)
                                o4 = qk4[:, :].rearrange("p (g t e) -> p g t e", t=2, e=8)
                                s4 = rpt[:, 1, :].rearrange("p (g t e) -> p g t e", t=2, e=8)
                                S.op('pool', lambda: G_.tensor_tensor(out=o4[:, :, 0, :], in0=q4[:, :, 1, :], in1=s4[:, :, 0, :], op=ALU.mult), reads=['qk3', rk], writes=['qk4'])
                                yield
                                S.op('pool', lambda: G_.tensor_tensor(out=o4[:, :, 1, :], in0=q4[:, :, 0, :], in1=s4[:, :, 1, :], op=ALU.mult), reads=['qk3', rk], writes=['qk4'])
                                yield
                                S.op('dve', lambda: V.tensor_tensor(out=qkbC[:, :], in0=qk2[:, :], in1=qk4[:, :], op=ALU.add), reads=['qk2', 'qk4'], writes=['qkbC'])
                                yield
                            for half, dstT, dk in ((0, qTC, 'qTC'), (1, kTC, 'kTC')):
                                for h in range(4):
                                    S.op('pe', lambda h=h, half=half: P_.transpose(out=psQc[0:64, h, :], in_=qkbC[:, half * 256 + h * 64: half * 256 + (h + 1) * 64], identity=identb[:]),
                                         reads=['qkbC', 'identb'], writes=['psQc'], sig=(h == 3))
                                yield
                                S.op('act', lambda dstT=dstT: A_.copy(out=dstT[:, :, ti * 128:(ti + 1) * 128], in_=psQc[0:64, 0:4, :]), reads=['psQc'], writes=[dk])
                                yield

                        def backD(ti):
                            sl = ti % 2
                            yield from gnorm_g(qkD[sl][:, :], [f'qkD{sl}'], 8, 64, sqD[:, :], 'sqD', ssD[:, :], 'ssD', qkbD[:, :], 'qkbD', gain_ap=gD[:, :], gain_key='gD')
                            for half, dstT, dk in ((0, qTD, 'qTD'), (1, kTD, 'kTD')):
                                for t2 in range(2):
                                    S.op('pe', lambda t2=t2, half=half: P_.transpose(out=psQd[:, t2, :], in_=qkbD[:, half * 256 + t2 * 128: half * 256 + (t2 + 1) * 128], identity=identb[:]),
                                         reads=['qkbD', 'identb'], writes=['psQd'], sig=(t2 == 1))
                                yield
                                S.op('act', lambda dstT=dstT: A_.copy(out=dstT[:, :, ti * 128:(ti + 1) * 128], in_=psQd[:, 0:2, :]), reads=['psQd'], writes=[dk])
                                yield

                        def fm(ti):
                            yield from front(ti)
                            yield from mid(ti)

                        for k in range(NTA + 2):
                            gens = []
                            if k < NTA:
                                gens.append(front(k))
                            if 0 <= k - 1 < NTA:
                                gens.append(mid(k - 1))
                            if 0 <= k - 2 < NTA:
                                gens += [backA(k - 2), backC(k - 2), backD(k - 2)]
                            run_interleaved(gens)
                        S.barrier()
                    if stop_after == 'p1':
                        S.finish()
                        return nc

                    qtiles = list(range(NT)) + ([] if last else [NT, NT + 1])

                    with ExitStack() as p3:
                        dl = T(p3, "dl", [128, 128], F32)
                        dl2 = T(p3, "dl2", [128, 64], F32)
                        ET = [T(p3, f"ET{i}", [128, 512], BF16) for i in range(4)]
                        yC = T(p3, "yC", [128, 4, 256], F32)
                        rr = T(p3, "rr", [128, 2, 4], F32)
                        hn_sq = T(p3, "hn_sq3", [128, 256], F32)
                        hn_ss = T(p3, "hn_ss3", [128, 4], F32)
                        hn_yb = T(p3, "hn_yb3", [128, 256], BF16)
                        psS = [PS(p3, f"psS{i}", [128, 512]) for i in range(4)]
                        psO = [PS(p3, f"psO{i}", [128, 4, 128]) for i in range(4)]
                        bcast_row(p3, 'sp', dl[:], dlam[l], 'dl')
                        d4 = dl[:, :].rearrange("p (a b e) -> p a b e", a=2, b=2)
                        S.op('dve', lambda: V.tensor_tensor(out=dl2[:, :].rearrange("p (a e) -> p a e", a=2), in0=d4[:, :, 0, :], in1=d4[:, :, 1, :], op=ALU.mult),
                             reads=['dl'], writes=['dl2'])
                        S.op('dve', lambda: V.tensor_reduce(out=lamc[:, 0:2], in_=dl2[:, :].rearrange("p (a e) -> p a e", a=2), axis=AX.X, op=ALU.add),
                             reads=['dl2'], writes=['lamc'])
                        S.op('act', lambda: A_.activation(out=lamc[:, 0:2], in_=lamc[:, 0:2], func=AF.Exp), reads=['lamc'], writes=['lamc'])
                        S.op('dve', lambda: V.tensor_tensor(out=lamc[:, 2:3], in0=lamc[:, 1:2], in1=lamc[:, 0:1], op=ALU.subtract), reads=['lamc'], writes=['lamc'])
                        S.op('dve', lambda: V.tensor_scalar(out=lamc[:, 3:4], in0=lamc[:, 2:3], scalar1=-lam_init, scalar2=None, op0=ALU.add), reads=['lamc'], writes=['lamc'])
                        qblocks = [(qb * 512, 512, list(range(NTA))) for qb in range(4)]
                        if not last:
                            qblocks.append((SEQ, 256, [NT, NT + 1]))
                        steps = []
                        gi = 0
                        for (q0, qn, kts) in qblocks:
                            for h in range(4):
                                for ki, kt in enumerate(kts):
                                    for m in range(2):
                                        steps.append(dict(q0=q0, qn=qn, kts=kts, h=h, m=m, ki=ki, kt=kt, g=gi,
                                                          glast=(m == 1 and ki == len(kts) - 1), blast=(h == 3 and m == 1 and ki == len(kts) - 1)))
                                gi += 1
                        NB3 = 4
                        NBE = 4
                        LOOK = 2

                        def emit_S(n):
                            sp_ = steps[n]
                            pss = psS[n % NB3]
                            et = ET[n % NBE]
                            h, m, kt, q0, qn = sp_['h'], sp_['m'], sp_['kt'], sp_['q0'], sp_['qn']
                            S.op('pe', lambda: P_.matmul(pss[:, 0:qn], lhsT=kTC[32 * m:32 * m + 32, h, kt * 128:(kt + 1) * 128],
                                                         rhs=qTC[32 * m:32 * m + 32, h, q0:q0 + qn], start=True, stop=True),
                                 reads=['kTC', 'qTC'], writes=[f'psS{n % NB3}'])
                            S.op('act', lambda: A_.activation(out=et[:, 0:qn], in_=pss[:, 0:qn], func=AF.Exp, scale=32.0 ** -0.5),
                                 reads=[f'psS{n % NB3}'], writes=[f'ET{n % NBE}'])

                        def emit_AV(n):
                            sp_ = steps[n]
                            et = ET[n % NBE]
                            h, m, kt, ki, kts, q0, qn, g = sp_['h'], sp_['m'], sp_['kt'], sp_['ki'], sp_['kts'], sp_['q0'], sp_['qn'], sp_['g']
                            nq = qn // 128
                            po = psO[2 * (g % 2) + m]
                            pk = f'psO{2 * (g % 2) + m}'
                            for qi in range(nq):
                                S.op('pe', lambda qi=qi: P_.matmul(po[:, qi, 0:65], lhsT=et[:, qi * 128:(qi + 1) * 128], rhs=vC[:, kt, h, :],
                                                                   start=(ki == 0 and qi == 0), stop=(ki == len(kts) - 1), skip_group_check=True),
                                     reads=[f'ET{n % NBE}', 'vC'], writes=[pk], sig=(qi == nq - 1))
                            if sp_['glast']:
                                p0, p1_ = psO[2 * (g % 2)], psO[2 * (g % 2) + 1]
                                k0, k1 = f'psO{2 * (g % 2)}', f'psO{2 * (g % 2) + 1}'
                                S.op('dve', lambda: V.reciprocal(out=rr[:, 0, 0:nq], in_=p0[:, 0:nq, 64]), reads=[k0], writes=['rr'])
                                S.op('dve', lambda: V.reciprocal(out=rr[:, 1, 0:nq], in_=p1_[:, 0:nq, 64]), reads=[k1], writes=['rr'])
                                S.op('dve', lambda: V.tensor_scalar(out=rr[:, 1, 0:nq], in0=rr[:, 1, 0:nq], scalar1=lamc[:, 3:4], scalar2=None, op0=ALU.mult),
                                     reads=['rr', 'lamc'], writes=['rr'])
                                for qi in range(nq):
                                    S.op('dve', lambda qi=qi: V.tensor_scalar(out=yC[:, qi, h * 64:(h + 1) * 64], in0=p0[:, qi, 0:64], scalar1=rr[:, 0, qi:qi + 1], scalar2=None, op0=ALU.mult),
                                         reads=[k0, 'rr'], writes=['yC'])
                                    S.op('dve', lambda qi=qi: V.scalar_tensor_tensor(out=yC[:, qi, h * 64:(h + 1) * 64], in0=p1_[:, qi, 0:64], scalar=rr[:, 1, qi:qi + 1],
                                                                                   in1=yC[:, qi, h * 64:(h + 1) * 64], op0=ALU.mult, op1=ALU.add),
                                         reads=[k1, 'rr', 'yC'], writes=['yC'])
                            if sp_['blast']:
                                for qi in range(nq):
                                    head_norm(yC[:, qi, :], ['yC'], 2, q0 + qi * 128, (hn_sq, hn_ss, hn_yb), 'hn')

                        for n in range(0, len(steps) + LOOK, 2):
                            for d_ in range(2):
                                if n + d_ < len(steps):
                                    emit_S(n + d_)
                            for d_ in range(2):
                                if 0 <= n + d_ - LOOK < len(steps):
                                    emit_AV(n + d_ - LOOK)
                        S.barrier()
                    if stop_after == 'p3':
                        S.finish()
                        return nc

                    with ExitStack() as p4:
                        nbf = [T(p4, f"nbf{i}", [128, NVAR, 128], F32) for i in range(2)]
                        nbb = T(p4, "nbb", [128, 4, NVAR, 128], BF16)
                        ETd = [T(p4, f"ETd{i}", [128, 7, 128], BF16) for i in range(4)]
                        yD = T(p4, "yD", [128, 256], F32)
                        rr = T(p4, "rr4", [128, 4], F32)
                        hn_sq = T(p4, "hn_sq4", [128, 256], F32)
                        hn_ss = T(p4, "hn_ss4", [128, 4], F32)
                        hn_yb = T(p4, "hn_yb4", [128, 256], BF16)
                        psS = [PS(p4, f"psS4{i}", [128, 8, 128]) for i in range(2)]
                        psO = [PS(p4, f"psO4{i}", [128, 4, 128]) for i in range(2)]
                        for h in range(4):
                            nb_ = nbf[h % 2]
                            S.dma('sp' if h % 2 == 0 else 'pool', nb_[:], nbias[l, h].rearrange("v k q -> k v q"), writes=[f'nbf{h % 2}'])
                            S.op('dve' if h % 2 == 0 else 'act', (lambda h=h, nb_=nb_: V.tensor_scalar(out=nbb[:, h, :, :], in0=nb_[:], scalar1=8.0, scalar2=None, op0=ALU.mult)) if h % 2 == 0
                                 else (lambda h=h, nb_=nb_: A_.mul(out=nbb[:, h, :, :], in_=nb_[:], mul=8.0)),
                                 reads=[f'nbf{h % 2}'], writes=['nbb'])
                        units = []
                        for t in qtiles:
                            if t < NT:
                                kl = [(kt, v) for (kt, v) in NA_PER_T[t]] + [(NT, None), (NT + 1, None)]
                            else:
                                kl = [(NT, None), (NT + 1, None)]
                            for h in range(4):
                                units.append((t, h, kl))

                        def s4_mms(n):
                            t, h, kl = units[n]
                            pss = psS[n % 2]
                            pb = 64 * (h % 2)
                            mms = []
                            for idx, (kt, v) in enumerate(kl):
                                mms.append(lambda idx=idx, kt=kt, v=v: P_.matmul(pss[:, idx, :], lhsT=kTD[pb:pb + 64, h // 2, kt * 128:(kt + 1) * 128],
                                                                                 rhs=qTD[pb:pb + 64, h // 2, t * 128:(t + 1) * 128], start=True, stop=(v is None)))
                                if v is not None:
                                    mms.append(lambda idx=idx, v=v: P_.matmul(pss[:, idx, :], lhsT=identb[:], rhs=nbb[:, h, v, :], start=False, stop=True))
                            return mms

                        def emit_S4pair(p):
                            ns = [2 * p, 2 * p + 1]
                            lists = [s4_mms(n) for n in ns]
                            L_ = len(lists[0])
                            for i_ in range(L_):
                                for n, mm in zip(ns, lists):
                                    S.op('pe', mm[i_], reads=['kTD', 'qTD', 'identb', 'nbb'], writes=[f'psS4{n % 2}'], sig=(i_ == L_ - 1))
                            for n in ns:
                                t, h, kl = units[n]
                                nk = len(kl)
                                pss = psS[n % 2]
                                et = ETd[n % 4]
                                S.op('act', lambda pss=pss, et=et, nk=nk: A_.activation(out=et[:, 0:nk, :], in_=pss[:, 0:nk, :], func=AF.Exp, scale=0.125),
                                     reads=[f'psS4{n % 2}'], writes=[f'ETd{n % 4}'])

                        def emit_AV4(n):
                            t, h, kl = units[n]
                            et = ETd[n % 4]
                            nk = len(kl)
                            tp = (n // 4) % 2
                            po = psO[tp]
                            pk = f'psO4{tp}'
                            for idx, (kt, v) in enumerate(kl):
                                S.op('pe', lambda idx=idx, kt=kt: P_.matmul(po[:, h, 0:65], lhsT=et[:, idx, :], rhs=vD[:, kt, h, :], start=(idx == 0), stop=(idx == nk - 1)),
                                     reads=[f'ETd{n % 4}', 'vD'], writes=[pk], sig=(idx == nk - 1))
                            if h == 3:
                                S.op('dve', lambda: V.reciprocal(out=rr[:, 0:4], in_=po[:, 0:4, 64]), reads=[pk], writes=['rr4'])
                                for hh in range(4):
                                    S.op('dve', lambda hh=hh: V.tensor_scalar(out=yD[:, hh * 64:(hh + 1) * 64], in0=po[:, hh, 0:64], scalar1=rr[:, hh:hh + 1], scalar2=None, op0=ALU.mult),
                                         reads=[pk, 'rr4'], writes=['yD'])
                                head_norm(yD[:, :], ['yD'], 3, t * 128, (hn_sq, hn_ss, hn_yb), 'hn')

                        npairs = len(units) // 2
                        for p in range(npairs + 1):
                            if p < npairs:
                                emit_S4pair(p)
                            if p - 1 >= 0:
                                emit_AV4(2 * (p - 1))
                                emit_AV4(2 * (p - 1) + 1)
                        S.barrier()
                    if stop_after == 'p4':
                        S.finish()
                        return nc

                with ExitStack() as p5:
                    woutb = T(p5, "woutb", [128, 8, D], BF16)
                    g1v = [T(p5, f"g1v{i}", [128, D], F32) for i in range(2)]
                    A2 = [T(p5, f"A2_{i}", [128, D], F32) for i in range(2)]
                    sh2 = [T(p5, f"sh2_{i}", [128, D], F32) for i in range(2)]
                    tmpv = T(p5, "tmpv5", [128, D], F32)
                    rwf = T(p5, "rwf", [128, 8, 48], F32)
                    ynb = [T(p5, f"ynb{i}", [128, D], BF16) for i in range(2)]
                    ynT = T(p5, "ynT", [128, 8, 128], BF16)
                    xt = [T(p5, f"xt5_{i}", [128, D], F32) for i in range(2)]
                    xn = [T(p5, f"xn{i}", [128, D], F32) for i in range(2)]
                    sq5 = T(p5, "sq5", [128, D], F32)
                    ss5 = T(p5, "ss5", [128, 1], F32)
                    h2f = T(p5, "h2f", [128, D], F32)
                    h2b = T(p5, "h2b", [128, D], BF16)
                    h2T = T(p5, "h2T", [128, 8, 128], F32)
                    psT = PS(p5, "psT5", [128, 8, 128], BF16)
                    psM = PS(p5, "psM", [128, 2, 512])
                    psH = PS(p5, "psH", [128, 8, 128])
                    psR = PS(p5, "psR", [128, 512])
                    S.dmaf('pool', lambda: G_.dma_start(out=woutb[:], in_=w_out[l].rearrange("(c p) n -> p c n", p=128)), writes=['woutb'])
                    S.op('dve', lambda: V.memset(rwf[:], 0.0), writes=['rwf'])
                    S.dma('sp', rwf[:, :, 32 * b:32 * b + 16], rw[l].rearrange("(c p) e -> p c e", p=128), reads=['rwf'], writes=['rwf'])
                    for i, r in enumerate((b, 2)):
                        if i == 1 and last:
                            continue
                        bcast_row(p5, 'sp', g1v[i][:], modrow(r, 2), f'g1v{i}')
                        bcast_row(p5, 'sp', tmpv[:], modrow(r, 4), 'tmpv5')
                        bcast_row(p5, 'sp', A2[i][:], n2g[l], f'A2_{i}')
                        S.op('dve', lambda i=i: V.scalar_tensor_tensor(out=A2[i][:], in0=tmpv[:], scalar=1.0, in1=A2[i][:], op0=ALU.add, op1=ALU.mult),
                             reads=['tmpv5', f'A2_{i}'], writes=[f'A2_{i}'])
                        bcast_row(p5, 'sp', sh2[i][:], modrow(r, 3), f'sh2_{i}')
                    qtiles = list(range(NT)) + ([] if last else [NT, NT + 1])
                    M = 32 * b + 16
                    dt = [T(p5, f"dt{i}", [128, 2, NT, 128], BF16) for i in range(2)]
                    f_sq = T(p5, "f_sq", [128, 256], F32)
                    f_ss = T(p5, "f_ss", [128, 4], F32)
                    f_yb = T(p5, "f_yb", [128, 256], BF16)
                    psY = PS(p5, "psY", [128, 512])

                    def fourier_load(j):
                        isctx = j >= NT
                        tb = dt[j % 2]
                        tk = f'dt{j % 2}'
                        if not isctx:
                            S.dma('pool', tb[:], dftN[j], writes=[tk])
                        else:
                            jj = j - NT
                            for cs in range(2):
                                S.dma('pool', tb[:, cs, 0:2, :], dftC[cs][:, jj * 128:(jj + 1) * 128].rearrange("(i p) n -> p i n", p=128), writes=[tk])

                    def fourier_group(j):
                        isctx = j >= NT
                        tb = dt[j % 2]
                        tk = f'dt{j % 2}'
                        ins = [(0, NT), (1, NT + 1)] if isctx else [(i, i) for i in range(NT)]
                        n_mm = 2 * len(ins)
                        k = 0
                        for cs in range(2):
                            for (ii, ti) in ins:
                                S.op('pe', lambda cs=cs, ii=ii, ti=ti, k=k: P_.matmul(psY[:, 0:256], lhsT=tb[:, cs, ii, :], rhs=Ub[:, ti, cs * 256:(cs + 1) * 256],
                                                                                   start=(k == 0), stop=(k == n_mm - 1)),
                                     reads=[tk, 'Ub'], writes=['psY'], sig=(k == n_mm - 1))
                                k += 1
                        head_norm(psY[:, 0:256], ['psY'], 1, j * 128, (f_sq, f_ss, f_yb), 'fh', q='pool')


                    def p5A(ti):
                        isctx = ti >= NT
                        mi = 1 if isctx else 0
                        yb_ = ynb[ti % 2]
                        yk = f'ynb{ti % 2}'
                        xb_ = xt[ti % 2]
                        xk = f'xt5_{ti % 2}'
                        xn_ = xn[ti % 2]
                        xnk = f'xn{ti % 2}'
                        S.dma('sp', yb_[:], ynd[ti * 128:(ti + 1) * 128, :], reads=[f'ynd{ti * 128}_{g}' for g in range(4)], writes=[yk])
                        yield
                        src = csrc(b)[(ti - NT) * 128:(ti - NT + 1) * 128, :] if isctx else xsrc(b)[ti * 128:(ti + 1) * 128, :]
                        dst = xc[b][(ti - NT) * 128:(ti - NT + 1) * 128, :] if isctx else out[b][ti * 128:(ti + 1) * 128, :]
                        S.dma('sp', xb_[:], src, reads=['xres'], writes=[xk])
                        yield
                        for c in range(8):
                            S.op('pe', lambda c=c: P_.transpose(out=psT[:, c, :], in_=yb_[:, c * 128:(c + 1) * 128], identity=identb[:]),
                                 reads=[yk, 'identb'], writes=['psT5'], sig=(c == 7))
                        yield
                        S.op('act', lambda: A_.copy(out=ynT[:], in_=psT[:]), reads=['psT5'], writes=['ynT'])
                        yield
                        for hf in range(2):
                            for c in range(8):
                                S.op('pe', lambda c=c, hf=hf: P_.matmul(psM[:, hf, :], lhsT=ynT[:, c, :], rhs=woutb[:, c, hf * 512:(hf + 1) * 512], start=(c == 0), stop=(c == 7)),
                                     reads=['ynT', 'woutb'], writes=['psM'], sig=(c == 7 and hf == 1))
                        yield
                        S.op('dve', lambda: V.tensor_tensor(out=xn_[:], in0=psM[:].rearrange("p a n -> p (a n)"), in1=g1v[mi][:], op=ALU.mult),
                             reads=['psM', f'g1v{mi}'], writes=[xnk])
                        yield
                        S.op('dve', lambda: V.tensor_tensor(out=xn_[:], in0=xn_[:], in1=xb_[:], op=ALU.add), reads=[xnk, xk], writes=[xnk])
                        yield
                        S.dma('sp', dst, xn_[:], reads=[xnk], writes=['xres_w'])
                        yield

                    def p5B(ti):
                        isctx = ti >= NT
                        mi = 1 if isctx else 0
                        xn_ = xn[ti % 2]
                        xnk = f'xn{ti % 2}'
                        S.op('act', lambda: A_.activation(out=sq5[:], in_=xn_[:], func=AF.Square), reads=[xnk], writes=['sq5'])
                        yield
                        S.op('dve', lambda: V.tensor_reduce(out=ss5[:, 0:1], in_=sq5[:], axis=AX.X, op=ALU.add), reads=['sq5'], writes=['ss5'])
                        yield
                        yield from rstd_g('ss5', ss5[:, 0:1], 1.0 / D)
                        S.op('dve', lambda: V.scalar_tensor_tensor(out=sq5[:], in0=xn_[:], scalar=ss5[:, 0:1], in1=A2[mi][:], op0=ALU.mult, op1=ALU.mult),
                             reads=[xnk, 'ss5', f'A2_{mi}'], writes=['sq5'])
                        yield
                        S.op('dve', lambda: V.tensor_tensor(out=h2f[:], in0=sq5[:], in1=sh2[mi][:], op=ALU.add), reads=['sq5', f'sh2_{mi}'], writes=['h2f'])
                        yield
                        S.op('act', lambda: A_.copy(out=h2b[:], in_=h2f[:]), reads=['h2f'], writes=['h2b'])
                        yield
                        hdst = h2c[b][(ti - NT) * 128:(ti - NT + 1) * 128, :] if isctx else h2l[b][ti * 128:(ti + 1) * 128, :]
                        S.dma('sp', hdst, h2b[:], reads=['h2b'], writes=['h2d'])
                        yield
                        for c in range(8):
                            S.op('pe', lambda c=c: P_.transpose(out=psH[:, c, :], in_=h2f[:, c * 128:(c + 1) * 128], identity=identf[:]),
                                 reads=['h2f', 'identf'], writes=['psH'], sig=(c == 7))
                        yield
                        S.op('dve', lambda: V.tensor_copy(out=h2T[:], in_=psH[:]), reads=['psH'], writes=['h2T'])
                        yield
                        for c in range(8):
                            S.op('pe', lambda c=c: P_.matmul(psR[0:M, 0:128], lhsT=rwf[:, c, 0:M], rhs=h2T[:, c, :], start=(c == 0), stop=(c == 7)),
                                 reads=['rwf', 'h2T'], writes=['psR'], sig=(c == 7))
                        yield
                        S.op('act', lambda: A_.activation(out=Eaff[32 * b:32 * b + 16, ti * 128:(ti + 1) * 128], in_=psR[32 * b:32 * b + 16, 0:128], func=AF.Exp),
                             reads=['psR'], writes=['Eaff'])
                        yield

                    FLEAD = 2
                    fourier_load(qtiles[0])
                    for j_ in range(min(FLEAD, len(qtiles))):
                        if j_ + 1 < len(qtiles):
                            fourier_load(qtiles[j_ + 1])
                        fourier_group(qtiles[j_])
                    for k in range(len(qtiles) + 1):
                        if k + FLEAD < len(qtiles):
                            if k + FLEAD + 1 < len(qtiles):
                                fourier_load(qtiles[k + FLEAD + 1])
                            fourier_group(qtiles[k + FLEAD])
                        gens = []
                        if k < len(qtiles):
                            gens.append(p5A(qtiles[k]))
                        if k - 1 >= 0:
                            gens.append(p5B(qtiles[k - 1]))
                        run_interleaved(gens)
                    S.barrier()
                sb.close()
                if stop_after == 'p5' and b == samples[-1]:
                    S.finish()
                    return nc

            with ExitStack() as p6:
                nslot = 512 + (0 if last else 64)
                wbuf = [[T(p6, f"w{n}{i}", [128, 8, D], BF16) for n in ('g', 'u', 'd')] for i in range(2)]
                g2v = [T(p6, f"g2v{i}", [128, D], F32) for i in range(3)]
                aff = T(p6, "aff", [48, SEQ + CTXL], F32)
                aff0 = T(p6, "aff0", [48, SEQ + CTXL], F32)
                rec = T(p6, "rec", [48, 512], F32)
                vals = T(p6, "vals", [48, CAP + CAPC], F32)
                idxu = T(p6, "idxu", [48, CAP + CAPC], U32)
                idxf = T(p6, "idxf", [48, CAP + CAPC], F32)
                gateT = T(p6, "gateT", [128, 3, 48], F32)
                idxI = T(p6, "idxI", [128, 3, 48], I32)
                idxS = T(p6, "idxS", [128, 3, 48], I32)
                xg = [[T(p6, f"xg{p_}_{i}", [128, D], BF16) for i in range(4)] for p_ in range(2)]
                xgc = [[T(p6, f"xgc{p_}_{i}", [32, D], BF16) for i in range(2)] for p_ in range(2)]
                xgT = T(p6, "xgT", [128, 8, 576], BF16)
                hidT = T(p6, "hidT", [128, 8, 576], BF16)
                sil = [T(p6, f"sil{i}", [128, 576], F32) for i in range(2)]
                osb = [T(p6, f"osb{i}", [128, D], F32) for i in range(2)]
                psA = PS(p6, "psA6", [128, 512])
                psG = [PS(p6, f"psG6{i}", [128, 512]) for i in range(2)]
                psU = [PS(p6, f"psU6{i}", [128, 512]) for i in range(2)]
                psT = PS(p6, "psT6", [128, 8, 128], BF16)
                psD = [PS(p6, f"psD6{i}", [128, 512]) for i in range(2)]
                for i, r in enumerate((0, 1, 2)):
                    bcast_row(p6, 'sp', g2v[i][:], modrow(r, 5), f'g2v{i}')
                segs = [(0, 512), (512, 512), (1024, 512), (1536, 512)] + ([] if last else [(SEQ, 256)])
                for (c0, cn) in segs:
                    S.op('pe', lambda c0=c0, cn=cn: P_.matmul(psA[0:48, 0:cn], lhsT=onesblk[:, :], rhs=Eaff[:, c0:c0 + cn], start=True, stop=True),
                         reads=['onesblk', 'Eaff'], writes=['psA6'])
                    S.op('dve', lambda cn=cn: V.reciprocal(out=rec[:, 0:cn], in_=psA[0:48, 0:cn]), reads=['psA6'], writes=['rec'])
                    S.op('dve', lambda c0=c0, cn=cn: V.tensor_tensor(out=aff[:, c0:c0 + cn], in0=Eaff[:, c0:c0 + cn], in1=rec[:, 0:cn], op=ALU.mult),
                         reads=['Eaff', 'rec'], writes=['aff'])
                    S.op('act', lambda c0=c0, cn=cn: A_.copy(out=aff0[:, c0:c0 + cn], in_=aff[:, c0:c0 + cn]), reads=['aff'], writes=['aff0'])
                tsegs = [(0, SEQ, CAP, 0)] + ([] if last else [(SEQ, CTXL, CAPC, CAP)])
                for (c0, cn, cap, o0) in tsegs:
                    for j in range(cap // 8):
                        vk = f'vals{o0}_{j}'
                        S.op('dve', lambda c0=c0, cn=cn, j=j, o0=o0: V.max(out=vals[:, o0 + 8 * j:o0 + 8 * j + 8], in_=aff[:, c0:c0 + cn]), reads=['aff'], writes=[vk])
                        S.op('dve', lambda c0=c0, cn=cn, j=j, o0=o0: V.match_replace(out=aff[:, c0:c0 + cn], in_to_replace=vals[:, o0 + 8 * j:o0 + 8 * j + 8], in_values=aff[:, c0:c0 + cn], imm_value=-1.0),
                             reads=['aff', vk], writes=['aff'])
                        S.op('dve', lambda c0=c0, cn=cn, j=j, o0=o0: V.max_index(out=idxu[:, o0 + 8 * j:o0 + 8 * j + 8], in_max=vals[:, o0 + 8 * j:o0 + 8 * j + 8], in_values=aff0[:, c0:c0 + cn]),
                             reads=['aff0', vk], writes=['idxu'])
                ncol_ = CAP + (0 if last else CAPC)
                S.op('dve', lambda: V.tensor_copy(out=idxf[:, 0:ncol_], in_=idxu[:, 0:ncol_]), reads=['idxu'], writes=['idxf', 'vals_all'])
                tl = [(0, 128, 0), (128, 128, 1)] + ([] if last else [(CAP, CAPC, 2)])
                for (o0, on, j) in tl:
                    for srcT, dstT, dk in ((vals, gateT, 'gateT'), (idxf, idxI, 'idxI')):
                        S.op('pe', lambda srcT=srcT, o0=o0, on=on: P_.transpose(out=psA[0:on, 0:48], in_=srcT[0:48, o0:o0 + on], identity=identf[0:48, 0:48]),
                             reads=['vals_all', 'idxf', 'identf'], writes=['psA6'])
                        S.op('dve', lambda dstT=dstT, on=on, j=j: V.tensor_copy(out=dstT[0:on, j, :], in_=psA[0:on, 0:48]), reads=['psA6'], writes=[dk])
                        if dk == 'idxI':
                            off = float(SEQ if j < 2 else CTXL)
                            S.op('dve', lambda on=on, j=j: V.tensor_copy(out=idxS[0:on, j, 0:32], in_=psA[0:on, 0:32]), reads=['psA6'], writes=['idxS'])
                            S.op('dve', lambda on=on, j=j, off=off: V.tensor_scalar(out=idxS[0:on, j, 32:48], in0=psA[0:on, 32:48], scalar1=off, scalar2=None, op0=ALU.add),
                                 reads=['psA6'], writes=['idxS'])

                def load_w(e):
                    bufs = wbuf[e % 2]
                    for n, (wt, bt) in enumerate(zip((wg, wu, wd), bufs)):
                        S.dmaf('pool', lambda wt=wt, bt=bt: G_.dma_start(out=bt[:], in_=wt[l, e].rearrange("(c p) f -> p c f", p=128)),
                               writes=[f'w{"gud"[n]}{e % 2}'])

                def gather(e):
                    par = e % 2
                    for bb in range(2):
                        for hf in range(2):
                            j = bb * 2 + hf
                            S.dmaf('pool', lambda j=j, bb=bb, hf=hf: G_.indirect_dma_start(out=xg[par][j][:, :], out_offset=None, in_=h2l[bb][:, :],
                                                                                         in_offset=bass.IndirectOffsetOnAxis(ap=idxI[:, hf, 32 * bb + e:32 * bb + e + 1], axis=0)),
                                   reads=['idxI', 'h2d'], writes=[f'xg{par}_{j}'])
                        if not last:
                            S.dmaf('pool', lambda bb=bb: G_.indirect_dma_start(out=xgc[par][bb][:, :], out_offset=None, in_=h2c[bb][:, :],
                                                                             in_offset=bass.IndirectOffsetOnAxis(ap=idxI[0:32, 2, 32 * bb + e:32 * bb + e + 1], axis=0)),
                                   reads=['idxI', 'h2d'], writes=[f'xgc{par}_{bb}'])

                load_w(0)
                gather(0)
                for e in range(NE):
                    par = e % 2
                    if e + 1 < NE:
                        load_w(e + 1)
                        gather(e + 1)
                    wgb, wub, wdb = wbuf[e % 2]
                    wk = [f'w{n}{e % 2}' for n in 'gud']
                    for j in range(4):
                        for c in range(8):
                            S.op('pe', lambda j=j, c=c: P_.transpose(out=psT[:, c, :], in_=xg[par][j][:, c * 128:(c + 1) * 128], identity=identb[:]),
                                 reads=[f'xg{par}_{j}', 'identb'], writes=['psT6'], sig=(c == 7))
                        S.op('act', lambda j=j: A_.copy(out=xgT[:, :, j * 128:(j + 1) * 128], in_=psT[:]), reads=['psT6'], writes=['xgT'])
                    if not last:
                        for bb in range(2):
                            for c in range(8):
                                S.op('pe', lambda bb=bb, c=c: P_.transpose(out=psT[:, c, 0:32], in_=xgc[par][bb][0:32, c * 128:(c + 1) * 128], identity=identb[0:32, 0:32]),
                                     reads=[f'xgc{par}_{bb}', 'identb'], writes=['psT6'], sig=(c == 7))
                            S.op('act', lambda bb=bb: A_.copy(out=xgT[:, :, 512 + 32 * bb:512 + 32 * bb + 32], in_=psT[:, :, 0:32]), reads=['psT6'], writes=['xgT'])
                    for f in range(8):
                        fp = f % 2
                        for (wb_, ps_, pk, wkk, coff) in ((wgb, psG[fp], f'psG6{fp}', wk[0], 0), (wub, psU[fp], f'psU6{fp}', wk[1], 64)):
                            for c in range(8):
                                S.op('pe', lambda wb_=wb_, ps_=ps_, c=c, f=f: P_.matmul(ps_[:, :], lhsT=wb_[:, c, f * 128:(f + 1) * 128], rhs=xgT[:, c, 0:512], start=(c == 0), stop=(c == 7)),
                                     reads=[wkk, 'xgT'], writes=[pk], sig=(c == 7))
                            if not last:
                                for c in range(8):
                                    S.op('pe', lambda wb_=wb_, c=c, f=f, coff=coff: P_.matmul(psA[:, coff:coff + 64], lhsT=wb_[:, c, f * 128:(f + 1) * 128], rhs=xgT[:, c, 512:576], start=(c == 0), stop=(c == 7)),
                                         reads=[wkk, 'xgT'], writes=['psA6'], sig=(c == 7))
                        S.op('act', lambda fp=fp: A_.activation(out=sil[fp][:, 0:512], in_=psG[fp][:, :], func=AF.Silu), reads=[f'psG6{fp}'], writes=[f'sil{fp}'])
                        S.op('dve', lambda f=f, fp=fp: V.tensor_tensor(out=hidT[:, f, 0:512], in0=psU[fp][:, :], in1=sil[fp][:, 0:512], op=ALU.mult), reads=[f'psU6{fp}', f'sil{fp}'], writes=['hidT'])
                        if not last:
                            S.op('act', lambda fp=fp: A_.activation(out=sil[fp][:, 512:576], in_=psA[:, 0:64], func=AF.Silu), reads=['psA6'], writes=[f'sil{fp}'])
                            S.op('dve', lambda f=f, fp=fp: V.tensor_tensor(out=hidT[:, f, 512:576], in0=psA[:, 64:128], in1=sil[fp][:, 512:576], op=ALU.mult), reads=['psA6', f'sil{fp}'], writes=['hidT'])
                    jobs = [(bb * 2 + hf, 128, bb, hf, False) for bb in range(2) for hf in range(2)]
                    if not last:
                        jobs += [(None, 32, 0, 2, True), (None, 32, 1, 2, True)]
                    for jn, (j, mrows, bb, hf, isc) in enumerate(jobs):
                        s0 = (512 + 32 * bb) if isc else j * 128
                        ob = osb[jn % 2]
                        ok_ = f'osb{jn % 2}'
                        gi = 2 if isc else bb
                        for hh in range(2):
                            for f in range(8):
                                S.op('pe', lambda s0=s0, mrows=mrows, hh=hh, f=f: P_.matmul(psD[hh][0:mrows, :], lhsT=hidT[:, f, s0:s0 + mrows], rhs=wdb[:, f, hh * 512:(hh + 1) * 512], start=(f == 0), stop=(f == 7)),
                                     reads=['hidT', wk[2]], writes=[f'psD6{hh}'], sig=(f == 7))
                            S.op('dve', lambda ob=ob, mrows=mrows, hf=hf, bb=bb, e=e, gi=gi, hh=hh: V.scalar_tensor_tensor(out=ob[0:mrows, hh * 512:(hh + 1) * 512], in0=psD[hh][0:mrows, :],
                                                                                                            scalar=gateT[0:mrows, hf, 32 * bb + e:32 * bb + e + 1], in1=g2v[gi][0:mrows, hh * 512:(hh + 1) * 512], op0=ALU.mult, op1=ALU.mult),
                                 reads=[f'psD6{hh}', 'gateT', f'g2v{gi}'], writes=[ok_])
                        tgt = xc.rearrange("b n d -> (b n) d") if isc else out.rearrange("b n d -> (b n) d")
                        S.dmaf('pool', lambda ob=ob, mrows=mrows, hf=hf, bb=bb, e=e, tgt=tgt: G_.indirect_dma_start(out=tgt, out_offset=bass.IndirectOffsetOnAxis(ap=idxS[0:mrows, hf, 32 * bb + e:32 * bb + e + 1], axis=0),
                                                                                                           in_=ob[0:mrows, :], in_offset=None, compute_op=ALU.add),
                               reads=[ok_, 'idxS'], writes=['xres_w'])
                S.barrier()
        S.finish()
    return nc


_CONSTS = None


def prep_shared(inp):
    global _CONSTS
    if _CONSTS is None:
        _CONSTS = _consts()
    f = lambda a: np.ascontiguousarray(np.asarray(a, dtype=np.float32))
    sh = dict(_CONSTS)
    sh["ada_w"] = f(inp["ada_w"]); sh["ada_b"] = f(inp["ada_b"])
    sh["n1g"] = f(inp["norm1_g"]); sh["n2g"] = f(inp["norm2_g"])
    sh["w_in"] = f(inp["w_in"]); sh["w_out"] = f(inp["w_out"]); sh["hog"] = f(inp["head_out_g"])
    sh["sguT"] = np.ascontiguousarray(f(inp["sgu_w"]).transpose(0, 1, 3, 2))
    sh["sgubT"] = np.ascontiguousarray(f(inp["sgu_b"]).transpose(0, 2, 1))
    sh["qkgC"] = np.ascontiguousarray(np.concatenate([np.tile(f(inp["diff_qn_g"]), (1, 8)), np.tile(f(inp["diff_kn_g"]), (1, 8))], axis=1))
    sh["qkgD"] = np.ascontiguousarray(np.concatenate([np.tile(f(inp["na_qn_g"]), (1, 4)), np.tile(f(inp["na_kn_g"]), (1, 4))], axis=1))
    sh["dlam"] = np.ascontiguousarray(f(inp["diff_lambda"]).reshape(DEPTH, 128))
    sh["nbias"] = _na_bias_host(f(inp["na_rpb"]))
    sh["rw"] = f(inp["router_w"])
    sh["wg"] = f(inp["exp_w_gate"]); sh["wu"] = f(inp["exp_w_up"]); sh["wd"] = f(inp["exp_w_down"])
    return sh


def prep_core(inp, shared, core):
    m = dict(shared)
    b0 = 2 * core
    m["x"] = np.ascontiguousarray(np.asarray(inp["x"][b0:b0 + 2], dtype=np.float32))
    m["ctx"] = np.ascontiguousarray(np.asarray(inp["ctx"][b0:b0 + 2], dtype=np.float32))
    cv = np.concatenate([np.asarray(inp["c"][b0:b0 + 2], dtype=np.float32), np.asarray(inp["c_ctx"], dtype=np.float32)[None]], axis=0)
    m["cT"] = np.ascontiguousarray(cv.reshape(3, 8, 128).transpose(2, 1, 0))
    return m


def kernel(**inputs):
    inp = {k: np.asarray(v) for k, v in inputs.items()}
    shared = prep_shared(inp)
    nc = build_program()
    in_maps = [prep_core(inp, shared, c) for c in range(8)]
    res = run_bass_kernel_spmd(nc, in_maps, core_ids=list(range(8)))
    outs = [np.asarray(res.results[c]["out"], dtype=np.float32) for c in range(8)]
    return np.concatenate(outs, axis=0)
```

```python
import math
import numpy as np
import ml_dtypes
from contextlib import ExitStack
import concourse.bass as bass
import concourse.mybir as mybir
from concourse.bass_utils import run_bass_kernel_spmd

F32 = mybir.dt.float32
BF16 = mybir.dt.bfloat16
I32 = mybir.dt.int32
U32 = mybir.dt.uint32
AF = mybir.ActivationFunctionType
ALU = mybir.AluOpType
AX = mybir.AxisListType

D = 1024
SEQ = 2048
CTXL = 256
NT = 16
NTC = 2
NTA = NT + NTC
DEPTH = 2
IN_W = 2304
NE = 16
CAP = 256
CAPC = 32
EPS = 1e-6
NEGBIG = -30000.0
LAST_NAMES = {}


class Sched:
    NDS = 24

    def __init__(self, nc, stack):
        self.nc = nc
        self.stack = stack
        self.E = dict(pe=nc.tensor, act=nc.scalar, dve=nc.vector, pool=nc.gpsimd, sp=nc.sync)
        self.esem = {}
        self.ecnt = {}
        self.etok = {e: None for e in self.E}
        self.nsem = 0
        for e in self.E:
            self._newsem(e)
        self.dsem = [stack.enter_context(nc.semaphore(f"dq{i}")) for i in range(self.NDS)]
        self.dcnt = [0] * self.NDS
        self.dnext = {'sp': 0, 'pool': 0, 'act': 0}
        self.drange = {'sp': (0, 14), 'pool': (14, 24), 'act': (0, 14)}
        self.last = {}
        self.waited = {e: {} for e in self.E}
        self.pending = {e: [] for e in self.E}
        self.nwaits = 0
        self.ninst = 0

    def _newsem(self, e):
        self.esem[e] = self.stack.enter_context(self.nc.semaphore(f"s_{e}_{self.nsem}"))
        self.nsem += 1
        self.ecnt[e] = 0

    def _wait(self, e, tok):
        if tok is None:
            return
        sem, val, sid = tok[0], tok[1], tok[2]
        w = self.waited[e]
        if w.get(sid, 0) >= val:
            return
        w[sid] = val
        self.E[e].wait_ge(sem, val)
        self.nwaits += 1

    def _deps(self, e, reads, writes, is_pe=False):
        for k in reads:
            st = self.last.get(k)
            if st is not None:
                self._wait(e, st['w'])
                if k.startswith('ps'):
                    for rt in st['r']:
                        if rt[3] != e:
                            self._wait(e, rt)
        for k in writes:
            st = self.last.get(k)
            if st is not None:
                wt = st['w']
                if not (is_pe and wt is not None and wt[3] == 'pe'):
                    self._wait(e, wt)
                for rt in st['r']:
                    if is_pe and rt[3] == 'pe':
                        continue
                    self._wait(e, rt)

    def _record(self, tok, reads, writes):
        for k in reads:
            st = self.last.setdefault(k, {'w': None, 'r': []})
            st['r'].append(tok)
            if len(st['r']) > 48:
                st['r'] = st['r'][-48:]
        for k in writes:
            self.last[k] = {'w': tok, 'r': []}

    def op(self, e, fn, reads=(), writes=(), sig=True):
        self._deps(e, reads, writes, is_pe=(e == 'pe'))
        inst = fn()
        self.ninst += 1
        if not sig:
            self.pending[e].append((tuple(reads), tuple(writes)))
            return inst
        if self.ecnt[e] >= 30000:
            self._newsem(e)
        self.ecnt[e] += 1
        sem = self.esem[e]
        inst.then_inc(sem, 1)
        tok = (sem, self.ecnt[e], id(sem), e)
        self.etok[e] = tok
        for (r, w) in self.pending[e]:
            self._record(tok, r, w)
        self.pending[e] = []
        self._record(tok, reads, writes)
        return inst

    def dmaf(self, q, fn, reads=(), writes=()):
        lo, hi = self.drange[q]
        i = lo + self.dnext[q]
        self.dnext[q] = (self.dnext[q] + 1) % (hi - lo)
        sem = self.dsem[i]
        if self.dcnt[i] > 0:
            self._wait(q, (sem, 16 * self.dcnt[i], id(sem), 'dma'))
        self._deps(q, reads, writes)
        inst = fn()
        self.ninst += 1
        self.dcnt[i] += 1
        inst.then_inc(sem, 16)
        tok = (sem, 16 * self.dcnt[i], id(sem), 'dma')
        self._record(tok, reads, writes)
        return tok

    def dma(self, q, out, in_, reads=(), writes=(), **kw):
        return self.dmaf(q, lambda: self.E[q].dma_start(out=out, in_=in_, **kw), reads, writes)

    def barrier(self):
        for e in self.E:
            assert not self.pending[e], f"pending non-signaled ops on {e}"
        toks = [t for t in self.etok.values() if t is not None]
        for i in range(self.NDS):
            if self.dcnt[i] > 0:
                toks.append((self.dsem[i], 16 * self.dcnt[i], id(self.dsem[i]), 'dma'))
        for e in self.E:
            for t in toks:
                if t[3] == e:
                    continue
                self._wait(e, t)
        self.last = {}

    def finish(self):
        self.barrier()


def _na_structure():
    rows = 32
    variants = {}
    var_list = []
    per_t = []
    for t in range(NT):
        lst = []
        for kt in range(NT):
            sig = []
            anyv = False
            for a in range(2):
                for b_ in range(2):
                    r = 2 * t + b_
                    kr = 2 * kt + a
                    rs = min(max(r - 4, 0), rows - 8)
                    if rs <= kr <= rs + 7:
                        sig.append(kr - r + 7)
                        anyv = True
                    else:
                        sig.append(-1)
            if not anyv:
                continue
            sig = tuple(sig)
            if sig not in variants:
                variants[sig] = len(var_list)
                var_list.append(sig)
            lst.append((kt, variants[sig]))
        per_t.append(lst)
    return per_t, var_list


NA_PER_T, NA_VARS = _na_structure()
NVAR = len(NA_VARS)


def _na_bias_host(rpb):
    L, H = rpb.shape[0], rpb.shape[1]
    qc = np.arange(64)
    kc = np.arange(64)
    wstart = np.clip(qc - 8, 0, 48)
    valid_c = (kc[:, None] >= wstart[None, :]) & (kc[:, None] < wstart[None, :] + 16)
    coff = np.clip(kc[:, None] - qc[None, :], -15, 15) + 15
    flat = np.concatenate([rpb.reshape(L, H, 15 * 31), np.full((L, H, 1), NEGBIG, np.float32)], axis=-1)
    idx = np.full((NVAR, 128, 128), 15 * 31, np.int64)
    for v, sig in enumerate(NA_VARS):
        n = 0
        for a in range(2):
            for b_ in range(2):
                dr = sig[n]
                n += 1
                if dr < 0:
                    continue
                blk = np.where(valid_c, dr * 31 + coff, 15 * 31)
                idx[v, a * 64:(a + 1) * 64, b_ * 64:(b_ + 1) * 64] = blk
    out = flat[:, :, idx.reshape(-1)].reshape(L, H, NVAR, 128, 128)
    return np.ascontiguousarray(out.astype(np.float32))


def _consts():
    c = {}
    half = 16
    inv = 1.0 / (10000.0 ** (np.arange(0, half, 2, dtype=np.float32) / half))
    t = np.arange(SEQ)
    row = (t // 64).astype(np.float32)[:, None] * inv
    col = (t % 64).astype(np.float32)[:, None] * inv
    cr, sr, cc, sc = np.cos(row), np.sin(row), np.cos(col), np.sin(col)
    cos32 = np.concatenate([cr, cr, cc, cc], axis=1)
    sin32 = np.concatenate([-sr, sr, -sc, sc], axis=1)
    rope = np.stack([np.tile(cos32, (1, 16)), np.tile(sin32, (1, 16))], axis=1)
    c["ropeT"] = np.ascontiguousarray(rope.astype(np.float32))

    def dft(n, scale):
        k = np.arange(n, dtype=np.float64)
        ang = 2.0 * np.pi * np.outer(k, k) / n
        return np.cos(ang) * scale, -np.sin(ang) * scale
    cN, sN = dft(SEQ, 1.0 / math.sqrt(SEQ * 64.0))
    tb = np.stack([cN, sN]).reshape(2, NT, 128, NT, 128)
    c["dftN"] = np.ascontiguousarray(tb.transpose(3, 2, 0, 1, 4)).astype(ml_dtypes.bfloat16)
    cC, sC = dft(CTXL, 1.0 / math.sqrt(CTXL * 64.0))
    c["dftC"] = np.stack([cC, sC]).astype(ml_dtypes.bfloat16)
    c64, s64 = dft(64, 1.0)
    s64 = -s64
    cb = np.zeros((256, 256)); sb = np.zeros((256, 256))
    for h in range(4):
        cb[h * 64:(h + 1) * 64, h * 64:(h + 1) * 64] = c64
        sb[h * 64:(h + 1) * 64, h * 64:(h + 1) * 64] = s64
    c["dftH"] = np.stack([cb, sb]).astype(ml_dtypes.bfloat16)
    c["identb"] = np.eye(128).astype(ml_dtypes.bfloat16)
    c["identf"] = np.eye(128, dtype=np.float32)
    ob = np.zeros((48, 48), np.float32)
    for g in range(3):
        ob[g * 16:(g + 1) * 16, g * 16:(g + 1) * 16] = 1.0
    c["onesblk"] = ob
    return c


def build_program(stop_after=None, n_layers=DEPTH, samples=(0, 1)):
    nc = bass.Bass("TRN2", target_bir_lowering=False)
    dt_in = lambda name, shape, dt=F32: nc.dram_tensor(name, list(shape), dt, kind="ExternalInput").ap()
    x_in = dt_in("x", [2, SEQ, D])
    ctx_in = dt_in("ctx", [2, CTXL, D])
    cT_in = dt_in("cT", [128, 8, 3])
    ada_w = dt_in("ada_w", [DEPTH, D, 6 * D])
    ada_b = dt_in("ada_b", [DEPTH, 6 * D])
    n1g = dt_in("n1g", [DEPTH, D])
    n2g = dt_in("n2g", [DEPTH, D])
    w_in = dt_in("w_in", [DEPTH, D, IN_W])
    w_out = dt_in("w_out", [DEPTH, D, D])
    hog = dt_in("hog", [DEPTH, D])
    sguT = dt_in("sguT", [DEPTH, 4, 128, 128])
    sgubT = dt_in("sgubT", [DEPTH, 128, 4])
    qkgC = dt_in("qkgC", [DEPTH, 512])
    qkgD = dt_in("qkgD", [DEPTH, 512])
    dlam = dt_in("dlam", [DEPTH, 128])
    nbias = dt_in("nbias", [DEPTH, 4, NVAR, 128, 128])
    rw = dt_in("rw", [DEPTH, D, NE])
    wg = dt_in("wg", [DEPTH, NE, D, D])
    wu = dt_in("wu", [DEPTH, NE, D, D])
    wd = dt_in("wd", [DEPTH, NE, D, D])
    ropeT = dt_in("ropeT", [SEQ, 2, 512])
    dftN = dt_in("dftN", [NT, 128, 2, NT, 128], BF16)
    dftC = dt_in("dftC", [2, CTXL, CTXL], BF16)
    dftH = dt_in("dftH", [2, 256, 256], BF16)
    identb_in = dt_in("identb", [128, 128], BF16)
    identf_in = dt_in("identf", [128, 128])
    onesblk_in = dt_in("onesblk", [48, 48])
    out = nc.dram_tensor("out", [2, SEQ, D], F32, kind="ExternalOutput").ap()
    xc = nc.dram_tensor("xc_s", [2, CTXL, D], F32).ap()
    modd = nc.dram_tensor("modd_s", [DEPTH, 3, 6 * D], F32).ap()
    ynd = nc.dram_tensor("yn_s", [NTA * 128, D], BF16).ap()
    h2l = [nc.dram_tensor(f"h2l_s{b}", [SEQ, D], BF16).ap() for b in range(2)]
    h2c = [nc.dram_tensor(f"h2c_s{b}", [CTXL, D], BF16).ap() for b in range(2)]

    with ExitStack() as top:
        S = Sched(nc, top)

        uid = [0]

        def T(st, name, shape, dt):
            uid[0] += 1
            LAST_NAMES[name] = f"{name}__{uid[0]}"
            return st.enter_context(nc.sbuf_tensor(LAST_NAMES[name], list(shape), dt))

        def PS(st, name, shape, dt=F32):
            uid[0] += 1
            return st.enter_context(nc.psum_tensor(f"{name}__{uid[0]}", list(shape), dt))

        V, A_, G_, P_ = nc.vector, nc.scalar, nc.gpsimd, nc.tensor

        identb = T(top, "identb_t", [128, 128], BF16)
        identf = T(top, "identf_t", [128, 128], F32)
        onesblk = T(top, "onesblk_t", [48, 48], F32)
        Eaff = T(top, "Eaff", [48, SEQ + CTXL], F32)
        S.dma('sp', identb[:], identb_in[:, :], writes=['identb'])
        S.dma('sp', identf[:], identf_in[:, :], writes=['identf'])
        S.dma('sp', onesblk[:], onesblk_in[:, :], writes=['onesblk'])
        S.op('dve', lambda: V.memset(Eaff[:], 1.0), writes=['Eaff'])

        def run(g):
            for _ in g:
                pass

        def run_interleaved(gens):
            gens = list(gens)
            while gens:
                for g in list(gens):
                    try:
                        next(g)
                    except StopIteration:
                        gens.remove(g)

        def rstd_g(st_key, ssq_ap, n_inv):
            S.op('dve', lambda: V.tensor_scalar(out=ssq_ap, in0=ssq_ap, scalar1=n_inv, scalar2=EPS, op0=ALU.mult, op1=ALU.add),
                 reads=[st_key], writes=[st_key])
            yield
            S.op('act', lambda: A_.activation(out=ssq_ap, in_=ssq_ap, func=AF.Sqrt), reads=[st_key], writes=[st_key])
            yield
            S.op('dve', lambda: V.reciprocal(out=ssq_ap, in_=ssq_ap), reads=[st_key], writes=[st_key])
            yield

        def rstd_from_ssq(st_key, ssq_ap, n_inv, tmp_ap):
            run(rstd_g(st_key, ssq_ap, n_inv))

        def gnorm_g(src_ap, src_keys, G, E, sq_ap, sq_key, ss_ap, ss_key, out_ap, out_key, gain_ap=None, gain_key=None, sq_eng='act', gain_eng='dve'):
            if sq_eng == 'act':
                S.op('act', lambda: A_.activation(out=sq_ap, in_=src_ap, func=AF.Square), reads=src_keys, writes=[sq_key])
            else:
                S.op('pool', lambda: G_.tensor_tensor(out=sq_ap, in0=src_ap, in1=src_ap, op=ALU.mult), reads=src_keys, writes=[sq_key])
            yield
            S.op('dve', lambda: V.tensor_reduce(out=ss_ap, in_=sq_ap.rearrange("p (g e) -> p g e", e=E), axis=AX.X, op=ALU.add),
                 reads=[sq_key], writes=[ss_key])
            yield
            yield from rstd_g(ss_key, ss_ap, 1.0 / E)
            bc = ss_ap.unsqueeze(2).to_broadcast([128, G, E])
            if gain_ap is None:
                S.op('dve', lambda: V.tensor_tensor(out=out_ap.rearrange("p (g e) -> p g e", e=E),
                                                    in0=src_ap.rearrange("p (g e) -> p g e", e=E), in1=bc, op=ALU.mult),
                     reads=list(src_keys) + [ss_key], writes=[out_key])
                yield
            else:
                S.op('dve', lambda: V.tensor_tensor(out=sq_ap.rearrange("p (g e) -> p g e", e=E),
                                                    in0=src_ap.rearrange("p (g e) -> p g e", e=E), in1=bc, op=ALU.mult),
                     reads=list(src_keys) + [ss_key], writes=[sq_key])
                yield
                if gain_eng == 'pool':
                    S.op('pool', lambda: G_.tensor_tensor(out=out_ap, in0=sq_ap, in1=gain_ap, op=ALU.mult),
                         reads=[sq_key, gain_key], writes=[out_key])
                else:
                    S.op('dve', lambda: V.tensor_tensor(out=out_ap, in0=sq_ap, in1=gain_ap, op=ALU.mult),
                         reads=[sq_key, gain_key], writes=[out_key])
                yield

        def gnorm(*a, **k):
            run(gnorm_g(*a, **k))

        def bcast_row(st, q, dst, src_row_ap, key):
            S.dma(q, dst, src_row_ap.partition_broadcast(128), writes=[key])

        for l in range(n_layers):
            last = (l == DEPTH - 1)
            lam_init = 0.8 - 0.6 * math.exp(-0.3 * l)
            xsrc = (lambda b: x_in[b]) if l == 0 else (lambda b: out[b])
            csrc = (lambda b: ctx_in[b]) if l == 0 else (lambda b: xc[b])

            with ExitStack() as st:
                cT = T(st, "cT_t", [128, 8, 3], F32)
                sT = T(st, "sT_t", [128, 8, 3], F32)
                awb = [T(st, f"awb{i}", [128, 8, 512], F32) for i in range(2)]
                abt = T(st, "abt", [3, 6 * D], F32)
                modt = T(st, "modt", [3, 6 * D], F32)
                psm = PS(st, "psm", [128, 512])
                S.dma('sp', cT[:], cT_in[:, :, :], writes=['cT'])
                S.dma('sp', abt[:], ada_b[l].partition_broadcast(3), writes=['abt'])
                S.op('act', lambda: A_.activation(out=sT[:], in_=cT[:], func=AF.Silu), reads=['cT'], writes=['sT'])
                for nb in range(12):
                    bw = awb[nb % 2]
                    S.dma('sp', bw[:], ada_w[l][:, nb * 512:(nb + 1) * 512].rearrange("(c p) n -> p c n", p=128),
                          writes=[f'awb{nb % 2}'])
                    for c in range(8):
                        S.op('pe', lambda c=c, bw=bw: P_.matmul(psm[0:3, :], lhsT=sT[:, c, :], rhs=bw[:, c, :], start=(c == 0), stop=(c == 7)),
                             reads=['sT', f'awb{nb % 2}'], writes=['psm'], sig=(c == 7))
                    S.op('dve', lambda nb=nb: V.tensor_tensor(out=modt[:, nb * 512:(nb + 1) * 512], in0=psm[0:3, :],
                                                             in1=abt[:, nb * 512:(nb + 1) * 512], op=ALU.add),
                         reads=['psm', 'abt'], writes=['modt'])
                S.dma('sp', modd[l], modt[:], reads=['modt'], writes=['modd'])
                S.barrier()
            if stop_after == 'ada' and l == n_layers - 1:
                S.finish()
                return nc

            def modrow(r, k):
                return modd[l][r, k * D:(k + 1) * D]

            for b in samples:
                sb = ExitStack()
                Ub = T(sb, "Ub", [128, NTA, 512], BF16)
                hgb = T(sb, "hgb", [128, D], F32)
                with ExitStack() as st:
                    qTC = T(st, "qTC", [64, 4, NTA * 128], BF16)
                    kTC = T(st, "kTC", [64, 4, NTA * 128], BF16)
                    vC = T(st, "vC", [128, NTA, 4, 65], BF16)
                    qTD = T(st, "qTD", [128, 2, NTA * 128], BF16)
                    kTD = T(st, "kTD", [128, 2, NTA * 128], BF16)
                    vD = T(st, "vD", [128, NTA, 4, 65], BF16)
                    lamc = T(st, "lamc", [128, 4], F32)
                    S.op('pool', lambda: G_.memset(vC[:], 1.0), writes=['vC'])
                    S.op('pool', lambda: G_.memset(vD[:], 1.0), writes=['vD'])
                    bcast_row(st, 'sp', hgb[:], hog[l], 'hgb')
                    S.op('dve', lambda: V.tensor_scalar(out=hgb[:, 512:768], in0=hgb[:, 512:768], scalar1=1.0 - lam_init, scalar2=None, op0=ALU.mult),
                         reads=['hgb'], writes=['hgb'])

                    def head_norm_g(y_ap, y_keys, g, rows0, stw, pfx, q='sp', sq_eng='act'):
                        sq, ss, ynb = stw
                        yield from gnorm_g(y_ap, y_keys, 4, 64, sq[:, 0:256], pfx + 'sq', ss[:, 0:4], pfx + 'ss', ynb[:, :], pfx + 'ynb',
                                           gain_ap=hgb[:, g * 256:(g + 1) * 256], gain_key='hgb', sq_eng=sq_eng)
                        S.dma(q, ynd[rows0:rows0 + 128, g * 256:(g + 1) * 256], ynb[:, :], reads=[pfx + 'ynb'], writes=[f'ynd{rows0}_{g}'])
                        yield

                    def head_norm(*a, **k):
                        run(head_norm_g(*a, **k))

                    with ExitStack() as p1:
                        winb = T(p1, "winb", [128, 8, IN_W], BF16)
                        A1 = [T(p1, "A1_0", [128, D], F32)]
                        sh1 = [T(p1, "sh1_0", [128, D], F32)]
                        gC = T(p1, "gC", [128, 512], F32)
                        gD = T(p1, "gD", [128, 512], F32)
                        sguTb = T(p1, "sguTb", [128, 4, 128], BF16)
                        bsT = T(p1, "bsT", [128, 4], F32)
                        dHb = T(p1, "dHb", [128, 2, 2, 256], BF16)
                        xt = [T(p1, "xt0", [128, D], F32)]
                        rp = [T(p1, f"rp{i}", [128, 2, 512], F32) for i in range(2)]
                        sqx = T(p1, "sqx", [128, D], F32)
                        ssx = T(p1, "ssx", [128, 1], F32)
                        hxb = [T(p1, f"hxb{i}", [128, D], BF16) for i in range(2)]
                        hxT = [T(p1, f"hxT{i}", [128, 8, 128], BF16) for i in range(2)]
                        zA = [T(p1, f"zA{i}", [128, 512], F32) for i in range(2)]
                        qkC = [T(p1, f"qkC{i}", [128, 512], F32) for i in range(2)]
                        qkD = [T(p1, f"qkD{i}", [128, 512], F32) for i in range(2)]
                        vnb = T(p1, "vnb", [128, 256], BF16)
                        yA = T(p1, "yA", [128, 256], F32)
                        hn_sq = T(p1, "hn_sq", [128, 256], F32)
                        hn_ss = T(p1, "hn_ss", [128, 4], F32)
                        hn_yb = T(p1, "hn_yb", [128, 256], BF16)
                        hn_sq2 = T(p1, "hn_sqb", [128, 256], F32)
                        hn_ss2 = T(p1, "hn_ssb", [128, 4], F32)
                        ZTb = T(p1, "ZTb", [128, 2, 128], BF16)
                        qk2 = T(p1, "qk2", [128, 512], F32)
                        qk3 = T(p1, "qk3", [128, 512], F32)
                        qk4 = T(p1, "qk4", [128, 512], F32)
                        ssC = T(p1, "ssC", [128, 16], F32)
                        qkbC = T(p1, "qkbC", [128, 512], BF16)
                        sqD = T(p1, "sqD", [128, 512], F32)
                        ssD = T(p1, "ssD", [128, 8], F32)
                        qkbD = T(p1, "qkbD", [128, 512], BF16)
                        psP = [PS(p1, f"psP{i}", [128, 512]) for i in range(2)]
                        psT = PS(p1, "psT", [128, 8, 128], BF16)
                        psX = PS(p1, "psX", [128, 512])
                        psG1 = PS(p1, "psG1", [128, 512])
                        psQc = PS(p1, "psQc", [128, 8, 128], BF16)
                        psQd = PS(p1, "psQd", [128, 8, 128], BF16)

                        S.dmaf('pool', lambda: G_.dma_start(out=winb[:], in_=w_in[l].rearrange("(c p) n -> p c n", p=128)), writes=['winb'])
                        S.dmaf('pool', lambda: G_.dma_start(out=sguTb[:], in_=sguT[l].rearrange("h q p -> q h p")), writes=['sguTb'])
                        S.dmaf('pool', lambda: G_.dma_start(out=dHb[:], in_=dftH.rearrange("s (t p) n -> p s t n", p=128)), writes=['dHb'])
                        S.dma('sp', bsT[:], sgubT[l], writes=['bsT'])
                        bcast_row(p1, 'sp', gC[:], qkgC[l], 'gC')
                        bcast_row(p1, 'sp', gD[:], qkgD[l], 'gD')
                        def load_mod1(r):
                            bcast_row(p1, 'sp', sqx[:], modrow(r, 1), 'sqx')
                            bcast_row(p1, 'sp', A1[0][:], n1g[l], 'A1_0')
                            S.op('dve', lambda: V.scalar_tensor_tensor(out=A1[0][:], in0=sqx[:], scalar=1.0, in1=A1[0][:], op0=ALU.add, op1=ALU.mult),
                                 reads=['sqx', 'A1_0'], writes=['A1_0'])
                            bcast_row(p1, 'sp', sh1[0][:], modrow(r, 0), 'sh1_0')

                        load_mod1(b)

                        def front(ti):
                            isctx = ti >= NT
                            mi = 0
                            sl = ti % 2
                            xb_ = xt[0]
                            xk = 'xt0'
                            if ti == NT:
                                load_mod1(2)
                                yield
                            src = csrc(b)[(ti - NT) * 128:(ti - NT + 1) * 128, :] if isctx else xsrc(b)[ti * 128:(ti + 1) * 128, :]
                            S.dma('sp', xb_[:], src, reads=['xres'], writes=[xk])
                            yield
                            S.op('act', lambda: A_.activation(out=sqx[:], in_=xb_[:], func=AF.Square), reads=[xk], writes=['sqx'])
                            yield
                            S.op('dve', lambda: V.tensor_reduce(out=ssx[:, 0:1], in_=sqx[:], axis=AX.X, op=ALU.add), reads=['sqx'], writes=['ssx'])
                            yield
                            yield from rstd_g('ssx', ssx[:, 0:1], 1.0 / D)
                            S.op('dve', lambda: V.scalar_tensor_tensor(out=sqx[:], in0=xb_[:], scalar=ssx[:, 0:1], in1=A1[mi][:], op0=ALU.mult, op1=ALU.mult),
                                 reads=[xk, 'ssx', f'A1_{mi}'], writes=['sqx'])
                            yield
                            S.op('dve', lambda: V.tensor_tensor(out=hxb[sl][:], in0=sqx[:], in1=sh1[mi][:], op=ALU.add),
                                 reads=['sqx', f'sh1_{mi}'], writes=[f'hxb{sl}'])
                            yield
                            for c in range(8):
                                S.op('pe', lambda c=c: P_.transpose(out=psT[:, c, :], in_=hxb[sl][:, c * 128:(c + 1) * 128], identity=identb[:]),
                                     reads=[f'hxb{sl}', 'identb'], writes=['psT'], sig=(c == 7))
                            yield
                            S.op('act', lambda: A_.copy(out=hxT[sl][:], in_=psT[:]), reads=['psT'], writes=[f'hxT{sl}'])
                            yield

                        def mid(ti):
                            isctx = ti >= NT
                            sl = ti % 2
                            hT = hxT[sl]
                            hk = f'hxT{sl}'
                            needA = not (isctx and last)
                            blocks = []
                            if needA:
                                blocks.append((0, 512, 'A'))
                            blocks += [(768, 1024, 'Cq'), (1024, 1536, 'CkCv'), (1536, 2048, 'Dqk'), (2048, 2304, 'Dv')]
                            for bi, (n0, n1, kind) in enumerate(blocks):
                                pp = psP[bi % 2]
                                pk = f'psP{bi % 2}'
                                for c in range(8):
                                    S.op('pe', lambda c=c, n0=n0, n1=n1, pp=pp: P_.matmul(pp[:, 0:n1 - n0], lhsT=hT[:, c, :], rhs=winb[:, c, n0:n1], start=(c == 0), stop=(c == 7)),
                                         reads=[hk, 'winb'], writes=[pk], sig=(c == 7))
                                yield
                                if kind == 'A':
                                    S.op('act', lambda pp=pp: A_.activation(out=zA[sl][:], in_=pp[:, :], func=AF.Gelu), reads=[pk], writes=[f'zA{sl}'])
                                elif kind == 'Cq':
                                    S.op('act', lambda pp=pp: A_.copy(out=qkC[sl][:, 0:256], in_=pp[:, 0:256]), reads=[pk], writes=[f'qkC{sl}'])
                                elif kind == 'CkCv':
                                    S.op('act', lambda pp=pp: A_.copy(out=qkC[sl][:, 256:512], in_=pp[:, 0:256]), reads=[pk], writes=[f'qkC{sl}'])
                                    yield
                                    S.op('act', lambda pp=pp: A_.copy(out=vC[:, ti, :, 0:64], in_=pp[:, 256:512].rearrange("p (h e) -> p h e", e=64)), reads=[pk], writes=['vC'])
                                elif kind == 'Dqk':
                                    S.op('act', lambda pp=pp: A_.copy(out=qkD[sl][:, :], in_=pp[:, :]), reads=[pk], writes=[f'qkD{sl}'])
                                else:
                                    S.op('dve', lambda pp=pp: V.tensor_copy(out=vD[:, ti, :, 0:64], in_=pp[:, 0:256].rearrange("p (h e) -> p h e", e=64)), reads=[pk], writes=['vD'])
                                yield
                            for tch in range(2):
                                for c in range(8):
                                    S.op('pe', lambda c=c, tch=tch: P_.matmul(psX[:, tch * 128:(tch + 1) * 128], lhsT=winb[:, c, 512 + tch * 128:512 + (tch + 1) * 128],
                                                                           rhs=hT[:, c, :], start=(c == 0), stop=(c == 7)),
                                         reads=[hk, 'winb'], writes=['psX'], sig=(c == 7 and tch == 1))
                            yield
                            S.op('act', lambda: A_.copy(out=ZTb[:].rearrange("p t n -> p (t n)"), in_=psX[:, 0:256]), reads=['psX'], writes=['ZTb'])
                            yield
                            for cs in range(2):
                                for tch in range(2):
                                    S.op('pe', lambda cs=cs, tch=tch: P_.matmul(psX[:, cs * 256:(cs + 1) * 256], lhsT=ZTb[:, tch, :], rhs=dHb[:, cs, tch, :], start=(tch == 0), stop=(tch == 1)),
                                         reads=['ZTb', 'dHb'], writes=['psX'], sig=(tch == 1 and cs == 1))
                            yield
                            S.op('act', lambda: A_.copy(out=Ub[:, ti, :], in_=psX[:, :]), reads=['psX'], writes=['Ub'])
                            yield

                        def backA(ti):
                            isctx = ti >= NT
                            if isctx and last:
                                return
                            sl = ti % 2
                            z = zA[sl]
                            zk = f'zA{sl}'
                            yield from gnorm_g(z[:, 256:512], [zk], 4, 64, hn_sq2[:, :], 'hnsqb', hn_ss2[:, :], 'hnssb', vnb[:, :], 'vnb')
                            for h in range(4):
                                S.op('pe', lambda h=h: P_.matmul(psG1[:, h * 64:(h + 1) * 64], lhsT=sguTb[:, h, :], rhs=vnb[:, h * 64:(h + 1) * 64], start=True, stop=True),
                                     reads=['vnb', 'sguTb'], writes=['psG1'], sig=(h == 3))
                            yield
                            for h in range(4):
                                S.op('dve', lambda h=h: V.scalar_tensor_tensor(out=yA[:, h * 64:(h + 1) * 64], in0=psG1[:, h * 64:(h + 1) * 64], scalar=bsT[:, h:h + 1],
                                                                             in1=z[:, h * 64:(h + 1) * 64], op0=ALU.add, op1=ALU.mult),
                                     reads=['psG1', 'bsT', zk], writes=['yA'])
                                yield
                            yield from head_norm_g(yA[:, :], ['yA'], 0, ti * 128, (hn_sq, hn_ss, hn_yb), 'hn', q='pool')

                        def backC(ti):
                            isctx = ti >= NT
                            sl = ti % 2
                            src = qkC[sl]
                            sk = f'qkC{sl}'
                            if isctx:
                                yield from gnorm_g(src[:, :], [sk], 16, 32, qk2[:, :], 'qk2', ssC[:, :], 'ssC', qkbC[:, :], 'qkbC', gain_ap=gC[:, :], gain_key='gC')
                            else:
                                rpt = rp[sl]
                                rk = f'rp{sl}'
                                S.dma('sp', rpt[:], ropeT[ti * 128:(ti + 1) * 128, :, :], writes=[rk])
                                yield
                                yield from gnorm_g(src[:, :], [sk], 16, 32, qk2[:, :], 'qk2', ssC[:, :], 'ssC', qk3[:, :], 'qk3', gain_ap=gC[:, :], gain_key='gC')
                                S.op('pool', lambda: G_.tensor_tensor(out=qk2[:, :], in0=qk3[:, :], in1=rpt[:, 0, :], op=ALU.mult), reads=['qk3', rk], writes=['qk2'])
                                yield
                                q4 = qk3[:, :].rearrange("p (g t e) -> p g t e", t=2, e=8)
                                o4 = qk4[:, :].rearrange("p (g t e) -> p g t e", t=2, e=8)
                                s4 = rpt[:, 1, :].rearrange("p (g t e) -> p g t e", t=2, e=8)
                                S.op('pool', lambda: G_.tensor_tensor(out=o4[:, :, 0, :], in0=q4[:, :, 1, :], in1=s4[:, :, 0, :], op=ALU.mult), reads=['qk3', rk], writes=['qk4'])
                                yield
                                S.op('pool', lambda: G_.tensor_tensor(out=o4[:, :, 1, :], in0=q4[:, :, 0, :], in1=s4[:, :, 1, :], op=ALU.mult), reads=['qk3', rk], writes=['qk4'])
                                yield
                                S.op('dve', lambda: V.tensor_tensor(out=qkbC[:, :], in0=qk2[:, :], in1=qk4[:, :], op=ALU.add), reads=['qk2', 'qk4'], writes=['qkbC'])
                                yield
                            for half, dstT, dk in ((0, qTC, 'qTC'), (1, kTC, 'kTC')):
                                for h in range(4):
                                    S.op('pe', lambda h=h, half=half: P_.transpose(out=psQc[0:64, h, :], in_=qkbC[:, half * 256 + h * 64: half * 256 + (h + 1) * 64], identity=identb[:]),
                                         reads=['qkbC', 'identb'], writes=['psQc'], sig=(h == 3))
                                yield
                                S.op('act', lambda dstT=dstT: A_.copy(out=dstT[:, :, ti * 128:(ti + 1) * 128], in_=psQc[0:64, 0:4, :]), reads=['psQc'], writes=[dk])
                                yield

                        def backD(ti):
                            sl = ti % 2
                            yield from gnorm_g(qkD[sl][:, :], [f'qkD{sl}'], 8, 64, sqD[:, :], 'sqD', ssD[:, :], 'ssD', qkbD[:, :], 'qkbD', gain_ap=gD[:, :], gain_key='gD')
                            for half, dstT, dk in ((0, qTD, 'qTD'), (1, kTD, 'kTD')):
                                for t2 in range(2):
                                    S.op('pe', lambda t2=t2, half=half: P_.transpose(out=psQd[:, t2, :], in_=qkbD[:, half * 256 + t2 * 128: half * 256 + (t2 + 1) * 128], identity=identb[:]),
                                         reads=['qkbD', 'identb'], writes=['psQd'], sig=(t2 == 1))
                                yield
                                S.op('act', lambda dstT=dstT: A_.copy(out=dstT[:, :, ti * 128:(ti + 1) * 128], in_=psQd[:, 0:2, :]), reads=['psQd'], writes=[dk])
                                yield

                        def fm(ti):
                            yield from front(ti)
                            yield from mid(ti)

                        for k in range(NTA + 2):
                            gens = []
                            if k < NTA:
                                gens.append(front(k))
                            if 0 <= k - 1 < NTA:
                                gens.append(mid(k - 1))
                            if 0 <= k - 2 < NTA:
                                gens += [backA(k - 2), backC(k - 2), backD(k - 2)]
                            run_interleaved(gens)
                        S.barrier()
                    if stop_after == 'p1':
                        S.finish()
                        return nc

                    qtiles = list(range(NT)) + ([] if last else [NT, NT + 1])

                    with ExitStack() as p3:
                        dl = T(p3, "dl", [128, 128], F32)
                        dl2 = T(p3, "dl2", [128, 64], F32)
                        ET = [T(p3, f"ET{i}", [128, 512], BF16) for i in range(4)]
                        yC = T(p3, "yC", [128, 4, 256], F32)
                        rr = T(p3, "rr", [128, 2, 4], F32)
                        hn_sq = T(p3, "hn_sq3", [128, 256], F32)
                        hn_ss = T(p3, "hn_ss3", [128, 4], F32)
                        hn_yb = T(p3, "hn_yb3", [128, 256], BF16)
                        psS = [PS(p3, f"psS{i}", [128, 512]) for i in range(4)]
                        psO = [PS(p3, f"psO{i}", [128, 4, 128]) for i in range(4)]
                        bcast_row(p3, 'sp', dl[:], dlam[l], 'dl')
                        d4 = dl[:, :].rearrange("p (a b e) -> p a b e", a=2, b=2)
                        S.op('dve', lambda: V.tensor_tensor(out=dl2[:, :].rearrange("p (a e) -> p a e", a=2), in0=d4[:, :, 0, :], in1=d4[:, :, 1, :], op=ALU.mult),
                             reads=['dl'], writes=['dl2'])
                        S.op('dve', lambda: V.tensor_reduce(out=lamc[:, 0:2], in_=dl2[:, :].rearrange("p (a e) -> p a e", a=2), axis=AX.X, op=ALU.add),
                             reads=['dl2'], writes=['lamc'])
                        S.op('act', lambda: A_.activation(out=lamc[:, 0:2], in_=lamc[:, 0:2], func=AF.Exp), reads=['lamc'], writes=['lamc'])
                        S.op('dve', lambda: V.tensor_tensor(out=lamc[:, 2:3], in0=lamc[:, 1:2], in1=lamc[:, 0:1], op=ALU.subtract), reads=['lamc'], writes=['lamc'])
                        S.op('dve', lambda: V.tensor_scalar(out=lamc[:, 3:4], in0=lamc[:, 2:3], scalar1=-lam_init, scalar2=None, op0=ALU.add), reads=['lamc'], writes=['lamc'])
                        qblocks = [(qb * 512, 512, list(range(NTA))) for qb in range(4)]
                        if not last:
                            qblocks.append((SEQ, 256, [NT, NT + 1]))
                        steps = []
                        gi = 0
                        for (q0, qn, kts) in qblocks:
                            for h in range(4):
                                for ki, kt in enumerate(kts):
                                    for m in range(2):
                                        steps.append(dict(q0=q0, qn=qn, kts=kts, h=h, m=m, ki=ki, kt=kt, g=gi,
                                                          glast=(m == 1 and ki == len(kts) - 1), blast=(h == 3 and m == 1 and ki == len(kts) - 1)))
                                gi += 1
                        NB3 = 4
                        NBE = 4
                        LOOK = 2

                        def emit_S(n):
                            sp_ = steps[n]
                            pss = psS[n % NB3]
                            et = ET[n % NBE]
                            h, m, kt, q0, qn = sp_['h'], sp_['m'], sp_['kt'], sp_['q0'], sp_['qn']
                            S.op('pe', lambda: P_.matmul(pss[:, 0:qn], lhsT=kTC[32 * m:32 * m + 32, h, kt * 128:(kt + 1) * 128],
                                                         rhs=qTC[32 * m:32 * m + 32, h, q0:q0 + qn], start=True, stop=True),
                                 reads=['kTC', 'qTC'], writes=[f'psS{n % NB3}'])
                            S.op('act', lambda: A_.activation(out=et[:, 0:qn], in_=pss[:, 0:qn], func=AF.Exp, scale=32.0 ** -0.5),
                                 reads=[f'psS{n % NB3}'], writes=[f'ET{n % NBE}'])

                        def emit_AV(n):
                            sp_ = steps[n]
                            et = ET[n % NBE]
                            h, m, kt, ki, kts, q0, qn, g = sp_['h'], sp_['m'], sp_['kt'], sp_['ki'], sp_['kts'], sp_['q0'], sp_['qn'], sp_['g']
                            nq = qn // 128
                            po = psO[2 * (g % 2) + m]
                            pk = f'psO{2 * (g % 2) + m}'
                            for qi in range(nq):
                                S.op('pe', lambda qi=qi: P_.matmul(po[:, qi, 0:65], lhsT=et[:, qi * 128:(qi + 1) * 128], rhs=vC[:, kt, h, :],
                                                                   start=(ki == 0 and qi == 0), stop=(ki == len(kts) - 1), skip_group_check=True),
                                     reads=[f'ET{n % NBE}', 'vC'], writes=[pk], sig=(qi == nq - 1))
                            if sp_['glast']:
                                p0, p1_ = psO[2 * (g % 2)], psO[2 * (g % 2) + 1]
                                k0, k1 = f'psO{2 * (g % 2)}', f'psO{2 * (g % 2) + 1}'
                                S.op('dve', lambda: V.reciprocal(out=rr[:, 0, 0:nq], in_=p0[:, 0:nq, 64]), reads=[k0], writes=['rr'])
                                S.op('dve', lambda: V.reciprocal(out=rr[:, 1, 0:nq], in_=p1_[:, 0:nq, 64]), reads=[k1], writes=['rr'])
                                S.op('dve', lambda: V.tensor_scalar(out=rr[:, 1, 0:nq], in0=rr[:, 1, 0:nq], scalar1=lamc[:, 3:4], scalar2=None, op0=ALU.mult),
                                     reads=['rr', 'lamc'], writes=['rr'])
                                for qi in range(nq):
                                    S.op('dve', lambda qi=qi: V.tensor_scalar(out=yC[:, qi, h * 64:(h + 1) * 64], in0=p0[:, qi, 0:64], scalar1=rr[:, 0, qi:qi + 1], scalar2=None, op0=ALU.mult),
                                         reads=[k0, 'rr'], writes=['yC'])
                                    S.op('dve', lambda qi=qi: V.scalar_tensor_tensor(out=yC[:, qi, h * 64:(h + 1) * 64], in0=p1_[:, qi, 0:64], scalar=rr[:, 1, qi:qi + 1],
                                                                                   in1=yC[:, qi, h * 64:(h + 1) * 64], op0=ALU.mult, op1=ALU.add),
                                         reads=[k1, 'rr', 'yC'], writes=['yC'])
                            if sp_['blast']:
                                for qi in range(nq):
                                    head_norm(yC[:, qi, :], ['yC'], 2, q0 + qi * 128, (hn_sq, hn_ss, hn_yb), 'hn')

                        for n in range(0, len(steps) + LOOK, 2):
                            for d_ in range(2):
                                if n + d_ < len(steps):
                                    emit_S(n + d_)
                            for d_ in range(2):
                                if 0 <= n + d_ - LOOK < len(steps):
                                    emit_AV(n + d_ - LOOK)
                        S.barrier()
                    if stop_after == 'p3':
                        S.finish()
                        return nc

                    with ExitStack() as p4:
                        nbf = [T(p4, f"nbf{i}", [128, NVAR, 128], F32) for i in range(2)]
                        nbb = T(p4, "nbb", [128, 4, NVAR, 128], BF16)
                        ETd = [T(p4, f"ETd{i}", [128, 7, 128], BF16) for i in range(4)]
                        yD = T(p4, "yD", [128, 256], F32)
                        rr = T(p4, "rr4", [128, 4], F32)
                        hn_sq = T(p4, "hn_sq4", [128, 256], F32)
                        hn_ss = T(p4, "hn_ss4", [128, 4], F32)
                        hn_yb = T(p4, "hn_yb4", [128, 256], BF16)
                        psS = [PS(p4, f"psS4{i}", [128, 8, 128]) for i in range(2)]
                        psO = [PS(p4, f"psO4{i}", [128, 4, 128]) for i in range(2)]
                        for h in range(4):
                            nb_ = nbf[h % 2]
                            S.dma('sp' if h % 2 == 0 else 'pool', nb_[:], nbias[l, h].rearrange("v k q -> k v q"), writes=[f'nbf{h % 2}'])
                            S.op('dve' if h % 2 == 0 else 'act', (lambda h=h, nb_=nb_: V.tensor_scalar(out=nbb[:, h, :, :], in0=nb_[:], scalar1=8.0, scalar2=None, op0=ALU.mult)) if h % 2 == 0
                                 else (lambda h=h, nb_=nb_: A_.mul(out=nbb[:, h, :, :], in_=nb_[:], mul=8.0)),
                                 reads=[f'nbf{h % 2}'], writes=['nbb'])
                        units = []
                        for t in qtiles:
                            if t < NT:
                                kl = [(kt, v) for (kt, v) in NA_PER_T[t]] + [(NT, None), (NT + 1, None)]
                            else:
                                kl = [(NT, None), (NT + 1, None)]
                            for h in range(4):
                                units.append((t, h, kl))

                        def s4_mms(n):
                            t, h, kl = units[n]
                            pss = psS[n % 2]
                            pb = 64 * (h % 2)
                            mms = []
                            for idx, (kt, v) in enumerate(kl):
                                mms.append(lambda idx=idx, kt=kt, v=v: P_.matmul(pss[:, idx, :], lhsT=kTD[pb:pb + 64, h // 2, kt * 128:(kt + 1) * 128],
                                                                                 rhs=qTD[pb:pb + 64, h // 2, t * 128:(t + 1) * 128], start=True, stop=(v is None)))
                                if v is not None:
                                    mms.append(lambda idx=idx, v=v: P_.matmul(pss[:, idx, :], lhsT=identb[:], rhs=nbb[:, h, v, :], start=False, stop=True))
                            return mms

                        def emit_S4pair(p):
                            ns = [2 * p, 2 * p + 1]
                            lists = [s4_mms(n) for n in ns]
                            L_ = len(lists[0])
                            for i_ in range(L_):
                                for n, mm in zip(ns, lists):
                                    S.op('pe', mm[i_], reads=['kTD', 'qTD', 'identb', 'nbb'], writes=[f'psS4{n % 2}'], sig=(i_ == L_ - 1))
                            for n in ns:
                                t, h, kl = units[n]
                                nk = len(kl)
                                pss = psS[n % 2]
                                et = ETd[n % 4]
                                S.op('act', lambda pss=pss, et=et, nk=nk: A_.activation(out=et[:, 0:nk, :], in_=pss[:, 0:nk, :], func=AF.Exp, scale=0.125),
                                     reads=[f'psS4{n % 2}'], writes=[f'ETd{n % 4}'])

                        def emit_AV4(n):
                            t, h, kl = units[n]
                            et = ETd[n % 4]
                            nk = len(kl)
                            tp = (n // 4) % 2
                            po = psO[tp]
                            pk = f'psO4{tp}'
                            for idx, (kt, v) in enumerate(kl):
                                S.op('pe', lambda idx=idx, kt=kt: P_.matmul(po[:, h, 0:65], lhsT=et[:, idx, :], rhs=vD[:, kt, h, :], start=(idx == 0), stop=(idx == nk - 1)),
                                     reads=[f'ETd{n % 4}', 'vD'], writes=[pk], sig=(idx == nk - 1))
                            if h == 3:
                                S.op('dve', lambda: V.reciprocal(out=rr[:, 0:4], in_=po[:, 0:4, 64]), reads=[pk], writes=['rr4'])
                                for hh in range(4):
                                    S.op('dve', lambda hh=hh: V.tensor_scalar(out=yD[:, hh * 64:(hh + 1) * 64], in0=po[:, hh, 0:64], scalar1=rr[:, hh:hh + 1], scalar2=None, op0=ALU.mult),
                                         reads=[pk, 'rr4'], writes=['yD'])
                                head_norm(yD[:, :], ['yD'], 3, t * 128, (hn_sq, hn_ss, hn_yb), 'hn')

                        npairs = len(units) // 2
                        for p in range(npairs + 1):
                            if p < npairs:
                                emit_S4pair(p)
                            if p - 1 >= 0:
                                emit_AV4(2 * (p - 1))
                                emit_AV4(2 * (p - 1) + 1)
                        S.barrier()
                    if stop_after == 'p4':
                        S.finish()
                        return nc

                with ExitStack() as p5:
                    woutb = T(p5, "woutb", [128, 8, D], BF16)
                    g1v = [T(p5, f"g1v{i}", [128, D], F32) for i in range(2)]
                    A2 = [T(p5, f"A2_{i}", [128, D], F32) for i in range(2)]
                    sh2 = [T(p5, f"sh2_{i}", [128, D], F32) for i in range(2)]
                    tmpv = T(p5, "tmpv5", [128, D], F32)
                    rwf = T(p5, "rwf", [128, 8, 48], F32)
                    ynb = [T(p5, f"ynb{i}", [128, D], BF16) for i in range(2)]
                    ynT = T(p5, "ynT", [128, 8, 128], BF16)
                    xt = [T(p5, f"xt5_{i}", [128, D], F32) for i in range(2)]
                    xn = [T(p5, f"xn{i}", [128, D], F32) for i in range(2)]
                    sq5 = T(p5, "sq5", [128, D], F32)
                    ss5 = T(p5, "ss5", [128, 1], F32)
                    h2f = T(p5, "h2f", [128, D], F32)
                    h2b = T(p5, "h2b", [128, D], BF16)
                    h2T = T(p5, "h2T", [128, 8, 128], F32)
                    psT = PS(p5, "psT5", [128, 8, 128], BF16)
                    psM = PS(p5, "psM", [128, 2, 512])
                    psH = PS(p5, "psH", [128, 8, 128])
                    psR = PS(p5, "psR", [128, 512])
                    S.dmaf('pool', lambda: G_.dma_start(out=woutb[:], in_=w_out[l].rearrange("(c p) n -> p c n", p=128)), writes=['woutb'])
                    S.op('dve', lambda: V.memset(rwf[:], 0.0), writes=['rwf'])
                    S.dma('sp', rwf[:, :, 32 * b:32 * b + 16], rw[l].rearrange("(c p) e -> p c e", p=128), reads=['rwf'], writes=['rwf'])
                    for i, r in enumerate((b, 2)):
                        if i == 1 and last:
                            continue
                        bcast_row(p5, 'sp', g1v[i][:], modrow(r, 2), f'g1v{i}')
                        bcast_row(p5, 'sp', tmpv[:], modrow(r, 4), 'tmpv5')
                        bcast_row(p5, 'sp', A2[i][:], n2g[l], f'A2_{i}')
                        S.op('dve', lambda i=i: V.scalar_tensor_tensor(out=A2[i][:], in0=tmpv[:], scalar=1.0, in1=A2[i][:], op0=ALU.add, op1=ALU.mult),
                             reads=['tmpv5', f'A2_{i}'], writes=[f'A2_{i}'])
                        bcast_row(p5, 'sp', sh2[i][:], modrow(r, 3), f'sh2_{i}')
                    qtiles = list(range(NT)) + ([] if last else [NT, NT + 1])
                    M = 32 * b + 16
                    dt = [T(p5, f"dt{i}", [128, 2, NT, 128], BF16) for i in range(2)]
                    f_sq = T(p5, "f_sq", [128, 256], F32)
                    f_ss = T(p5, "f_ss", [128, 4], F32)
                    f_yb = T(p5, "f_yb", [128, 256], BF16)
                    psY = PS(p5, "psY", [128, 512])

                    def fourier_load(j):
                        isctx = j >= NT
                        tb = dt[j % 2]
                        tk = f'dt{j % 2}'
                        if not isctx:
                            S.dma('pool', tb[:], dftN[j], writes=[tk])
                        else:
                            jj = j - NT
                            for cs in range(2):
                                S.dma('pool', tb[:, cs, 0:2, :], dftC[cs][:, jj * 128:(jj + 1) * 128].rearrange("(i p) n -> p i n", p=128), writes=[tk])

                    def fourier_group(j):
                        isctx = j >= NT
                        tb = dt[j % 2]
                        tk = f'dt{j % 2}'
                        ins = [(0, NT), (1, NT + 1)] if isctx else [(i, i) for i in range(NT)]
                        n_mm = 2 * len(ins)
                        k = 0
                        for cs in range(2):
                            for (ii, ti) in ins:
                                S.op('pe', lambda cs=cs, ii=ii, ti=ti, k=k: P_.matmul(psY[:, 0:256], lhsT=tb[:, cs, ii, :], rhs=Ub[:, ti, cs * 256:(cs + 1) * 256],
                                                                                   start=(k == 0), stop=(k == n_mm - 1)),
                                     reads=[tk, 'Ub'], writes=['psY'], sig=(k == n_mm - 1))
                                k += 1
                        head_norm(psY[:, 0:256], ['psY'], 1, j * 128, (f_sq, f_ss, f_yb), 'fh', q='pool')


                    def p5A(ti):
                        isctx = ti >= NT
                        mi = 1 if isctx else 0
                        yb_ = ynb[ti % 2]
                        yk = f'ynb{ti % 2}'
                        xb_ = xt[ti % 2]
                        xk = f'xt5_{ti % 2}'
                        xn_ = xn[ti % 2]
                        xnk = f'xn{ti % 2}'
                        S.dma('sp', yb_[:], ynd[ti * 128:(ti + 1) * 128, :], reads=[f'ynd{ti * 128}_{g}' for g in range(4)], writes=[yk])
                        yield
                        src = csrc(b)[(ti - NT) * 128:(ti - NT + 1) * 128, :] if isctx else xsrc(b)[ti * 128:(ti + 1) * 128, :]
                        dst = xc[b][(ti - NT) * 128:(ti - NT + 1) * 128, :] if isctx else out[b][ti * 128:(ti + 1) * 128, :]
                        S.dma('sp', xb_[:], src, reads=['xres'], writes=[xk])
                        yield
                        for c in range(8):
                            S.op('pe', lambda c=c: P_.transpose(out=psT[:, c, :], in_=yb_[:, c * 128:(c + 1) * 128], identity=identb[:]),
                                 reads=[yk, 'identb'], writes=['psT5'], sig=(c == 7))
                        yield
                        S.op('act', lambda: A_.copy(out=ynT[:], in_=psT[:]), reads=['psT5'], writes=['ynT'])
                        yield
                        for hf in range(2):
                            for c in range(8):
                                S.op('pe', lambda c=c, hf=hf: P_.matmul(psM[:, hf, :], lhsT=ynT[:, c, :], rhs=woutb[:, c, hf * 512:(hf + 1) * 512], start=(c == 0), stop=(c == 7)),
                                     reads=['ynT', 'woutb'], writes=['psM'], sig=(c == 7 and hf == 1))
                        yield
                        S.op('dve', lambda: V.tensor_tensor(out=xn_[:], in0=psM[:].rearrange("p a n -> p (a n)"), in1=g1v[mi][:], op=ALU.mult),
                             reads=['psM', f'g1v{mi}'], writes=[xnk])
                        yield
                        S.op('dve', lambda: V.tensor_tensor(out=xn_[:], in0=xn_[:], in1=xb_[:], op=ALU.add), reads=[xnk, xk], writes=[xnk])
                        yield
                        S.dma('pool', dst, xn_[:], reads=[xnk], writes=['xres_w'])
                        yield

                    def p5B(ti):
                        isctx = ti >= NT
                        mi = 1 if isctx else 0
                        xn_ = xn[ti % 2]
                        xnk = f'xn{ti % 2}'
                        S.op('act', lambda: A_.activation(out=sq5[:], in_=xn_[:], func=AF.Square), reads=[xnk], writes=['sq5'])
                        yield
                        S.op('dve', lambda: V.tensor_reduce(out=ss5[:, 0:1], in_=sq5[:], axis=AX.X, op=ALU.add), reads=['sq5'], writes=['ss5'])
                        yield
                        yield from rstd_g('ss5', ss5[:, 0:1], 1.0 / D)
                        S.op('dve', lambda: V.scalar_tensor_tensor(out=sq5[:], in0=xn_[:], scalar=ss5[:, 0:1], in1=A2[mi][:], op0=ALU.mult, op1=ALU.mult),
                             reads=[xnk, 'ss5', f'A2_{mi}'], writes=['sq5'])
                        yield
                        S.op('dve', lambda: V.tensor_tensor(out=h2f[:], in0=sq5[:], in1=sh2[mi][:], op=ALU.add), reads=['sq5', f'sh2_{mi}'], writes=['h2f'])
                        yield
                        S.op('act', lambda: A_.copy(out=h2b[:], in_=h2f[:]), reads=['h2f'], writes=['h2b'])
                        yield
                        hdst = h2c[b][(ti - NT) * 128:(ti - NT + 1) * 128, :] if isctx else h2l[b][ti * 128:(ti + 1) * 128, :]
                        S.dma('pool', hdst, h2b[:], reads=['h2b'], writes=['h2d'])
                        yield
                        for c in range(8):
                            S.op('pe', lambda c=c: P_.transpose(out=psH[:, c, :], in_=h2f[:, c * 128:(c + 1) * 128], identity=identf[:]),
                                 reads=['h2f', 'identf'], writes=['psH'], sig=(c == 7))
                        yield
                        S.op('dve', lambda: V.tensor_copy(out=h2T[:], in_=psH[:]), reads=['psH'], writes=['h2T'])
                        yield
                        for c in range(8):
                            S.op('pe', lambda c=c: P_.matmul(psR[0:M, 0:128], lhsT=rwf[:, c, 0:M], rhs=h2T[:, c, :], start=(c == 0), stop=(c == 7)),
                                 reads=['rwf', 'h2T'], writes=['psR'], sig=(c == 7))
                        yield
                        S.op('act', lambda: A_.activation(out=Eaff[32 * b:32 * b + 16, ti * 128:(ti + 1) * 128], in_=psR[32 * b:32 * b + 16, 0:128], func=AF.Exp),
                             reads=['psR'], writes=['Eaff'])
                        yield

                    FLEAD = 2
                    fourier_load(qtiles[0])
                    for j_ in range(min(FLEAD, len(qtiles))):
                        if j_ + 1 < len(qtiles):
                            fourier_load(qtiles[j_ + 1])
                        fourier_group(qtiles[j_])
                    for k in range(len(qtiles) + 1):
                        if k + FLEAD < len(qtiles):
                            if k + FLEAD + 1 < len(qtiles):
                                fourier_load(qtiles[k + FLEAD + 1])
                            fourier_group(qtiles[k + FLEAD])
                        gens = []
                        if k < len(qtiles):
                            gens.append(p5A(qtiles[k]))
                        if k - 1 >= 0:
                            gens.append(p5B(qtiles[k - 1]))
                        run_interleaved(gens)
                    S.barrier()
                sb.close()
                if stop_after == 'p5' and b == samples[-1]:
                    S.finish()
                    return nc

            with ExitStack() as p6:
                nslot = 512 + (0 if last else 64)
                wbuf = [[T(p6, f"w{n}{i}", [128, 8, D], BF16) for n in ('g', 'u', 'd')] for i in range(2)]
                g2v = [T(p6, f"g2v{i}", [128, D], F32) for i in range(3)]
                aff = T(p6, "aff", [48, SEQ + CTXL], F32)
                aff0 = T(p6, "aff0", [48, SEQ + CTXL], F32)
                rec = T(p6, "rec", [48, 512], F32)
                vals = T(p6, "vals", [48, CAP + CAPC], F32)
                idxu = T(p6, "idxu", [48, CAP + CAPC], U32)
                idxf = T(p6, "idxf", [48, CAP + CAPC], F32)
                gateT = T(p6, "gateT", [128, 3, 48], F32)
                idxI = T(p6, "idxI", [128, 3, 48], I32)
                idxS = T(p6, "idxS", [128, 3, 48], I32)
                xg = [[T(p6, f"xg{p_}_{i}", [128, D], BF16) for i in range(4)] for p_ in range(2)]
                xgc = [[T(p6, f"xgc{p_}_{i}", [32, D], BF16) for i in range(2)] for p_ in range(2)]
                xgT = T(p6, "xgT", [128, 8, 576], BF16)
                hidT = T(p6, "hidT", [128, 8, 576], BF16)
                sil = [T(p6, f"sil{i}", [128, 576], F32) for i in range(2)]
                osb = [T(p6, f"osb{i}", [128, D], F32) for i in range(2)]
                psA = PS(p6, "psA6", [128, 512])
                psG = [PS(p6, f"psG6{i}", [128, 512]) for i in range(2)]
                psU = [PS(p6, f"psU6{i}", [128, 512]) for i in range(2)]
                psT = PS(p6, "psT6", [128, 8, 128], BF16)
                psD = [PS(p6, f"psD6{i}", [128, 512]) for i in range(2)]
                for i, r in enumerate((0, 1, 2)):
                    bcast_row(p6, 'sp', g2v[i][:], modrow(r, 5), f'g2v{i}')
                segs = [(0, 512), (512, 512), (1024, 512), (1536, 512)] + ([] if last else [(SEQ, 256)])
                for (c0, cn) in segs:
                    S.op('pe', lambda c0=c0, cn=cn: P_.matmul(psA[0:48, 0:cn], lhsT=onesblk[:, :], rhs=Eaff[:, c0:c0 + cn], start=True, stop=True),
                         reads=['onesblk', 'Eaff'], writes=['psA6'])
                    S.op('dve', lambda cn=cn: V.reciprocal(out=rec[:, 0:cn], in_=psA[0:48, 0:cn]), reads=['psA6'], writes=['rec'])
                    S.op('dve', lambda c0=c0, cn=cn: V.tensor_tensor(out=aff[:, c0:c0 + cn], in0=Eaff[:, c0:c0 + cn], in1=rec[:, 0:cn], op=ALU.mult),
                         reads=['Eaff', 'rec'], writes=['aff'])
                    S.op('act', lambda c0=c0, cn=cn: A_.copy(out=aff0[:, c0:c0 + cn], in_=aff[:, c0:c0 + cn]), reads=['aff'], writes=['aff0'])
                tsegs = [(0, SEQ, CAP, 0)] + ([] if last else [(SEQ, CTXL, CAPC, CAP)])
                for (c0, cn, cap, o0) in tsegs:
                    for j in range(cap // 8):
                        vk = f'vals{o0}_{j}'
                        S.op('dve', lambda c0=c0, cn=cn, j=j, o0=o0: V.max(out=vals[:, o0 + 8 * j:o0 + 8 * j + 8], in_=aff[:, c0:c0 + cn]), reads=['aff'], writes=[vk])
                        S.op('dve', lambda c0=c0, cn=cn, j=j, o0=o0: V.match_replace(out=aff[:, c0:c0 + cn], in_to_replace=vals[:, o0 + 8 * j:o0 + 8 * j + 8], in_values=aff[:, c0:c0 + cn], imm_value=-1.0),
                             reads=['aff', vk], writes=['aff'])
                        S.op('dve', lambda c0=c0, cn=cn, j=j, o0=o0: V.max_index(out=idxu[:, o0 + 8 * j:o0 + 8 * j + 8], in_max=vals[:, o0 + 8 * j:o0 + 8 * j + 8], in_values=aff0[:, c0:c0 + cn]),
                             reads=['aff0', vk], writes=['idxu'])
                ncol_ = CAP + (0 if last else CAPC)
                S.op('dve', lambda: V.tensor_copy(out=idxf[:, 0:ncol_], in_=idxu[:, 0:ncol_]), reads=['idxu'], writes=['idxf', 'vals_all'])
                tl = [(0, 128, 0), (128, 128, 1)] + ([] if last else [(CAP, CAPC, 2)])
                for (o0, on, j) in tl:
                    for srcT, dstT, dk in ((vals, gateT, 'gateT'), (idxf, idxI, 'idxI')):
                        S.op('pe', lambda srcT=srcT, o0=o0, on=on: P_.transpose(out=psA[0:on, 0:48], in_=srcT[0:48, o0:o0 + on], identity=identf[0:48, 0:48]),
                             reads=['vals_all', 'idxf', 'identf'], writes=['psA6'])
                        S.op('dve', lambda dstT=dstT, on=on, j=j: V.tensor_copy(out=dstT[0:on, j, :], in_=psA[0:on, 0:48]), reads=['psA6'], writes=[dk])
                        if dk == 'idxI':
                            off = float(SEQ if j < 2 else CTXL)
                            S.op('dve', lambda on=on, j=j: V.tensor_copy(out=idxS[0:on, j, 0:32], in_=psA[0:on, 0:32]), reads=['psA6'], writes=['idxS'])
                            S.op('dve', lambda on=on, j=j, off=off: V.tensor_scalar(out=idxS[0:on, j, 32:48], in0=psA[0:on, 32:48], scalar1=off, scalar2=None, op0=ALU.add),
                                 reads=['psA6'], writes=['idxS'])

                def load_w(e):
                    bufs = wbuf[e % 2]
                    for n, (wt, bt) in enumerate(zip((wg, wu, wd), bufs)):
                        S.dmaf('pool', lambda wt=wt, bt=bt: G_.dma_start(out=bt[:], in_=wt[l, e].rearrange("(c p) f -> p c f", p=128)),
                               writes=[f'w{"gud"[n]}{e % 2}'])

                def gather(e):
                    par = e % 2
                    for bb in range(2):
                        for hf in range(2):
                            j = bb * 2 + hf
                            S.dmaf('pool', lambda j=j, bb=bb, hf=hf: G_.indirect_dma_start(out=xg[par][j][:, :], out_offset=None, in_=h2l[bb][:, :],
                                                                                         in_offset=bass.IndirectOffsetOnAxis(ap=idxI[:, hf, 32 * bb + e:32 * bb + e + 1], axis=0)),
                                   reads=['idxI', 'h2d'], writes=[f'xg{par}_{j}'])
                        if not last:
                            S.dmaf('pool', lambda bb=bb: G_.indirect_dma_start(out=xgc[par][bb][:, :], out_offset=None, in_=h2c[bb][:, :],
                                                                             in_offset=bass.IndirectOffsetOnAxis(ap=idxI[0:32, 2, 32 * bb + e:32 * bb + e + 1], axis=0)),
                                   reads=['idxI', 'h2d'], writes=[f'xgc{par}_{bb}'])

                load_w(0)
                gather(0)
                for e in range(NE):
                    par = e % 2
                    if e + 1 < NE:
                        load_w(e + 1)
                        gather(e + 1)
                    wgb, wub, wdb = wbuf[e % 2]
                    wk = [f'w{n}{e % 2}' for n in 'gud']
                    for j in range(4):
                        for c in range(8):
                            S.op('pe', lambda j=j, c=c: P_.transpose(out=psT[:, c, :], in_=xg[par][j][:, c * 128:(c + 1) * 128], identity=identb[:]),
                                 reads=[f'xg{par}_{j}', 'identb'], writes=['psT6'], sig=(c == 7))
                        S.op('act', lambda j=j: A_.copy(out=xgT[:, :, j * 128:(j + 1) * 128], in_=psT[:]), reads=['psT6'], writes=['xgT'])
                    if not last:
                        for bb in range(2):
                            for c in range(8):
                                S.op('pe', lambda bb=bb, c=c: P_.transpose(out=psT[:, c, 0:32], in_=xgc[par][bb][0:32, c * 128:(c + 1) * 128], identity=identb[0:32, 0:32]),
                                     reads=[f'xgc{par}_{bb}', 'identb'], writes=['psT6'], sig=(c == 7))
                            S.op('act', lambda bb=bb: A_.copy(out=xgT[:, :, 512 + 32 * bb:512 + 32 * bb + 32], in_=psT[:, :, 0:32]), reads=['psT6'], writes=['xgT'])
                    for f in range(8):
                        fp = f % 2
                        for (wb_, ps_, pk, wkk, coff) in ((wgb, psG[fp], f'psG6{fp}', wk[0], 0), (wub, psU[fp], f'psU6{fp}', wk[1], 64)):
                            for c in range(8):
                                S.op('pe', lambda wb_=wb_, ps_=ps_, c=c, f=f: P_.matmul(ps_[:, :], lhsT=wb_[:, c, f * 128:(f + 1) * 128], rhs=xgT[:, c, 0:512], start=(c == 0), stop=(c == 7)),
                                     reads=[wkk, 'xgT'], writes=[pk], sig=(c == 7))
                            if not last:
                                for c in range(8):
                                    S.op('pe', lambda wb_=wb_, c=c, f=f, coff=coff: P_.matmul(psA[:, coff:coff + 64], lhsT=wb_[:, c, f * 128:(f + 1) * 128], rhs=xgT[:, c, 512:576], start=(c == 0), stop=(c == 7)),
                                         reads=[wkk, 'xgT'], writes=['psA6'], sig=(c == 7))
                        S.op('act', lambda fp=fp: A_.activation(out=sil[fp][:, 0:512], in_=psG[fp][:, :], func=AF.Silu), reads=[f'psG6{fp}'], writes=[f'sil{fp}'])
                        S.op('dve', lambda f=f, fp=fp: V.tensor_tensor(out=hidT[:, f, 0:512], in0=psU[fp][:, :], in1=sil[fp][:, 0:512], op=ALU.mult), reads=[f'psU6{fp}', f'sil{fp}'], writes=['hidT'])
                        if not last:
                            S.op('act', lambda fp=fp: A_.activation(out=sil[fp][:, 512:576], in_=psA[:, 0:64], func=AF.Silu), reads=['psA6'], writes=[f'sil{fp}'])
                            S.op('dve', lambda f=f, fp=fp: V.tensor_tensor(out=hidT[:, f, 512:576], in0=psA[:, 64:128], in1=sil[fp][:, 512:576], op=ALU.mult), reads=['psA6', f'sil{fp}'], writes=['hidT'])
                    jobs = [(bb * 2 + hf, 128, bb, hf, False) for bb in range(2) for hf in range(2)]
                    if not last:
                        jobs += [(None, 32, 0, 2, True), (None, 32, 1, 2, True)]
                    for jn, (j, mrows, bb, hf, isc) in enumerate(jobs):
                        s0 = (512 + 32 * bb) if isc else j * 128
                        ob = osb[jn % 2]
                        ok_ = f'osb{jn % 2}'
                        gi = 2 if isc else bb
                        for hh in range(2):
                            for f in range(8):
                                S.op('pe', lambda s0=s0, mrows=mrows, hh=hh, f=f: P_.matmul(psD[hh][0:mrows, :], lhsT=hidT[:, f, s0:s0 + mrows], rhs=wdb[:, f, hh * 512:(hh + 1) * 512], start=(f == 0), stop=(f == 7)),
                                     reads=['hidT', wk[2]], writes=[f'psD6{hh}'], sig=(f == 7))
                            S.op('dve', lambda ob=ob, mrows=mrows, hf=hf, bb=bb, e=e, gi=gi, hh=hh: V.scalar_tensor_tensor(out=ob[0:mrows, hh * 512:(hh + 1) * 512], in0=psD[hh][0:mrows, :],
                                                                                                            scalar=gateT[0:mrows, hf, 32 * bb + e:32 * bb + e + 1], in1=g2v[gi][0:mrows, hh * 512:(hh + 1) * 512], op0=ALU.mult, op1=ALU.mult),
                                 reads=[f'psD6{hh}', 'gateT', f'g2v{gi}'], writes=[ok_])
                        tgt = xc.rearrange("b n d -> (b n) d") if isc else out.rearrange("b n d -> (b n) d")
                        S.dmaf('pool', lambda ob=ob, mrows=mrows, hf=hf, bb=bb, e=e, tgt=tgt: G_.indirect_dma_start(out=tgt, out_offset=bass.IndirectOffsetOnAxis(ap=idxS[0:mrows, hf, 32 * bb + e:32 * bb + e + 1], axis=0),
                                                                                                           in_=ob[0:mrows, :], in_offset=None, compute_op=ALU.add),
                               reads=[ok_, 'idxS'], writes=['xres_w'])
                S.barrier()
        S.finish()
    return nc


_CONSTS = None


def prep_shared(inp):
    global _CONSTS
    if _CONSTS is None:
        _CONSTS = _consts()
    f = lambda a: np.ascontiguousarray(np.asarray(a, dtype=np.float32))
    sh = dict(_CONSTS)
    sh["ada_w"] = f(inp["ada_w"]); sh["ada_b"] = f(inp["ada_b"])
    sh["n1g"] = f(inp["norm1_g"]); sh["n2g"] = f(inp["norm2_g"])
    sh["w_in"] = f(inp["w_in"]); sh["w_out"] = f(inp["w_out"]); sh["hog"] = f(inp["head_out_g"])
    sh["sguT"] = np.ascontiguousarray(f(inp["sgu_w"]).transpose(0, 1, 3, 2))
    sh["sgubT"] = np.ascontiguousarray(f(inp["sgu_b"]).transpose(0, 2, 1))
    sh["qkgC"] = np.ascontiguousarray(np.concatenate([np.tile(f(inp["diff_qn_g"]), (1, 8)), np.tile(f(inp["diff_kn_g"]), (1, 8))], axis=1))
    sh["qkgD"] = np.ascontiguousarray(np.concatenate([np.tile(f(inp["na_qn_g"]), (1, 4)), np.tile(f(inp["na_kn_g"]), (1, 4))], axis=1))
    sh["dlam"] = np.ascontiguousarray(f(inp["diff_lambda"]).reshape(DEPTH, 128))
    sh["nbias"] = _na_bias_host(f(inp["na_rpb"]))
    sh["rw"] = f(inp["router_w"])
    sh["wg"] = f(inp["exp_w_gate"]); sh["wu"] = f(inp["exp_w_up"]); sh["wd"] = f(inp["exp_w_down"])
    return sh


def prep_core(inp, shared, core):
    m = dict(shared)
    b0 = 2 * core
    m["x"] = np.ascontiguousarray(np.asarray(inp["x"][b0:b0 + 2], dtype=np.float32))
    m["ctx"] = np.ascontiguousarray(np.asarray(inp["ctx"][b0:b0 + 2], dtype=np.float32))
    cv = np.concatenate([np.asarray(inp["c"][b0:b0 + 2], dtype=np.float32), np.asarray(inp["c_ctx"], dtype=np.float32)[None]], axis=0)
    m["cT"] = np.ascontiguousarray(cv.reshape(3, 8, 128).transpose(2, 1, 0))
    return m


def kernel(**inputs):
    inp = {k: np.asarray(v) for k, v in inputs.items()}
    shared = prep_shared(inp)
    nc = build_program()
    in_maps = [prep_core(inp, shared, c) for c in range(8)]
    res = run_bass_kernel_spmd(nc, in_maps, core_ids=list(range(8)))
    outs = [np.asarray(res.results[c]["out"], dtype=np.float32) for c in range(8)]
    return np.concatenate(outs, axis=0)
```
